# Optimizing a Trainium2 kernel written in Bass

```python
import math
import jax
import jax.numpy as jnp
from jax import lax
import numpy as np


D_MODEL = 1024
BATCH = 4
SEQ = 4096
DEPTH = 4

D_MIX = D_MODEL
FOX_HEADS = 8
FOX_HEAD_DIM = 64
FOX_WIDTH = FOX_HEADS * FOX_HEAD_DIM
Q_BLOCK = 128
S5_WIDTH = D_MIX - FOX_WIDTH
S5_GROUP = 16
S5_GROUPS = S5_WIDTH // S5_GROUP
S5_STATE = 64
EVEN_IN = 3 * FOX_WIDTH + FOX_HEADS + S5_WIDTH
MLSTM_HEADS = 8
MLSTM_HEAD_DIM = D_MIX // MLSTM_HEADS
MLSTM_CHUNK = 64
CONV_WIDTH = 4
ODD_IN = 4 * D_MIX + 2 * MLSTM_HEADS
N_GROUPS = 4
EXPERTS_PER_GROUP = 4
N_EXPERTS = N_GROUPS * EXPERTS_PER_GROUP
EXPERT_TOP_K = 2
D_EXPERT = 256
DN_ALPHA = (2 * DEPTH) ** 0.25
DN_BETA = (8 * DEPTH) ** -0.25
N_EVEN = (DEPTH + 1) // 2
N_ODD = DEPTH // 2
LN_EPS = 1e-5

kernel_name = 'fox_s5_mlstm_hmoe_deepnorm_trunk'


def layer_norm(x, g, b):
    xf = x.astype(jnp.float32)
    mu = jnp.mean(xf, axis=-1, keepdims=True)
    var = jnp.mean(jnp.square(xf - mu), axis=-1, keepdims=True)
    y = (xf - mu) * lax.rsqrt(var + LN_EPS) * g.astype(jnp.float32) + b.astype(jnp.float32)
    return y.astype(x.dtype)


def forgetting_attention(q, k, v, f_logit):
    B, S, H, Dh = q.shape
    nb = S // Q_BLOCK
    c = jnp.cumsum(jax.nn.log_sigmoid(f_logit.astype(jnp.float32)), axis=1).transpose(0, 2, 1)
    qh = q.transpose(0, 2, 1, 3)
    kh = k.transpose(0, 2, 1, 3)
    vh = v.transpose(0, 2, 1, 3)
    q_blocks = qh.reshape(B, H, nb, Q_BLOCK, Dh).transpose(2, 0, 1, 3, 4)
    c_blocks = c.reshape(B, H, nb, Q_BLOCK).transpose(2, 0, 1, 3)
    pos_k = jnp.arange(S)
    scale = Dh ** -0.5

    def one_block(args):
        qb, cb, i = args
        s = jnp.einsum('bhqd,bhkd->bhqk', qb, kh).astype(jnp.float32) * scale
        s = s + cb[..., :, None] - c[..., None, :]
        pos_q = i * Q_BLOCK + jnp.arange(Q_BLOCK)
        mask = pos_k[None, :] <= pos_q[:, None]
        s = jnp.where(mask, s, -jnp.inf)
        p = jax.nn.softmax(s, axis=-1).astype(vh.dtype)
        return jnp.einsum('bhqk,bhkd->bhqd', p, vh)

    out = lax.map(one_block, (q_blocks, c_blocks, jnp.arange(nb)))
    return out.transpose(1, 0, 3, 2, 4).reshape(B, S, H, Dh)


def s5_scan(u, a_re, a_im, log_dt, b_re, b_im, c_re, c_im, d_skip):
    dt = jnp.exp(log_dt)[:, None]
    mag = jnp.exp(a_re * dt)
    lb_re = mag * jnp.cos(a_im * dt)
    lb_im = mag * jnp.sin(a_im * dt)
    num_re = lb_re - 1.0
    num_im = lb_im
    den = a_re * a_re + a_im * a_im
    z_re = (num_re * a_re + num_im * a_im) / den
    z_im = (num_im * a_re - num_re * a_im) / den
    bb_re = z_re[..., None] * b_re - z_im[..., None] * b_im
    bb_im = z_re[..., None] * b_im + z_im[..., None] * b_re
    bu_re = jnp.einsum('bsgc,gpc->bsgp', u, bb_re)
    bu_im = jnp.einsum('bsgc,gpc->bsgp', u, bb_im)
    a_re_t = jnp.broadcast_to(lb_re, bu_re.shape)
    a_im_t = jnp.broadcast_to(lb_im, bu_im.shape)

    def combine(e1, e2):
        a1r, a1i, b1r, b1i = e1
        a2r, a2i, b2r, b2i = e2
        return (a2r * a1r - a2i * a1i,
                a2r * a1i + a2i * a1r,
                a2r * b1r - a2i * b1i + b2r,
                a2r * b1i + a2i * b1r + b2i)

    _, _, h_re, h_im = lax.associative_scan(combine, (a_re_t, a_im_t, bu_re, bu_im), axis=1)
    y = jnp.einsum('bsgp,gcp->bsgc', h_re, c_re) - jnp.einsum('bsgp,gcp->bsgc', h_im, c_im)
    return y + d_skip * u


def causal_dwconv(x, w, b):
    C = x.shape[-1]
    y = lax.conv_general_dilated(x, w[:, None, :], window_strides=(1,), padding=[(CONV_WIDTH - 1, 0)],
                                 dimension_numbers=('NWC', 'WIO', 'NWC'), feature_group_count=C)
    return y + b


def mlstm_chunkwise(q, k, v, i_pre, f_pre):
    B, S, H, Dh = q.shape
    L = MLSTM_CHUNK
    NC = S // L
    f32 = jnp.float32

    def to_chunks(t):
        return t.reshape(B, NC, L, H, Dh).transpose(0, 3, 1, 2, 4)

    qc = to_chunks(q)
    kc = to_chunks(k * (Dh ** -0.5))
    vc = to_chunks(v)
    log_i = i_pre.astype(f32).reshape(B, NC, L, H).transpose(0, 3, 1, 2)
    log_f = jax.nn.log_sigmoid(f_pre.astype(f32)).reshape(B, NC, L, H).transpose(0, 3, 1, 2)
    bcum = jnp.cumsum(log_f, axis=-1)
    b_last = bcum[..., -1]
    g = b_last[..., None] - bcum + log_i
    m_loc = jnp.max(g, axis=-1)

    def step(carry, inp):
        Cm, nv, m = carry
        k_n, v_n, g_n, bl_n, ml_n = inp
        m_new = jnp.maximum(bl_n + m, ml_n)
        decay = jnp.exp(bl_n + m - m_new)
        w = jnp.exp(g_n - m_new[..., None])
        C_new = decay[..., None, None] * Cm + jnp.einsum('bhld,bhle->bhde', v_n * w[..., None], k_n)
        n_new = decay[..., None] * nv + jnp.einsum('bhl,bhle->bhe', w, k_n)
        return (C_new, n_new, m_new), (Cm, nv, m)

    init = (jnp.zeros((B, H, Dh, Dh), f32), jnp.zeros((B, H, Dh), f32), jnp.zeros((B, H), f32))
    xs = (kc.transpose(2, 0, 1, 3, 4), vc.transpose(2, 0, 1, 3, 4), g.transpose(2, 0, 1, 3),
          b_last.transpose(2, 0, 1), m_loc.transpose(2, 0, 1))
    _, (C_prev, n_prev, m_prev) = lax.scan(step, init, xs)
    C_prev = C_prev.transpose(1, 2, 0, 3, 4)
    n_prev = n_prev.transpose(1, 2, 0, 3)
    m_prev = m_prev.transpose(1, 2, 0)

    causal = jnp.tril(jnp.ones((L, L), dtype=bool))
    log_D = bcum[..., :, None] - bcum[..., None, :] + log_i[..., None, :]
    log_D = jnp.where(causal, log_D, -jnp.inf)
    m_inter = bcum + m_prev[..., None]
    m_t = jnp.maximum(m_inter, jnp.max(log_D, axis=-1))
    Dmat = jnp.exp(log_D - m_t[..., None])
    s = jnp.einsum('bhnld,bhnjd->bhnlj', qc, kc) * Dmat
    inter_scale = jnp.exp(m_inter - m_t)
    num = jnp.einsum('bhnlj,bhnjd->bhnld', s, vc) + inter_scale[..., None] * jnp.einsum('bhnvk,bhnlk->bhnlv', C_prev, qc)
    den = jnp.sum(s, axis=-1) + inter_scale * jnp.einsum('bhnk,bhnlk->bhnl', n_prev, qc)
    h = num / jnp.maximum(jnp.abs(den), jnp.exp(-m_t))[..., None]
    return h.transpose(0, 2, 3, 1, 4).reshape(B, S, H, Dh).astype(v.dtype)


def fox_s5_mixer(x, w_in, f_bias, a_re, a_im, log_dt, b_re, b_im, c_re, c_im, d_skip, w_glu, b_glu, w_out):
    B, S, _ = x.shape
    z = x @ w_in
    q, k, v, f_logit, u = jnp.split(z, [FOX_WIDTH, 2 * FOX_WIDTH, 3 * FOX_WIDTH, 3 * FOX_WIDTH + FOX_HEADS], axis=-1)
    shp = (B, S, FOX_HEADS, FOX_HEAD_DIM)
    att = forgetting_attention(q.reshape(shp), k.reshape(shp), v.reshape(shp), f_logit + f_bias)
    att = att.reshape(B, S, FOX_WIDTH)
    y = s5_scan(u.reshape(B, S, S5_GROUPS, S5_GROUP), a_re, a_im, log_dt, b_re, b_im, c_re, c_im, d_skip)
    y = jax.nn.gelu(y.reshape(B, S, S5_WIDTH))
    y = y * jax.nn.sigmoid(y @ w_glu + b_glu)
    return jnp.concatenate([att, y], axis=-1) @ w_out


def mlstm_mixer(x, w_in, conv_w, conv_b, i_bias, f_bias, w_out):
    B, S, _ = x.shape
    z = x @ w_in
    qk, v, o, i_pre, f_pre = jnp.split(z, [2 * D_MIX, 3 * D_MIX, 4 * D_MIX, 4 * D_MIX + MLSTM_HEADS], axis=-1)
    qk = jax.nn.silu(causal_dwconv(qk, conv_w, conv_b))
    q, k = jnp.split(qk, 2, axis=-1)
    shp = (B, S, MLSTM_HEADS, MLSTM_HEAD_DIM)
    h = mlstm_chunkwise(q.reshape(shp), k.reshape(shp), v.reshape(shp), i_pre + i_bias, f_pre + f_bias)
    h = h.reshape(B, S, D_MIX) * jax.nn.sigmoid(o)
    return h @ w_out


def hier_moe(x, w_group, b_group, w_expert, b_expert, w_gate, w_up, w_down):
    B, S, D = x.shape
    t = x.reshape(B * S, D)
    g_prob = jax.nn.softmax((t @ w_group + b_group).astype(jnp.float32), axis=-1)
    g_val, g_idx = lax.top_k(g_prob, 1)
    e_all = jnp.einsum('td,gde->tge', t, w_expert) + b_expert
    e_logits = jnp.take_along_axis(e_all, g_idx[:, :, None], axis=1)[:, 0]
    e_prob = jax.nn.softmax(e_logits.astype(jnp.float32), axis=-1)
    e_val, e_idx = lax.top_k(e_prob, EXPERT_TOP_K)
    e_val = e_val / jnp.sum(e_val, axis=-1, keepdims=True)
    weights = g_val * e_val
    expert_id = g_idx * EXPERTS_PER_GROUP + e_idx
    combine = jnp.einsum('tk,tke->te', weights, jax.nn.one_hot(expert_id, N_EXPERTS, dtype=jnp.float32))
    hid = jax.nn.silu(jnp.einsum('td,edf->tef', t, w_gate)) * jnp.einsum('td,edf->tef', t, w_up)
    hid = hid * combine.astype(hid.dtype)[:, :, None]
    out = jnp.einsum('tef,efd->td', hid, w_down)
    return out.reshape(B, S, D)


def setup_inputs(seed: int = 0) -> dict:
    key = jax.random.key(seed)
    ks = iter(jax.random.split(key, 48))
    f32 = jnp.float32

    def nrm(shape, scale):
        return jax.random.normal(next(ks), shape, f32) * scale

    x = nrm((BATCH, SEQ, D_MODEL), 1.0)
    ln_g = 1.0 + nrm((DEPTH, 2, D_MODEL), 0.01)
    ln_b = nrm((DEPTH, 2, D_MODEL), 0.01)
    even_w_in = nrm((N_EVEN, D_MODEL, EVEN_IN), D_MODEL ** -0.5)
    fox_f_bias = jnp.linspace(2.0, 5.0, FOX_HEADS, dtype=f32)[None, :] + nrm((N_EVEN, FOX_HEADS), 0.1)
    s5_a_re = -0.5 + nrm((N_EVEN, S5_GROUPS, S5_STATE), 0.01)
    s5_a_im = jnp.pi * jnp.arange(S5_STATE, dtype=f32)[None, None, :] + nrm((N_EVEN, S5_GROUPS, S5_STATE), 0.01)
    s5_log_dt = jax.random.uniform(next(ks), (N_EVEN, S5_GROUPS), f32, minval=math.log(1e-3), maxval=math.log(1e-1))
    s5_b_re = nrm((N_EVEN, S5_GROUPS, S5_STATE, S5_GROUP), (2 * S5_GROUP) ** -0.5)
    s5_b_im = nrm((N_EVEN, S5_GROUPS, S5_STATE, S5_GROUP), (2 * S5_GROUP) ** -0.5)
    s5_c_re = nrm((N_EVEN, S5_GROUPS, S5_GROUP, S5_STATE), S5_STATE ** -0.5)
    s5_c_im = nrm((N_EVEN, S5_GROUPS, S5_GROUP, S5_STATE), S5_STATE ** -0.5)
    s5_d = nrm((N_EVEN, S5_GROUPS, S5_GROUP), 1.0)
    s5_w_glu = nrm((N_EVEN, S5_WIDTH, S5_WIDTH), S5_WIDTH ** -0.5)
    s5_b_glu = nrm((N_EVEN, S5_WIDTH), 0.01)
    even_w_out = nrm((N_EVEN, D_MIX, D_MODEL), D_MIX ** -0.5 * DN_BETA)
    odd_w_in = nrm((N_ODD, D_MODEL, ODD_IN), D_MODEL ** -0.5)
    mlstm_conv_w = nrm((N_ODD, CONV_WIDTH, 2 * D_MIX), CONV_WIDTH ** -0.5)
    mlstm_conv_b = nrm((N_ODD, 2 * D_MIX), 0.01)
    mlstm_i_bias = nrm((N_ODD, MLSTM_HEADS), 0.1)
    mlstm_f_bias = jnp.linspace(3.0, 6.0, MLSTM_HEADS, dtype=f32)[None, :] + nrm((N_ODD, MLSTM_HEADS), 0.01)
    odd_w_out = nrm((N_ODD, D_MIX, D_MODEL), D_MIX ** -0.5 * DN_BETA)
    moe_w_group = nrm((DEPTH, D_MODEL, N_GROUPS), D_MODEL ** -0.5)
    moe_b_group = nrm((DEPTH, N_GROUPS), 0.01)
    moe_w_expert = nrm((DEPTH, N_GROUPS, D_MODEL, EXPERTS_PER_GROUP), D_MODEL ** -0.5)
    moe_b_expert = nrm((DEPTH, N_GROUPS, EXPERTS_PER_GROUP), 0.01)
    moe_w_gate = nrm((DEPTH, N_EXPERTS, D_MODEL, D_EXPERT), D_MODEL ** -0.5)
    moe_w_up = nrm((DEPTH, N_EXPERTS, D_MODEL, D_EXPERT), D_MODEL ** -0.5)
    moe_w_down = nrm((DEPTH, N_EXPERTS, D_EXPERT, D_MODEL), D_EXPERT ** -0.5 * DN_BETA)
    return {'x': x, 'ln_g': ln_g, 'ln_b': ln_b,
            'even_w_in': even_w_in, 'fox_f_bias': fox_f_bias,
            's5_a_re': s5_a_re, 's5_a_im': s5_a_im, 's5_log_dt': s5_log_dt,
            's5_b_re': s5_b_re, 's5_b_im': s5_b_im, 's5_c_re': s5_c_re, 's5_c_im': s5_c_im,
            's5_d': s5_d, 's5_w_glu': s5_w_glu, 's5_b_glu': s5_b_glu, 'even_w_out': even_w_out,
            'odd_w_in': odd_w_in, 'mlstm_conv_w': mlstm_conv_w, 'mlstm_conv_b': mlstm_conv_b,
            'mlstm_i_bias': mlstm_i_bias, 'mlstm_f_bias': mlstm_f_bias, 'odd_w_out': odd_w_out,
            'moe_w_group': moe_w_group, 'moe_b_group': moe_b_group,
            'moe_w_expert': moe_w_expert, 'moe_b_expert': moe_b_expert,
            'moe_w_gate': moe_w_gate, 'moe_w_up': moe_w_up, 'moe_w_down': moe_w_down}


def reference(x, ln_g, ln_b, even_w_in, fox_f_bias, s5_a_re, s5_a_im, s5_log_dt, s5_b_re, s5_b_im,
              s5_c_re, s5_c_im, s5_d, s5_w_glu, s5_b_glu, even_w_out, odd_w_in, mlstm_conv_w,
              mlstm_conv_b, mlstm_i_bias, mlstm_f_bias, odd_w_out, moe_w_group, moe_b_group,
              moe_w_expert, moe_b_expert, moe_w_gate, moe_w_up, moe_w_down):
    h = x
    for layer in range(DEPTH):
        j = layer // 2
        if layer % 2 == 0:
            mix = fox_s5_mixer(h, even_w_in[j], fox_f_bias[j], s5_a_re[j], s5_a_im[j], s5_log_dt[j],
                               s5_b_re[j], s5_b_im[j], s5_c_re[j], s5_c_im[j], s5_d[j],
                               s5_w_glu[j], s5_b_glu[j], even_w_out[j])
        else:
            mix = mlstm_mixer(h, odd_w_in[j], mlstm_conv_w[j], mlstm_conv_b[j], mlstm_i_bias[j],
                              mlstm_f_bias[j], odd_w_out[j])
        h = layer_norm(DN_ALPHA * h + mix, ln_g[layer, 0], ln_b[layer, 0])
        ffn = hier_moe(h, moe_w_group[layer], moe_b_group[layer], moe_w_expert[layer], moe_b_expert[layer],
                       moe_w_gate[layer], moe_w_up[layer], moe_w_down[layer])
        h = layer_norm(DN_ALPHA * h + ffn, ln_g[layer, 1], ln_b[layer, 1])
    return h
```

```python
import numpy as np
import concourse.bass as bass
import concourse.mybir as mybir
from concourse.bass_utils import run_bass_kernel_spmd

F32 = mybir.dt.float32
BF16 = mybir.dt.bfloat16
ALU = mybir.AluOpType
AF = mybir.ActivationFunctionType
AX = mybir.AxisListType

ENGS = ["sync", "scalar", "vector", "gpsimd", "tensor"]
NDSEM = 24


class Op:
    __slots__ = ("eng", "fn", "idx", "waits", "dwaits", "signal", "sval", "is_dma", "dnum", "guard")

    def __init__(self, eng, fn, is_dma):
        self.eng = eng
        self.fn = fn
        self.waits = []
        self.dwaits = []
        self.signal = False
        self.sval = 0
        self.is_dma = is_dma
        self.dnum = -1
        self.guard = None


class Prog:
    def __init__(self, nc):
        self.nc = nc
        self.ops = {e: [] for e in ENGS}
        self.last_writer = {}
        self.readers = {}
        self.seen = {e: {f: -1 for f in ENGS} for e in ENGS}
        self.seen_dma = {e: set() for e in ENGS}
        self.ndma = 0
        self.out_dmas = []

    def add(self, eng, fn, reads=(), writes=(), dma=False, out=False):
        op = Op(eng, fn, dma)
        op.idx = len(self.ops[eng])
        deps = []
        for k in reads:
            w = self.last_writer.get(k)
            if w is not None:
                deps.append((w, "raw"))
        for k in writes:
            w = self.last_writer.get(k)
            if w is not None:
                deps.append((w, "waw"))
            for r in self.readers.get(k, ()):
                deps.append((r, "war"))
        for d, kind in deps:
            if d is op:
                continue
            if d.is_dma:
                if d.dnum in self.seen_dma[eng]:
                    continue
                self.seen_dma[eng].add(d.dnum)
                op.dwaits.append(d)
            else:
                if d.eng == eng and (eng == "tensor" or kind == "war"):
                    continue
                if self.seen[eng][d.eng] >= d.idx:
                    continue
                self.seen[eng][d.eng] = d.idx
                d.signal = True
                op.waits.append(d)
        if dma:
            op.dnum = self.ndma
            self.ndma += 1
            if op.dnum >= NDSEM:
                op.guard = op.dnum - NDSEM
                self.seen_dma[eng].add(op.guard)
            if out:
                self.out_dmas.append(op)
        for k in reads:
            self.readers.setdefault(k, []).append(op)
        for k in writes:
            self.last_writer[k] = op
            self.readers[k] = []
        self.ops[eng].append(op)
        return op

    def emit(self):
        nc = self.nc
        fin = Op("sync", None, False)
        fin.idx = len(self.ops["sync"])
        fin.dwaits = [d for d in self.out_dmas]
        self.ops["sync"].append(fin)
        for e in ENGS:
            c = 0
            for op in self.ops[e]:
                if op.signal:
                    c += 1
                    op.sval = c
        import contextlib
        with contextlib.ExitStack() as st:
            esem = {e: st.enter_context(nc.semaphore("s_" + e)) for e in ENGS}
            dsem = [st.enter_context(nc.semaphore("d_%d" % i)) for i in range(NDSEM)]
            block = st.enter_context(nc.Block())

            def mk(e):
                def body(eng):
                    for op in self.ops[e]:
                        for d in op.waits:
                            eng.wait_ge(esem[d.eng], d.sval)
                        for d in op.dwaits:
                            eng.wait_ge(dsem[d.dnum % NDSEM], 16 * (d.dnum // NDSEM + 1))
                        if op.guard is not None:
                            g = op.guard
                            eng.wait_ge(dsem[g % NDSEM], 16 * (g // NDSEM + 1))
                        if op.fn is None:
                            continue
                        ins = op.fn(eng)
                        if op.is_dma:
                            ins.then_inc(dsem[op.dnum % NDSEM], 16)
                        elif op.signal:
                            ins.then_inc(esem[e], 1)
                return body

            block.sync(mk("sync"))
            block.scalar(mk("scalar"))
            block.vector(mk("vector"))
            block.gpsimd(mk("gpsimd"))
            block.tensor(mk("tensor"))

    def nops(self):
        return {e: len(v) for e, v in self.ops.items()}

import contextlib

DN_ALPHA = 8 ** 0.25
LN_EPS = 1e-5
TOK = 2048
CH = 512
NCH = TOK // CH


class KB:
    def __init__(self):
        self.nc = bass.Bass("TRN2", target_bir_lowering=False)
        self.st = contextlib.ExitStack()
        self.P = Prog(self.nc)

    def din(self, name, shape, dt=F32):
        return self.nc.dram_tensor(name, list(shape), dt, kind="ExternalInput").ap()

    def dout(self, name, shape, dt=F32):
        return self.nc.dram_tensor(name, list(shape), dt, kind="ExternalOutput").ap()

    def sb(self, name, shape, dt=F32):
        return self.st.enter_context(self.nc.sbuf_tensor(name, list(shape), dt))

    def ps(self, name, shape, dt=F32):
        return self.st.enter_context(self.nc.psum_tensor(name, list(shape), dt))

    def finish(self):
        self.P.emit()
        self.st.close()
        return self.nc


def make_ident(kb, name="ident", dt=F32):
    P = kb.P
    ident = kb.sb(name, [128, 128], dt)
    P.add("gpsimd", lambda e: e.memset(ident[:], 0.0), writes=[name])
    P.add("gpsimd", lambda e: e.affine_select(out=ident[:], in_=ident[:], pattern=[[-1, 128]], compare_op=ALU.not_equal,
                                              fill=1.0, base=0, channel_multiplier=1), reads=[name], writes=[name])
    return ident


def ln_feature_major(kb, pfx, r, rk, g_col, b_col, gk, onesf, pm, pq, tmp, outs, n=CH):
    P = kb.P
    sq, mean, rstd, t = tmp["sq"], tmp["mean"], tmp["rstd"], tmp["t"]
    for m in range(8):
        P.add("tensor", lambda e, m=m: e.matmul(pm[:, :n], lhsT=onesf[:], rhs=r[:, m, :n], start=(m == 0), stop=(m == 7)),
              reads=[rk(m), "onesf"], writes=["pm"])
    for m in range(8):
        P.add("scalar", lambda e, m=m: e.activation(out=sq[:, m % 2, :n], in_=r[:, m, :n], func=AF.Square),
              reads=[rk(m)], writes=[(pfx + "sq", m % 2)])
        P.add("tensor", lambda e, m=m: e.matmul(pq[:, :n], lhsT=onesf[:], rhs=sq[:, m % 2, :n], start=(m == 0), stop=(m == 7)),
              reads=[(pfx + "sq", m % 2), "onesf"], writes=["pq"])
    P.add("scalar", lambda e: e.activation(out=mean[:, :n], in_=pm[:, :n], func=AF.Identity), reads=["pm"], writes=[pfx + "mean"])
    P.add("scalar", lambda e: e.activation(out=rstd[:, :n], in_=pm[:, :n], func=AF.Square), reads=["pm"], writes=[pfx + "rstd"])
    P.add("vector", lambda e: e.tensor_tensor(out=rstd[:, :n], in0=pq[:, :n], in1=rstd[:, :n], op=ALU.subtract),
          reads=["pq", pfx + "rstd"], writes=[pfx + "rstd"])
    P.add("scalar", lambda e: e.activation(out=rstd[:, :n], in_=rstd[:, :n], func=AF.Sqrt, bias=tmp["eps"][:], scale=1.0),
          reads=[pfx + "rstd", "eps"], writes=[pfx + "rstd"])
    P.add("vector", lambda e: e.reciprocal(out=rstd[:, :n], in_=rstd[:, :n]), reads=[pfx + "rstd"], writes=[pfx + "rstd"])
    for m in range(8):
        P.add("gpsimd", lambda e, m=m: e.tensor_tensor(out=t[:, m % 2, :n], in0=r[:, m, :n], in1=mean[:, :n], op=ALU.subtract),
              reads=[rk(m), pfx + "mean"], writes=[(pfx + "t", m % 2)])
        P.add("vector", lambda e, m=m: e.tensor_tensor(out=t[:, m % 2, :n], in0=t[:, m % 2, :n], in1=rstd[:, :n], op=ALU.mult),
              reads=[(pfx + "t", m % 2), pfx + "rstd"], writes=[(pfx + "t", m % 2)])
        for (ot, okf, oeng) in outs:
            if oeng == "scalar":
                P.add("scalar", lambda e, m=m, ot=ot: e.activation(out=ot[:, m, :n], in_=t[:, m % 2, :n], func=AF.Identity,
                                                                    bias=b_col(m), scale=g_col(m)),
                      reads=[(pfx + "t", m % 2), gk], writes=[okf(m)])
            else:
                P.add(oeng, lambda e, m=m, ot=ot: e.tensor_scalar(out=ot[:, m, :n], in0=t[:, m % 2, :n], scalar1=g_col(m), scalar2=b_col(m),
                                                                   op0=ALU.mult, op1=ALU.add),
                      reads=[(pfx + "t", m % 2), gk], writes=[okf(m)])


def build_post(even, tok=TOK):
    kb = KB()
    nc, P = kb.nc, kb.P
    nch = tok // CH
    hT = kb.din("hT", [1024, tok])
    if even:
        attT = kb.din("attT", [512, tok])
        ysT = kb.din("ysT", [512, tok])
        w_glu = kb.din("w_glu", [512, 512])
        b_glu = kb.din("b_glu", [512])
    else:
        mixTd = kb.din("mixT", [1024, tok])
    w_out = kb.din("w_out", [1024, 1024])
    lnp = kb.din("lnp", [4, 1024])
    wr = kb.din("wr", [1024, 20])
    br = kb.din("br", [20])
    wg = kb.din("wg", [16, 1024, 256])
    wu = kb.din("wu", [16, 1024, 256])
    wd = kb.din("wd", [16, 256, 1024])
    houtT = kb.dout("houtT", [1024, tok])

    wout_sb = kb.sb("wout_sb", [128, 8, 1024], BF16)
    lnc = kb.sb("lnc", [128, 4, 8])
    wr_sb = kb.sb("wr_sb", [128, 8, 20])
    br_sb = kb.sb("br_sb", [128, 20])
    onesf = kb.sb("onesf", [128, 128])
    eps = kb.sb("eps", [128, 1])
    sel = kb.sb("sel", [16, 16, 128])
    ident = make_ident(kb)
    mixT = kb.sb("mixT_sb", [128, 8, CH], BF16)
    hTs = kb.sb("hTs", [128, 8, CH])
    r = kb.sb("r", [128, 8, CH])
    x1 = kb.sb("x1", [128, 8, CH])
    x1b = kb.sb("x1b", [128, 8, CH], BF16)
    hid = kb.sb("hid", [128, 32, CH], BF16)
    wgs = kb.sb("wgs", [128, 2, 8, 256], BF16)
    wus = kb.sb("wus", [128, 2, 8, 256], BF16)
    wds = kb.sb("wds", [128, 2, 32, 128], BF16)
    tmp = dict(sq=kb.sb("sq", [128, 2, CH]), mean=kb.sb("mean", [128, CH]), rstd=kb.sb("rstd", [128, CH]),
               t=kb.sb("lt", [128, 2, CH]), eps=eps)
    combT = kb.sb("combT", [16, CH])
    cbs = kb.sb("cbs", [128, CH])
    sgs = kb.sb("sgs", [128, 2, CH])
    tts = kb.sb("tts", [128, 2, CH])
    lg = kb.sb("lg", [128, 20])
    rt = kb.sb("rt", [128, 64])
    comb = kb.sb("comb", [128, 16])
    if even:
        ys = kb.sb("ys", [128, 4, CH])
        yt = kb.sb("yt", [128, 2, CH])
        ygb = kb.sb("ygb", [128, 4, CH], BF16)
        wglu_sb = kb.sb("wglu_sb", [128, 4, 512], BF16)
        bglu_sb = kb.sb("bglu_sb", [128, 4])
    po = [kb.ps("po0", [128, CH]), kb.ps("po1", [128, CH])]
    pm = kb.ps("pm", [128, CH])
    pq = kb.ps("pq", [128, CH])
    pg = [kb.ps("pg0", [128, CH]), kb.ps("pg1", [128, CH])]
    pu = [kb.ps("pu0", [128, CH]), kb.ps("pu1", [128, CH])]

    P.add("gpsimd", lambda e: e.dma_start(out=wout_sb[:], in_=w_out.rearrange("(k p) n -> p k n", p=128)), writes=["wout"], dma=True)
    lnrow = kb.sb("lnrow", [32, 128])
    P.add("sync", lambda e: e.dma_start(out=lnrow[:], in_=lnp.rearrange("i (m p) -> (i m) p", p=128)), writes=["lnrow"], dma=True)
    P.add("tensor", lambda e: e.transpose(out=pq[:, 0:32], in_=lnrow[:], identity=ident[0:32, 0:32]), reads=["lnrow", "ident"], writes=["pq"])
    P.add("vector", lambda e: e.tensor_copy(out=lnc[:].rearrange("p i m -> p (i m)"), in_=pq[:, 0:32]), reads=["pq"], writes=["lnc"])
    P.add("sync", lambda e: e.dma_start(out=wr_sb[:], in_=wr.rearrange("(k p) n -> p k n", p=128)), writes=["wr"], dma=True)
    P.add("sync", lambda e: e.dma_start(out=br_sb[:], in_=br.rearrange("(o n) -> o n", o=1).to_broadcast([128, 20])), writes=["br"], dma=True)
    P.add("vector", lambda e: e.memset(onesf[:], 1.0 / 1024.0), writes=["onesf"])
    P.add("vector", lambda e: e.memset(eps[:], LN_EPS), writes=["eps"])
    P.add("gpsimd", lambda e: e.memset(sel[:], 1.0), writes=["sel"])
    P.add("gpsimd", lambda e: e.affine_select(out=sel[:], in_=sel[:], pattern=[[-1, 16], [0, 128]], compare_op=ALU.is_equal,
                                              fill=0.0, base=0, channel_multiplier=1), reads=["sel"], writes=["sel"])
    if even:
        P.add("gpsimd", lambda e: e.dma_start(out=wglu_sb[:], in_=w_glu.rearrange("(k p) n -> p k n", p=128)), writes=["wglu"], dma=True)
        bgrow = kb.sb("bgrow", [4, 128])
        P.add("sync", lambda e: e.dma_start(out=bgrow[:], in_=b_glu.rearrange("(n p) -> n p", p=128)), writes=["bgrow"], dma=True)
        P.add("tensor", lambda e: e.transpose(out=pq[:, 0:4], in_=bgrow[:], identity=ident[0:4, 0:4]), reads=["bgrow", "ident"], writes=["pq"])
        P.add("vector", lambda e: e.tensor_copy(out=bglu_sb[:], in_=pq[:, 0:4]), reads=["pq"], writes=["bglu"])

    gcol = lambda i: (lambda m: lnc[:, i, m:m + 1])

    for c in range(nch):
        c0 = c * CH
        csl = slice(c0, c0 + CH)
        P.add("sync", lambda e, csl=csl: e.dma_start(out=hTs[:], in_=hT.rearrange("(m p) t -> p m t", p=128)[:, :, csl]),
              writes=[("hTs", m) for m in range(8)], dma=True)
        if even:
            P.add("gpsimd", lambda e, csl=csl: e.dma_start(out=mixT[:, 0:4, :], in_=attT.rearrange("(m p) t -> p m t", p=128)[:, :, csl]),
                  writes=[("mixT", m) for m in range(4)], dma=True)
            P.add("sync", lambda e, csl=csl: e.dma_start(out=ys[:], in_=ysT.rearrange("(m p) t -> p m t", p=128)[:, :, csl]),
                  writes=[("ys", m) for m in range(4)], dma=True)
            for m in range(4):
                b = m % 2
                P.add("scalar", lambda e, m=m, b=b: e.activation(out=yt[:, b, :], in_=ys[:, m, :], func=AF.Square),
                      reads=[("ys", m)], writes=[("yt", b)])
                P.add("vector", lambda e, b=b: e.tensor_scalar(out=yt[:, b, :], in0=yt[:, b, :], scalar1=0.044715, scalar2=1.0,
                                                               op0=ALU.mult, op1=ALU.add), reads=[("yt", b)], writes=[("yt", b)])
                P.add("vector", lambda e, m=m, b=b: e.tensor_tensor(out=yt[:, b, :], in0=yt[:, b, :], in1=ys[:, m, :], op=ALU.mult),
                      reads=[("yt", b), ("ys", m)], writes=[("yt", b)])
                P.add("scalar", lambda e, b=b: e.activation(out=yt[:, b, :], in_=yt[:, b, :], func=AF.Sigmoid, scale=1.5957691216),
                      reads=[("yt", b)], writes=[("yt", b)])
                P.add("vector", lambda e, m=m, b=b: e.tensor_tensor(out=ygb[:, m, :], in0=yt[:, b, :], in1=ys[:, m, :], op=ALU.mult),
                      reads=[("yt", b), ("ys", m)], writes=[("ygb", m)])
            for n in range(4):
                pb = po[n % 2]
                for k in range(4):
                    P.add("tensor", lambda e, n=n, k=k, pb=pb: e.matmul(pb[:], lhsT=wglu_sb[:, k, n * 128:(n + 1) * 128], rhs=ygb[:, k, :],
                                                                         start=(k == 0), stop=(k == 3)),
                          reads=[("ygb", k), "wglu"], writes=["po%d" % (n % 2)])
                b = n % 2
                P.add("scalar", lambda e, n=n, pb=pb, b=b: e.activation(out=yt[:, b, :], in_=pb[:], func=AF.Sigmoid, bias=bglu_sb[:, n:n + 1], scale=1.0),
                      reads=["po%d" % (n % 2), "bglu"], writes=[("yt", b)])
                P.add("vector", lambda e, n=n, b=b: e.tensor_tensor(out=mixT[:, 4 + n, :], in0=yt[:, b, :], in1=ygb[:, n, :], op=ALU.mult),
                      reads=[("yt", b), ("ygb", n)], writes=[("mixT", 4 + n)])
        else:
            P.add("gpsimd", lambda e, csl=csl: e.dma_start(out=mixT[:], in_=mixTd.rearrange("(m p) t -> p m t", p=128)[:, :, csl]),
                  writes=[("mixT", m) for m in range(8)], dma=True)
        for m in range(8):
            pb = po[m % 2]
            for k in range(8):
                P.add("tensor", lambda e, m=m, k=k, pb=pb: e.matmul(pb[:], lhsT=wout_sb[:, k, m * 128:(m + 1) * 128], rhs=mixT[:, k, :],
                                                                     start=(k == 0), stop=(k == 7)),
                      reads=[("mixT", k), "wout"], writes=["po%d" % (m % 2)])
            P.add("vector", lambda e, m=m, pb=pb: e.scalar_tensor_tensor(out=r[:, m, :], in0=hTs[:, m, :], scalar=DN_ALPHA, in1=pb[:],
                                                                         op0=ALU.mult, op1=ALU.add),
                  reads=[("hTs", m), "po%d" % (m % 2)], writes=[("r", m)])
        ln_feature_major(kb, "l1", r, lambda m: ("r", m), gcol(0), gcol(1), "lnc", onesf, pm, pq, tmp,
                         [(x1, lambda m: ("x1", m), "scalar"), (x1b, lambda m: ("x1b", m), "gpsimd")])
        for tt in range(4):
            tsl = slice(tt * 128, (tt + 1) * 128)
            for m in range(8):
                P.add("tensor", lambda e, m=m, tsl=tsl: e.matmul(pm[:, 0:20], lhsT=x1[:, m, tsl], rhs=wr_sb[:, m, :], start=(m == 0), stop=(m == 7)),
                      reads=[("x1", m), "wr"], writes=["pm"])
            P.add("vector", lambda e: e.tensor_tensor(out=lg[:], in0=pm[:, 0:20], in1=br_sb[:], op=ALU.add), reads=["pm", "br"], writes=["lg"])
            P.add("vector", lambda e: e.tensor_reduce(out=rt[:, 0:1], in_=lg[:, 0:4], axis=AX.X, op=ALU.max), reads=["lg"], writes=["rt"])
            P.add("vector", lambda e: e.tensor_scalar(out=rt[:, 20:24], in0=lg[:, 0:4], scalar1=rt[:, 0:1], scalar2=None, op0=ALU.is_equal),
                  reads=["lg", "rt"], writes=["rt"])
            P.add("vector", lambda e: e.tensor_scalar(out=rt[:, 16:20], in0=lg[:, 0:4], scalar1=rt[:, 0:1], scalar2=None, op0=ALU.subtract),
                  reads=["lg", "rt"], writes=["rt"])
            P.add("scalar", lambda e: e.activation(out=rt[:, 16:20], in_=rt[:, 16:20], func=AF.Exp, accum_out=rt[:, 1:2]), reads=["rt"], writes=["rt"])
            P.add("vector", lambda e: e.reciprocal(out=rt[:, 2:3], in_=rt[:, 1:2]), reads=["rt"], writes=["rt"])
            P.add("vector", lambda e: e.tensor_scalar(out=rt[:, 20:24], in0=rt[:, 20:24], scalar1=-1.0, scalar2=1e30, op0=ALU.add, op1=ALU.mult),
                  reads=["rt"], writes=["rt"])
            P.add("vector", lambda e: e.tensor_tensor(out=rt[:, 24:40].rearrange("p (g e) -> p g e", g=4),
                                                      in0=lg[:, 4:20].rearrange("p (g e) -> p g e", g=4),
                                                      in1=rt[:, 20:24].rearrange("p (g o) -> p g o", o=1).to_broadcast([128, 4, 4]), op=ALU.add),
                  reads=["rt", "lg"], writes=["rt"])
            P.add("vector", lambda e: e.max(out=rt[:, 8:16], in_=rt[:, 24:40]), reads=["rt"], writes=["rt"])
            P.add("vector", lambda e: e.tensor_scalar(out=rt[:, 40:56], in0=rt[:, 24:40], scalar1=rt[:, 8:9], scalar2=None, op0=ALU.is_equal),
                  reads=["rt"], writes=["rt"])
            P.add("vector", lambda e: e.tensor_scalar(out=rt[:, 24:40], in0=rt[:, 24:40], scalar1=rt[:, 9:10], scalar2=None, op0=ALU.is_equal),
                  reads=["rt"], writes=["rt"])
            P.add("vector", lambda e: e.tensor_tensor(out=rt[:, 56:57], in0=rt[:, 9:10], in1=rt[:, 8:9], op=ALU.subtract), reads=["rt"], writes=["rt"])
            P.add("scalar", lambda e: e.activation(out=rt[:, 56:57], in_=rt[:, 56:57], func=AF.Exp), reads=["rt"], writes=["rt"])
            P.add("vector", lambda e: e.tensor_scalar(out=rt[:, 57:58], in0=rt[:, 56:57], scalar1=1.0, scalar2=None, op0=ALU.add), reads=["rt"], writes=["rt"])
            P.add("vector", lambda e: e.reciprocal(out=rt[:, 57:58], in_=rt[:, 57:58]), reads=["rt"], writes=["rt"])
            P.add("vector", lambda e: e.tensor_tensor(out=rt[:, 57:58], in0=rt[:, 57:58], in1=rt[:, 2:3], op=ALU.mult), reads=["rt"], writes=["rt"])
            P.add("vector", lambda e: e.tensor_tensor(out=rt[:, 58:59], in0=rt[:, 57:58], in1=rt[:, 56:57], op=ALU.mult), reads=["rt"], writes=["rt"])
            P.add("vector", lambda e: e.tensor_scalar(out=comb[:], in0=rt[:, 40:56], scalar1=rt[:, 57:58], scalar2=None, op0=ALU.mult),
                  reads=["rt"], writes=["comb"])
            P.add("vector", lambda e: e.scalar_tensor_tensor(out=comb[:], in0=rt[:, 24:40], scalar=rt[:, 58:59], in1=comb[:], op0=ALU.mult, op1=ALU.add),
                  reads=["rt", "comb"], writes=["comb"])
            P.add("tensor", lambda e: e.transpose(out=pq[0:16, 0:128], in_=comb[:], identity=ident[:]), reads=["comb", "ident"], writes=["pq"])
            P.add("scalar", lambda e, tsl=tsl: e.activation(out=combT[:, tsl], in_=pq[0:16, 0:128], func=AF.Identity), reads=["pq"], writes=[("combT", tt)])
        for ex in range(16):
            wb = ex % 2
            P.add("gpsimd", lambda e, ex=ex, wb=wb: e.dma_start(out=wgs[:, wb], in_=wg[ex].rearrange("(k p) n -> p k n", p=128)),
                  writes=[("wgs", wb)], dma=True)
            P.add("gpsimd", lambda e, ex=ex, wb=wb: e.dma_start(out=wus[:, wb], in_=wu[ex].rearrange("(k p) n -> p k n", p=128)),
                  writes=[("wus", wb)], dma=True)
            P.add("tensor", lambda e, ex=ex: e.matmul(pm[:], lhsT=sel[:, ex, :], rhs=combT[:], start=True, stop=True),
                  reads=[("combT", i) for i in range(4)] + ["sel"], writes=["pm"])
            P.add("scalar", lambda e: e.activation(out=cbs[:], in_=pm[:], func=AF.Identity), reads=["pm"], writes=["cbs"])
            for fh in range(2):
                pgb, pub = pg[fh], pu[fh]
                for k in range(8):
                    P.add("tensor", lambda e, k=k, fh=fh, wb=wb, pgb=pgb: e.matmul(pgb[:], lhsT=wgs[:, wb, k, fh * 128:(fh + 1) * 128], rhs=x1b[:, k, :],
                                                                                    start=(k == 0), stop=(k == 7)),
                          reads=[("x1b", k), ("wgs", wb)], writes=["pg%d" % fh])
                for k in range(8):
                    P.add("tensor", lambda e, k=k, fh=fh, wb=wb, pub=pub: e.matmul(pub[:], lhsT=wus[:, wb, k, fh * 128:(fh + 1) * 128], rhs=x1b[:, k, :],
                                                                                    start=(k == 0), stop=(k == 7)),
                          reads=[("x1b", k), ("wus", wb)], writes=["pu%d" % fh])
                P.add("scalar", lambda e, fh=fh, pgb=pgb: e.activation(out=sgs[:, fh, :], in_=pgb[:], func=AF.Silu), reads=["pg%d" % fh], writes=[("sgs", fh)])
                P.add("vector", lambda e, fh=fh, pub=pub: e.tensor_tensor(out=tts[:, fh, :], in0=sgs[:, fh, :], in1=pub[:], op=ALU.mult),
                      reads=[("sgs", fh), "pu%d" % fh], writes=[("tts", fh)])
                P.add("gpsimd", lambda e, fh=fh, ex=ex: e.tensor_tensor(out=hid[:, ex * 2 + fh, :], in0=tts[:, fh, :], in1=cbs[:], op=ALU.mult),
                      reads=[("tts", fh), "cbs"], writes=[("hid", ex * 2 + fh)])
        for m in range(8):
            wb = m % 2
            P.add("gpsimd", lambda e, m=m, wb=wb: e.dma_start(out=wds[:, wb], in_=wd.rearrange("e (fh p) d -> p (e fh) d", p=128)[:, :, m * 128:(m + 1) * 128]),
                  writes=[("wds", wb)], dma=True)
            pb = po[m % 2]
            for f in range(32):
                P.add("tensor", lambda e, f=f, wb=wb, pb=pb: e.matmul(pb[:], lhsT=wds[:, wb, f, :], rhs=hid[:, f, :], start=(f == 0), stop=(f == 31)),
                      reads=[("hid", f), ("wds", wb)], writes=["po%d" % (m % 2)])
            P.add("vector", lambda e, m=m, pb=pb: e.scalar_tensor_tensor(out=r[:, m, :], in0=x1[:, m, :], scalar=DN_ALPHA, in1=pb[:],
                                                                         op0=ALU.mult, op1=ALU.add),
                  reads=[("x1", m), "po%d" % (m % 2)], writes=[("r", m)])
        ln_feature_major(kb, "l2", r, lambda m: ("r", m), gcol(2), gcol(3), "lnc", onesf, pm, pq, tmp,
                         [(hTs, lambda m: ("hTs", m), "scalar")])
        P.add("sync", lambda e, csl=csl: e.dma_start(out=houtT.rearrange("(m p) t -> p m t", p=128)[:, :, csl], in_=hTs[:]),
              reads=[("hTs", m) for m in range(8)], dma=True, out=True)
    print("post ops", P.nops())
    return kb.finish()

import math

PI = math.pi

I32 = mybir.dt.int32


def sincos(P, x, sin_o, cos_o, t1, t2, ti, rd, wr_s, wr_c, K):
    P.add("vector", lambda e: e.tensor_scalar(out=t1, in0=x, scalar1=1.0 / (2 * PI), scalar2=None, op0=ALU.mult), reads=list(rd) + [K], writes=[K])
    P.add("vector", lambda e: e.tensor_copy(out=ti, in_=t1), reads=[K], writes=[K])
    P.add("vector", lambda e: e.tensor_copy(out=t1, in_=ti), reads=[K], writes=[K])
    P.add("vector", lambda e: e.scalar_tensor_tensor(out=t1, in0=t1, scalar=-2 * PI, in1=x, op0=ALU.mult, op1=ALU.add), reads=list(rd) + [K], writes=[K])
    P.add("scalar", lambda e: e.activation(out=t2, in_=t1, func=AF.Sin, scale=0.25), reads=[K], writes=[K])
    P.add("scalar", lambda e: e.activation(out=t1, in_=t1, func=AF.Sin, scale=0.5), reads=[K], writes=[K])
    P.add("vector", lambda e: e.tensor_tensor(out=t2, in0=t2, in1=t2, op=ALU.mult), reads=[K], writes=[K])
    P.add("vector", lambda e: e.tensor_scalar(out=t2, in0=t2, scalar1=-2.0, scalar2=1.0, op0=ALU.mult, op1=ALU.add), reads=[K], writes=[K])
    P.add("vector", lambda e: e.scalar_tensor_tensor(out=sin_o, in0=t1, scalar=2.0, in1=t2, op0=ALU.mult, op1=ALU.mult), reads=[K], writes=list(wr_s) + [K])
    P.add("vector", lambda e: e.tensor_tensor(out=t1, in0=t1, in1=t1, op=ALU.mult), reads=[K], writes=[K])
    P.add("vector", lambda e: e.tensor_scalar(out=cos_o, in0=t1, scalar1=-2.0, scalar2=1.0, op0=ALU.mult, op1=ALU.add), reads=[K], writes=list(wr_c) + [K])


def build_even(S=4096, mode='fox'):
    FOX = (mode == 'fox'); S5 = not FOX
    kb = KB()
    nc, P = kb.nc, kb.P
    NCk = S // 512
    NT = S // 128
    hT = kb.din("hT", [1024, S])
    if FOX:
        wq = kb.din("wq", [1024, 256]); wk = kb.din("wk", [1024, 256]); wv = kb.din("wv", [1024, 256])
        wf = kb.din("wf", [1024, 4])
        fbias = kb.din("fbias", [4, 1])
    else:
        wu = kb.din("wu", [1024, 256])
        are = kb.din("are", [128, 8]); aim = kb.din("aim", [128, 8]); ldt = kb.din("ldt", [128, 8])
        bre = kb.din("bre", [128, 8, 16]); bim = kb.din("bim", [128, 8, 16])
        cre = kb.din("cre", [128, 8, 16]); cim = kb.din("cim", [128, 8, 16])
        dsk = kb.din("dsk", [128, 2])
        jrow_d = kb.din("jrow", [128, 512])
    att = kb.dout("att", [S, 256]) if FOX else None
    ysT = kb.dout("ysT", [256, S]) if S5 else None

    ident = make_ident(kb)
    hTb = kb.sb("hTb", [128, 2, 8, 512], BF16)
    if FOX:
        wq_sb = kb.sb("wq_sb", [128, 8, 256], BF16); wk_sb = kb.sb("wk_sb", [128, 8, 256], BF16)
        wv_sb = kb.sb("wv_sb", [128, 8, 256], BF16)
        wf_sb = kb.sb("wf_sb", [128, 8, 4], BF16)
        fb_sb = kb.sb("fb_sb", [4, 1])
        wl = ((wq_sb, wq, "wq"), (wk_sb, wk, "wk"), (wv_sb, wv, "wv"), (wf_sb, wf, "wf"))
    else:
        wu_sb = kb.sb("wu_sb", [128, 8, 256], BF16)
        wl = ((wu_sb, wu, "wu"),)
    if FOX:
        QA = kb.sb("QA", [65, 4, S], BF16)
        KA = kb.sb("KA", [65, 4, S], BF16)
        V = kb.sb("V", [128, NT, 4, 72], BF16)
        fl = kb.sb("fl", [4, S])
        cc = fl
        ones4 = kb.sb("ones4", [4, 512])
    else:
        uTb = kb.sb("uTb", [128, 2, S], BF16)
    selc = kb.sb("selc", [4, 4, 65])
    negc = kb.sb("negc", [128, NT, 4])
    PT = kb.sb("PT", [128, 2, 512], BF16)
    ost = kb.sb("ost", [128, 2, 256])
    rec = kb.sb("rec", [128, 4])
    otmp = kb.sb("otmp", [128, 4, 65])
    pA = kb.ps("pA", [128, 512]); pB = kb.ps("pB", [128, 512])
    pS = [kb.ps("pS0", [128, 512]), kb.ps("pS1", [128, 512])]
    pO = [kb.ps("pO0", [128, 512]), kb.ps("pO1", [128, 512])]
    pC = kb.ps("pC", [128, 512]); pD = kb.ps("pD", [128, 512])

    for (wsb, wdr, nm) in wl:
        P.add("gpsimd", lambda e, wsb=wsb, wdr=wdr: e.dma_start(out=wsb[:], in_=wdr.rearrange("(k p) n -> p k n", p=128)), writes=[nm], dma=True)
    if FOX:
        P.add("sync", lambda e: e.dma_start(out=fb_sb[:], in_=fbias[:, :]), writes=["fb"], dma=True)
        P.add("vector", lambda e: e.memset(ones4[:], 1.0), writes=["ones4"])
        P.add("vector", lambda e: e.memset(KA[64:65, :, :], 1.0), writes=["KA1"])
        P.add("vector", lambda e: e.memset(V[:, :, :, 64:65], 1.0), writes=["V1"])
    P.add("gpsimd", lambda e: e.memset(selc[:], 1.0), writes=["selc"])
    P.add("gpsimd", lambda e: e.affine_select(out=selc[:], in_=selc[:], pattern=[[-1, 4], [0, 65]], compare_op=ALU.is_equal, fill=0.0, base=0,
                                              channel_multiplier=1), reads=["selc"], writes=["selc"])
    P.add("gpsimd", lambda e: e.affine_select(out=selc[:], in_=selc[:], pattern=[[0, 4], [1, 65]], compare_op=ALU.is_equal, fill=0.0, base=-64,
                                              channel_multiplier=0), reads=["selc"], writes=["selc"])

    for c in range(NCk):
        hb = c % 2
        csl = slice(c * 512, (c + 1) * 512)
        P.add("gpsimd", lambda e, hb=hb, csl=csl: e.dma_start(out=hTb[:, hb], in_=hT.rearrange("(m p) t -> p m t", p=128)[:, :, csl]),
              writes=[("hTb", hb)], dma=True)
        if FOX:
            for h in range(4):
                for (wsb, wn, dst, pb, pk, sc) in ((wq_sb, "wq", QA, pA, "pA", 0.125), (wk_sb, "wk", KA, pB, "pB", 1.0)):
                    for k in range(8):
                        P.add("tensor", lambda e, k=k, h=h, wsb=wsb, pb=pb, hb=hb: e.matmul(pb[0:64, :], lhsT=wsb[:, k, h * 64:(h + 1) * 64], rhs=hTb[:, hb, k, :],
                                                                                           start=(k == 0), stop=(k == 7)),
                              reads=[("hTb", hb), wn], writes=[pk])
                    P.add("scalar", lambda e, h=h, dst=dst, pb=pb, sc=sc, csl=csl: e.activation(out=dst[0:64, h, csl], in_=pb[0:64, :], func=AF.Copy, scale=sc),
                          reads=[pk], writes=[(wn + "o", h, c)])
            for k in range(8):
                P.add("tensor", lambda e, k=k, hb=hb: e.matmul(pD[0:4, :], lhsT=wf_sb[:, k, :], rhs=hTb[:, hb, k, :], start=(k == 0), stop=(k == 7)),
                      reads=[("hTb", hb), "wf"], writes=["pD"])
            P.add("scalar", lambda e, csl=csl: e.activation(out=fl[:, csl], in_=pD[0:4, :], func=AF.Identity, bias=fb_sb[:], scale=1.0),
                  reads=["pD", "fb"], writes=[("fl", c)])
            for t4 in range(4):
                t = c * 4 + t4
                for k in range(8):
                    P.add("tensor", lambda e, k=k, t4=t4, hb=hb: e.matmul(pD[:, 256:512], lhsT=hTb[:, hb, k, t4 * 128:(t4 + 1) * 128], rhs=wv_sb[:, k, :],
                                                                           start=(k == 0), stop=(k == 7)),
                          reads=[("hTb", hb), "wv"], writes=["pD"])
                P.add("vector", lambda e, t=t: e.tensor_copy(out=V[:, t, :, 0:64], in_=pD[:, 256:512].rearrange("p (h d) -> p h d", h=4)),
                      reads=["pD", "V1"], writes=[("V", t)])
        else:
            for ut in range(2):
                for k in range(8):
                    P.add("tensor", lambda e, k=k, ut=ut, hb=hb: e.matmul(pC[:], lhsT=wu_sb[:, k, ut * 128:(ut + 1) * 128], rhs=hTb[:, hb, k, :],
                                                                           start=(k == 0), stop=(k == 7)),
                          reads=[("hTb", hb), "wu"], writes=["pC"])
                P.add("vector", lambda e, ut=ut, csl=csl: e.tensor_copy(out=uTb[:, ut, csl], in_=pC[:]), reads=["pC"], writes=[("uTb", ut, c)])
    import os
    STAGE = int(os.environ.get('STAGE', '9')); KK = int(os.environ.get('KK', '65'))
    if FOX and STAGE >= 2:
        flk = [("fl", c) for c in range(NCk)]
        P.add("scalar", lambda e: e.activation(out=fl[:], in_=fl[:], func=AF.Exp, scale=-1.0), reads=flk, writes=["fl2"])
        P.add("scalar", lambda e: e.activation(out=fl[:], in_=fl[:], func=AF.Ln, bias=1.0, scale=1.0), reads=["fl2"], writes=["fl3"])
        P.add("vector", lambda e: e.tensor_scalar(out=fl[:], in0=fl[:], scalar1=-1.0, scalar2=None, op0=ALU.mult), reads=["fl3"], writes=["fl4"])
        for c in range(NCk):
            csl = slice(c * 512, (c + 1) * 512)
            ini = 0.0 if c == 0 else cc[:, c * 512 - 1:c * 512]
            P.add("vector", lambda e, csl=csl, ini=ini: e.tensor_tensor_scan(out=cc[:, csl], data0=ones4[:], data1=fl[:, csl], initial=ini, op0=ALU.mult, op1=ALU.add),
                  reads=["fl4", "ones4", "cc"], writes=["cc"])
        for c in range(NCk):
            csl = slice(c * 512, (c + 1) * 512)
            for h in range(4):
                P.add("tensor", lambda e, h=h, csl=csl: e.matmul(pA[0:65, :], lhsT=selc[:, h, :], rhs=cc[:, csl], start=True, stop=True),
                      reads=["cc", "selc"], writes=["pA"])
                P.add("scalar", lambda e, h=h, csl=csl: e.activation(out=QA[64:65, h, csl], in_=pA[64:65, :], func=AF.Copy),
                      reads=["pA"], writes=[("QAc", h, c)])
        for t in range(NT):
            P.add("tensor", lambda e, t=t: e.transpose(out=pB[:, t * 4:(t + 1) * 4], in_=cc[:, t * 128:(t + 1) * 128], identity=ident[0:4, 0:4]),
                  reads=["cc", "ident"], writes=["pB"])
        P.add("scalar", lambda e: e.activation(out=negc[:].rearrange("p t h -> p (t h)"), in_=pB[:, 0:NT * 4], func=AF.Copy, scale=-1.0),
              reads=["pB"], writes=["negc"])

        cnt = 0
        for h in range(4 if STAGE >= 3 else 0):
            for j in range(NCk):
                qk_reads = [("wqo", h, j), ("QAc", h, j)]
                for i in range(4 * j + 4):
                    r = i - 4 * j
                    q0 = 128 * r if r > 0 else 0
                    sb_ = cnt % 2
                    cnt += 1
                    ps = pS[sb_]
                    P.add("tensor", lambda e, h=h, i=i, j=j, q0=q0, ps=ps: e.matmul(ps[:, q0:512], lhsT=KA[0:KK, h, i * 128:(i + 1) * 128],
                                                                                    rhs=QA[0:KK, h, j * 512 + q0:(j + 1) * 512], start=True, stop=True),
                          reads=qk_reads + [("wko", h, i // 4), "KA1"], writes=["pS%d" % sb_])
                    P.add("scalar", lambda e, h=h, i=i, q0=q0, ps=ps, sb_=sb_: e.activation(out=PT[:, sb_, q0:512], in_=ps[:, q0:512], func=AF.Exp,
                                                                                           bias=negc[:, i, h:h + 1], scale=1.0),
                          reads=["pS%d" % sb_, "negc"], writes=[("PT", sb_)])
                    AL = int(os.environ.get('AL', '9'))
                    if r >= 0 and AL >= 2:
                        P.add("gpsimd", lambda e, sb_=sb_, q0=q0: e.affine_select(out=PT[:, sb_, q0:q0 + 128], in_=PT[:, sb_, q0:q0 + 128], pattern=[[1, 128]],
                                                                                   compare_op=ALU.is_ge, fill=0.0, base=0, channel_multiplier=-1),
                              reads=[("PT", sb_)], writes=[("PT", sb_)])
                    for u in range(max(r, 0), 4 if AL >= 3 else 0):
                        po_ = [pO[0], pO[1], pC, pD][u]
                        o0 = 0
                        okey = [('pO', 0), ('pO', 1), 'pC', 'pD'][u]
                        P.add("tensor", lambda e, h=h, i=i, u=u, sb_=sb_, po_=po_, o0=o0, j=j: e.matmul(po_[:, o0:o0 + 65], lhsT=PT[:, sb_, u * 128:(u + 1) * 128],
                                                                                                         rhs=V[:, i, h, 0:65], start=(i == 0), stop=(i == 4 * j + u)),
                              reads=[("PT", sb_), ("V", i), "V1"], writes=[okey])
                        if i == 4 * j + u and AL >= 4:
                            ob = (j * 4 + u) % 2
                            tt = j * 4 + u
                            P.add("vector", lambda e, po_=po_, o0=o0, u=u: e.tensor_copy(out=otmp[:, u, :], in_=po_[:, o0:o0 + 65]),
                                  reads=[okey], writes=[("otmp", u)])
                            P.add("vector", lambda e, u=u: e.reciprocal(out=rec[:, u:u + 1], in_=otmp[:, u, 64:65]),
                                  reads=[("otmp", u)], writes=[("rec", u)])
                            P.add("vector", lambda e, u=u, ob=ob, h=h: e.tensor_scalar(out=ost[:, ob, h * 64:(h + 1) * 64], in0=otmp[:, u, 0:64],
                                                                                     scalar1=rec[:, u:u + 1], scalar2=None, op0=ALU.mult),
                                  reads=[("otmp", u), ("rec", u)], writes=[("ost", ob, h)])
                            P.add("sync", lambda e, ob=ob, h=h, tt=tt: e.dma_start(out=att[tt * 128:(tt + 1) * 128, h * 64:(h + 1) * 64], in_=ost[:, ob, h * 64:(h + 1) * 64]),
                                  reads=[("ost", ob, h)], dma=True, out=True)

    if S5:
        prm = kb.sb("prm", [128, 24, 8])
        prmi = kb.sb("prmi", [128, 1, 8], I32)
        angi = kb.sb("angi", [128, 512], I32)
        bb = kb.sb("bb", [128, 2, 8, 16])
        cs = kb.sb("cs", [128, 2, 8, 16])
        btmp = kb.sb("btmp", [128, 2, 16])
        Xp = kb.sb("Xp", [128, 2, 128])
        Blhs = kb.sb("Blhs", [128, 8, 2, 128], BF16)
        Cl = kb.sb("Cl", [128, 8, 2, 128], BF16)
        Dl = kb.sb("Dl", [128, 2, 128], BF16)
        dsk_sb = kb.sb("dsk_sb", [128, 2])
        jrow = kb.sb("jrow_sb", [128, 512])
        cosT = kb.sb("cosT", [128, 8, 512]); sinT = kb.sb("sinT", [128, 8, 512]); rT = kb.sb("rT", [128, 8, 512])
        ang = kb.sb("ang", [128, 512])
        w = [kb.sb("w%d" % i, [128, 512]) for i in range(6)]
        hreb = kb.sb("hreb", [128, 512], BF16); himb = kb.sb("himb", [128, 512], BF16)
        init = kb.sb("init", [128, 8, 2])
        yst = kb.sb("yst", [128, 2, 512])
        pi_c = kb.sb("pi_c", [128, 1])

        def col(i):
            return prm[:, i, :]
        for (i, dr) in ((0, are), (1, aim), (2, ldt)):
            P.add("sync", lambda e, i=i, dr=dr: e.dma_start(out=prm[:, i, :], in_=dr[:, :]), writes=[("prm", i)], dma=True)
        P.add("sync", lambda e: e.dma_start(out=bb[:, 0], in_=bre[:, :, :]), writes=["bre"], dma=True)
        P.add("sync", lambda e: e.dma_start(out=bb[:, 1], in_=bim[:, :, :]), writes=["bim"], dma=True)
        P.add("sync", lambda e: e.dma_start(out=cs[:, 0], in_=cre[:, :, :]), writes=["cre"], dma=True)
        P.add("sync", lambda e: e.dma_start(out=cs[:, 1], in_=cim[:, :, :]), writes=["cim"], dma=True)
        P.add("sync", lambda e: e.dma_start(out=dsk_sb[:], in_=dsk[:, :]), writes=["dsk"], dma=True)
        P.add("sync", lambda e: e.dma_start(out=jrow[:], in_=jrow_d[:, :]), writes=["jrow"], dma=True)
        P.add("vector", lambda e: e.memset(pi_c[:], -PI), writes=["pi_c"])
        P.add("vector", lambda e: e.memset(init[:], 0.0), writes=[("init", pr) for pr in range(8)])
        K = "prmall"
        A = lambda fn, rd=(), eng="vector": P.add(eng, fn, reads=list(rd) + [K], writes=[K])
        TT = lambda o, a, b, op: A(lambda e: e.tensor_tensor(out=col(o), in0=col(a), in1=col(b), op=op))
        P.add("scalar", lambda e: e.activation(out=col(3), in_=col(2), func=AF.Exp), reads=[("prm", 0), ("prm", 1), ("prm", 2)], writes=[K])
        TT(4, 0, 3, ALU.mult)
        TT(5, 1, 3, ALU.mult)
        A(lambda e: e.activation(out=col(6), in_=col(4), func=AF.Exp), eng="scalar")
        sincos(P, col(5), col(7), col(8), col(20), col(21), prmi[:, 0, :], [K], [K], [K], K)
        TT(9, 6, 8, ALU.mult)
        TT(10, 6, 7, ALU.mult)
        A(lambda e: e.tensor_scalar(out=col(11), in0=col(9), scalar1=-1.0, scalar2=None, op0=ALU.add))
        TT(12, 0, 0, ALU.mult)
        TT(13, 1, 1, ALU.mult)
        TT(12, 12, 13, ALU.add)
        A(lambda e: e.reciprocal(out=col(12), in_=col(12)))
        TT(13, 11, 0, ALU.mult); TT(14, 10, 1, ALU.mult); TT(13, 13, 14, ALU.add); TT(13, 13, 12, ALU.mult)
        TT(14, 10, 0, ALU.mult); TT(15, 11, 1, ALU.mult); TT(14, 14, 15, ALU.subtract); TT(14, 14, 12, ALU.mult)
        A(lambda e: e.tensor_scalar(out=col(15), in0=col(14), scalar1=-1.0, scalar2=None, op0=ALU.mult))
        A(lambda e: e.tensor_scalar(out=col(16), in0=col(5), scalar1=512.0, scalar2=None, op0=ALU.mult))
        sincos(P, col(16), col(17), col(18), col(20), col(21), prmi[:, 0, :], [K], [K], [K], K)
        A(lambda e: e.tensor_scalar(out=col(19), in0=col(17), scalar1=-1.0, scalar2=None, op0=ALU.mult))
        for pr in range(8):
            zr = prm[:, 13, pr:pr + 1]; zi = prm[:, 14, pr:pr + 1]; nzi = prm[:, 15, pr:pr + 1]
            P.add("vector", lambda e, pr=pr, zr=zr: e.tensor_scalar(out=btmp[:, 0, :], in0=bb[:, 0, pr, :], scalar1=zr, scalar2=None, op0=ALU.mult),
                  reads=[K, "bre"], writes=["btmp0"])
            P.add("vector", lambda e, pr=pr, zi=zi: e.tensor_scalar(out=btmp[:, 1, :], in0=bb[:, 0, pr, :], scalar1=zi, scalar2=None, op0=ALU.mult),
                  reads=[K, "bre"], writes=["btmp1"])
            P.add("vector", lambda e, pr=pr, nzi=nzi: e.scalar_tensor_tensor(out=bb[:, 0, pr, :], in0=bb[:, 1, pr, :], scalar=nzi, in1=btmp[:, 0, :], op0=ALU.mult, op1=ALU.add),
                  reads=[K, "bim", "btmp0", "bre", "btmp1"], writes=["bre"])
            P.add("vector", lambda e, pr=pr, zr=zr: e.scalar_tensor_tensor(out=bb[:, 1, pr, :], in0=bb[:, 1, pr, :], scalar=zr, in1=btmp[:, 1, :], op0=ALU.mult, op1=ALU.add),
                  reads=[K, "bim", "btmp1", "bre"], writes=["bim"])
        P.add("gpsimd", lambda e: e.memset(Cl[:], 0.0), writes=["Cl"])
        for pr in range(8):
            pi_ = pr % 4
            for part in range(2):
                xb = part
                P.add("gpsimd", lambda e, xb=xb: e.memset(Xp[:, xb, :], 0.0), writes=[("Xp", xb)])
                P.add("vector", lambda e, xb=xb, pr=pr, part=part, pi_=pi_: e.tensor_copy(out=Xp[0:64, xb, 32 * pi_:32 * pi_ + 16], in_=bb[0:64, part, pr, :]),
                      reads=["bre", "bim", ("Xp", xb)], writes=[("Xp", xb)])
                P.add("vector", lambda e, xb=xb, pr=pr, part=part, pi_=pi_: e.tensor_copy(out=Xp[64:128, xb, 32 * pi_ + 16:32 * pi_ + 32], in_=bb[64:128, part, pr, :]),
                      reads=["bre", "bim", ("Xp", xb)], writes=[("Xp", xb)])
                P.add("tensor", lambda e, xb=xb: e.transpose(out=pD[:, xb * 128:(xb + 1) * 128], in_=Xp[:, xb, :], identity=ident[:]),
                      reads=[("Xp", xb), "ident"], writes=[("pDx", xb)])
                P.add("scalar", lambda e, xb=xb, pr=pr, part=part: e.activation(out=Blhs[:, pr, part, :], in_=pD[:, xb * 128:(xb + 1) * 128], func=AF.Copy),
                      reads=[("pDx", xb)], writes=[("Blhs", pr)])
                P.add("vector", lambda e, pr=pr, part=part, pi_=pi_: e.tensor_copy(out=Cl[0:64, pr, part, 32 * pi_:32 * pi_ + 16], in_=cs[0:64, part, pr, :]),
                      reads=["cre", "cim", "Cl"], writes=["Cl"])
                P.add("vector", lambda e, pr=pr, part=part, pi_=pi_: e.tensor_copy(out=Cl[64:128, pr, part, 32 * pi_ + 16:32 * pi_ + 32], in_=cs[64:128, part, pr, :]),
                      reads=["cre", "cim", "Cl"], writes=["Cl"])
        for ut in range(2):
            P.add("vector", lambda e, ut=ut: e.tensor_scalar(out=Dl[:, ut, :], in0=ident[:], scalar1=dsk_sb[:, ut:ut + 1], scalar2=None, op0=ALU.mult),
                  reads=["ident", "dsk"], writes=["Dl"])
        for pr in range(8):
            th = prm[:, 5, pr:pr + 1]
            P.add("vector", lambda e, th=th: e.tensor_scalar(out=ang[:], in0=jrow[:], scalar1=th, scalar2=None, op0=ALU.mult), reads=["jrow", K], writes=["ang"])
            sincos(P, ang[:], sinT[:, pr, :], cosT[:, pr, :], w[0][:], w[1][:], angi[:], ["ang"], [("sinT", pr)], [("cosT", pr)], "w01")
            P.add("gpsimd", lambda e, pr=pr: e.memset(rT[:, pr, :], 1.0), writes=[("rT", pr)])
            P.add("gpsimd", lambda e, pr=pr: e.tensor_scalar(out=rT[:, pr, :], in0=rT[:, pr, :], scalar1=prm[:, 6, pr:pr + 1], scalar2=None, op0=ALU.mult),
                  reads=[("rT", pr), K], writes=[("rT", pr)])
        for c in range(NCk):
            csl = slice(c * 512, (c + 1) * 512)
            for ut in range(2):
                for pi_ in range(4):
                    pr = ut * 4 + pi_
                    P.add("tensor", lambda e, pr=pr, ut=ut, csl=csl: e.matmul(pA[:], lhsT=Blhs[:, pr, 0, :], rhs=uTb[:, ut, csl], start=True, stop=True),
                          reads=[("Blhs", pr), ("uTb", ut, c)], writes=["pA"])
                    P.add("tensor", lambda e, pr=pr, ut=ut, csl=csl: e.matmul(pB[:], lhsT=Blhs[:, pr, 1, :], rhs=uTb[:, ut, csl], start=True, stop=True),
                          reads=[("Blhs", pr), ("uTb", ut, c)], writes=["pB"])
                    ck, sk = ("cosT", pr), ("sinT", pr)
                    P.add("vector", lambda e, pr=pr: e.tensor_tensor(out=w[0][:], in0=cosT[:, pr, :], in1=pA[:], op=ALU.mult), reads=[ck, "pA"], writes=["w0"])
                    P.add("vector", lambda e, pr=pr: e.tensor_tensor(out=w[1][:], in0=sinT[:, pr, :], in1=pB[:], op=ALU.mult), reads=[sk, "pB"], writes=["w1"])
                    P.add("vector", lambda e, pr=pr: e.tensor_tensor(out=w[2][:], in0=cosT[:, pr, :], in1=pB[:], op=ALU.mult), reads=[ck, "pB"], writes=["w2"])
                    P.add("vector", lambda e, pr=pr: e.tensor_tensor(out=w[3][:], in0=sinT[:, pr, :], in1=pA[:], op=ALU.mult), reads=[sk, "pA"], writes=["w3"])
                    P.add("gpsimd", lambda e: e.tensor_tensor(out=w[0][:], in0=w[0][:], in1=w[1][:], op=ALU.add), reads=["w0", "w1"], writes=["w0"])
                    P.add("gpsimd", lambda e: e.tensor_tensor(out=w[2][:], in0=w[2][:], in1=w[3][:], op=ALU.subtract), reads=["w2", "w3"], writes=["w2"])
                    P.add("vector", lambda e, pr=pr: e.tensor_tensor_scan(out=w[4][:], data0=rT[:, pr, :], data1=w[0][:], initial=init[:, pr, 0:1], op0=ALU.mult, op1=ALU.add),
                          reads=[("rT", pr), "w0", ("init", pr)], writes=["w4"])
                    P.add("vector", lambda e, pr=pr: e.tensor_tensor_scan(out=w[5][:], data0=rT[:, pr, :], data1=w[2][:], initial=init[:, pr, 1:2], op0=ALU.mult, op1=ALU.add),
                          reads=[("rT", pr), "w2", ("init", pr)], writes=["w5"])
                    cL = prm[:, 18, pr:pr + 1]; sL = prm[:, 17, pr:pr + 1]; nsL = prm[:, 19, pr:pr + 1]
                    P.add("vector", lambda e, pr=pr, cL=cL: e.tensor_scalar(out=init[:, pr, 0:1], in0=w[4][:, 511:512], scalar1=cL, scalar2=None, op0=ALU.mult),
                          reads=["w4", K, ("init", pr)], writes=[("init", pr)])
                    P.add("vector", lambda e, pr=pr, nsL=nsL: e.scalar_tensor_tensor(out=init[:, pr, 0:1], in0=w[5][:, 511:512], scalar=nsL, in1=init[:, pr, 0:1], op0=ALU.mult, op1=ALU.add),
                          reads=["w5", K, ("init", pr)], writes=[("init", pr)])
                    P.add("vector", lambda e, pr=pr, sL=sL: e.tensor_scalar(out=init[:, pr, 1:2], in0=w[4][:, 511:512], scalar1=sL, scalar2=None, op0=ALU.mult),
                          reads=["w4", K, ("init", pr)], writes=[("init", pr)])
                    P.add("vector", lambda e, pr=pr, cL=cL: e.scalar_tensor_tensor(out=init[:, pr, 1:2], in0=w[5][:, 511:512], scalar=cL, in1=init[:, pr, 1:2], op0=ALU.mult, op1=ALU.add),
                          reads=["w5", K, ("init", pr)], writes=[("init", pr)])
                    P.add("gpsimd", lambda e, pr=pr: e.tensor_tensor(out=w[0][:], in0=cosT[:, pr, :], in1=w[4][:], op=ALU.mult), reads=[ck, "w4", "w0"], writes=["w0"])
                    P.add("gpsimd", lambda e, pr=pr: e.tensor_tensor(out=w[1][:], in0=sinT[:, pr, :], in1=w[5][:], op=ALU.mult), reads=[sk, "w5", "w1"], writes=["w1"])
                    P.add("vector", lambda e: e.tensor_tensor(out=hreb[:], in0=w[0][:], in1=w[1][:], op=ALU.subtract), reads=["w0", "w1"], writes=["hreb"])
                    P.add("gpsimd", lambda e, pr=pr: e.tensor_tensor(out=w[2][:], in0=sinT[:, pr, :], in1=w[4][:], op=ALU.mult), reads=[sk, "w4", "w2"], writes=["w2"])
                    P.add("gpsimd", lambda e, pr=pr: e.tensor_tensor(out=w[3][:], in0=cosT[:, pr, :], in1=w[5][:], op=ALU.mult), reads=[ck, "w5", "w3"], writes=["w3"])
                    P.add("vector", lambda e: e.scalar_tensor_tensor(out=himb[:], in0=w[2][:], scalar=-1.0, in1=w[3][:], op0=ALU.mult, op1=ALU.subtract),
                          reads=["w2", "w3"], writes=["himb"])
                    P.add("tensor", lambda e, pr=pr, pi_=pi_: e.matmul(pC[:], lhsT=Cl[:, pr, 0, :], rhs=hreb[:], start=(pi_ == 0), stop=False),
                          reads=["Cl", "hreb"], writes=["pC"])
                    P.add("tensor", lambda e, pr=pr: e.matmul(pC[:], lhsT=Cl[:, pr, 1, :], rhs=himb[:], start=False, stop=False),
                          reads=["Cl", "himb"], writes=["pC"])
                P.add("tensor", lambda e, ut=ut, csl=csl: e.matmul(pC[:], lhsT=Dl[:, ut, :], rhs=uTb[:, ut, csl], start=False, stop=True),
                      reads=["Dl", ("uTb", ut, c)], writes=["pC"])
                ob = (c * 2 + ut) % 2
                P.add("scalar", lambda e, ob=ob: e.activation(out=yst[:, ob, :], in_=pC[:], func=AF.Copy), reads=["pC"], writes=[("yst", ob)])
                P.add("sync", lambda e, ob=ob, ut=ut, csl=csl: e.dma_start(out=ysT[ut * 128:(ut + 1) * 128, csl], in_=yst[:, ob, :]),
                      reads=[("yst", ob)], dma=True, out=True)
    print("even ops", P.nops())
    return kb.finish()


def build_odd(S=4096):
    kb = KB()
    nc, P = kb.nc, kb.P
    NCk = S // 512
    NT = S // 128
    SC = 128 ** -0.5
    hT = kb.din("hT", [1024, S])
    wq = kb.din("wq", [1024, 512]); wk = kb.din("wk", [1024, 512]); wv = kb.din("wv", [1024, 512]); wo = kb.din("wo", [1024, 512])
    wi = kb.din("wi", [1024, 4]); wf = kb.din("wf", [1024, 4])
    cw = kb.din("cw", [128, 8, 4]); cb = kb.din("cb", [128, 8])
    ibias = kb.din("ibias", [4, 1]); fbias = kb.din("fbias", [4, 1])
    mixg = kb.dout("mixg", [S, 512])

    ident = make_ident(kb)
    hTb = kb.sb("hTb", [128, 2, 8, 512], BF16)
    wq_sb = kb.sb("wq_sb", [128, 8, 512], BF16); wk_sb = kb.sb("wk_sb", [128, 8, 512], BF16)
    wv_sb = kb.sb("wv_sb", [128, 8, 512], BF16); wo_sb = kb.sb("wo_sb", [128, 8, 512], BF16)
    wi_sb = kb.sb("wi_sb", [128, 8, 4], BF16); wf_sb = kb.sb("wf_sb", [128, 8, 4], BF16)
    cw_sb = kb.sb("cw_sb", [128, 8, 4]); cb_sb = kb.sb("cb_sb", [128, 8])
    ib_sb = [kb.sb("ib0", [2, 1]), kb.sb("ib1", [2, 1])]
    fb_sb = [kb.sb("fb0", [2, 1]), kb.sb("fb1", [2, 1])]
    QT = kb.sb("QT", [128, 2, S], BF16)
    KT = kb.sb("KT", [128, 2, S], BF16)
    V = kb.sb("V", [128, NT, 2, 132], BF16)
    OG = kb.sb("OG", [128, NT, 256], BF16)
    gi = kb.sb("gi", [2, S]); gf = kb.sb("gf", [2, S])
    ones2 = kb.sb("ones2", [2, 512])
    selF = kb.sb("selF", [2, 2, 128])
    nbT = kb.sb("nbT", [128, NT, 2])
    pre = kb.sb("pre", [128, 4, 515])
    acc = kb.sb("acc", [128, 2, 512])
    E = kb.sb("E", [128, 2, 512])
    W = kb.sb("W", [128, 2, 512], BF16)
    otmp = kb.sb("otmp", [128, 4, 129])
    rec = kb.sb("rec", [128, 4])
    ost = kb.sb("ost", [128, 2, 128])
    pA = kb.ps("pA", [128, 512]); pB = kb.ps("pB", [128, 512])
    pS = [kb.ps("pS0", [128, 512]), kb.ps("pS1", [128, 512])]
    pO = [kb.ps("pO%d" % u, [128, 512]) for u in range(4)]

    for (wsb, wdr, nm) in ((wq_sb, wq, "wq"), (wk_sb, wk, "wk"), (wv_sb, wv, "wv"), (wo_sb, wo, "wo"), (wi_sb, wi, "wi"), (wf_sb, wf, "wf")):
        P.add("gpsimd", lambda e, wsb=wsb, wdr=wdr: e.dma_start(out=wsb[:], in_=wdr.rearrange("(k p) n -> p k n", p=128)), writes=[nm], dma=True)
    P.add("sync", lambda e: e.dma_start(out=cw_sb[:], in_=cw[:, :, :]), writes=["cw"], dma=True)
    P.add("sync", lambda e: e.dma_start(out=cb_sb[:], in_=cb[:, :]), writes=["cb"], dma=True)
    for hp in range(2):
        P.add("sync", lambda e, hp=hp: e.dma_start(out=ib_sb[hp][:], in_=ibias[hp * 2:hp * 2 + 2, :]), writes=[("ib", hp)], dma=True)
        P.add("sync", lambda e, hp=hp: e.dma_start(out=fb_sb[hp][:], in_=fbias[hp * 2:hp * 2 + 2, :]), writes=[("fb", hp)], dma=True)
    P.add("vector", lambda e: e.memset(ones2[:], 1.0), writes=["ones2"])
    P.add("vector", lambda e: e.memset(V[:, :, :, 128:129], 1.0), writes=["V1"])
    P.add("gpsimd", lambda e: e.memset(selF[:], 1.0), writes=["selF"])
    P.add("gpsimd", lambda e: e.affine_select(out=selF[:], in_=selF[:], pattern=[[-1, 2], [0, 128]], compare_op=ALU.is_equal, fill=0.0, base=0,
                                              channel_multiplier=1), reads=["selF"], writes=["selF"])
    cnt = 0
    for hp in range(2):
        P.add("vector", lambda e: e.memset(pre[:, :, 0:3], 0.0), reads=[("pre", i) for i in range(4)], writes=[("pre", i) for i in range(4)])
        for c in range(NCk):
            hb = c % 2
            csl = slice(c * 512, (c + 1) * 512)
            P.add("gpsimd", lambda e, hb=hb, csl=csl: e.dma_start(out=hTb[:, hb], in_=hT.rearrange("(m p) t -> p m t", p=128)[:, :, csl]),
                  writes=[("hTb", hb)], dma=True)
            for hl in range(2):
                h = hp * 2 + hl
                for qk, (wsb, wn, dst, pb, pk) in enumerate(((wq_sb, "wq", QT, pA, "pA"), (wk_sb, "wk", KT, pB, "pB"))):
                    idx = qk * 2 + hl
                    ci = qk * 4 + h
                    ab = qk
                    for k in range(8):
                        P.add("tensor", lambda e, k=k, h=h, wsb=wsb, pb=pb, hb=hb: e.matmul(pb[:], lhsT=wsb[:, k, h * 128:(h + 1) * 128], rhs=hTb[:, hb, k, :],
                                                                                           start=(k == 0), stop=(k == 7)),
                              reads=[("hTb", hb), wn], writes=[pk])
                    P.add("scalar", lambda e, idx=idx, pb=pb: e.activation(out=pre[:, idx, 3:515], in_=pb[:], func=AF.Copy),
                          reads=[pk, ("pre", idx)], writes=[("pre", idx)])
                    P.add("vector", lambda e, idx=idx, ci=ci, ab=ab: e.tensor_scalar(out=acc[:, ab, :], in0=pre[:, idx, 0:512], scalar1=cw_sb[:, ci, 0:1],
                                                                                     scalar2=None, op0=ALU.mult),
                          reads=[("pre", idx), "cw"], writes=[("acc", ab)])
                    for jj in range(1, 4):
                        P.add("vector", lambda e, idx=idx, ci=ci, ab=ab, jj=jj: e.scalar_tensor_tensor(out=acc[:, ab, :], in0=pre[:, idx, jj:jj + 512],
                                                                                                      scalar=cw_sb[:, ci, jj:jj + 1], in1=acc[:, ab, :],
                                                                                                      op0=ALU.mult, op1=ALU.add),
                              reads=[("pre", idx), "cw", ("acc", ab)], writes=[("acc", ab)])
                    P.add("scalar", lambda e, dst=dst, hl=hl, ab=ab, ci=ci, csl=csl: e.activation(out=dst[:, hl, csl], in_=acc[:, ab, :], func=AF.Silu,
                                                                                                 bias=cb_sb[:, ci:ci + 1], scale=1.0),
                          reads=[("acc", ab), "cb"], writes=[(wn + "o", hl, c)])
                    P.add("vector", lambda e, idx=idx: e.tensor_copy(out=pre[:, idx, 0:3], in_=pre[:, idx, 512:515]),
                          reads=[("pre", idx)], writes=[("pre", idx)])
            for (wsb, wn, gt, gk, bs, bk, u) in ((wi_sb, "wi", gi, "gi", ib_sb[hp], ("ib", hp), 0), (wf_sb, "wf", gf, "gf", fb_sb[hp], ("fb", hp), 1)):
                for k in range(8):
                    P.add("tensor", lambda e, k=k, wsb=wsb, u=u, hb=hb, hp=hp: e.matmul(pO[u][0:2, :], lhsT=wsb[:, k, hp * 2:hp * 2 + 2], rhs=hTb[:, hb, k, :],
                                                                                start=(k == 0), stop=(k == 7)),
                          reads=[("hTb", hb), wn], writes=[("pO", u)])
                P.add("scalar", lambda e, gt=gt, bs=bs, u=u, csl=csl: e.activation(out=gt[:, csl], in_=pO[u][0:2, :], func=AF.Identity, bias=bs[:], scale=1.0),
                      reads=[("pO", u), bk], writes=[(gk, c), gk + "2"])
            for t4 in range(4):
                t = c * 4 + t4
                for k in range(8):
                    P.add("tensor", lambda e, k=k, t4=t4, hb=hb, hp=hp: e.matmul(pO[2][:, 0:256], lhsT=hTb[:, hb, k, t4 * 128:(t4 + 1) * 128],
                                                                           rhs=wv_sb[:, k, hp * 256:(hp + 1) * 256], start=(k == 0), stop=(k == 7)),
                          reads=[("hTb", hb), "wv"], writes=[("pO", 2)])
                P.add("vector", lambda e, t=t: e.tensor_copy(out=V[:, t, :, 0:128], in_=pO[2][:, 0:256].rearrange("p (h d) -> p h d", h=2)),
                      reads=[("pO", 2)], writes=[("V", t)])
                for k in range(8):
                    P.add("tensor", lambda e, k=k, t4=t4, hb=hb, hp=hp: e.matmul(pO[3][:, 0:256], lhsT=hTb[:, hb, k, t4 * 128:(t4 + 1) * 128],
                                                                           rhs=wo_sb[:, k, hp * 256:(hp + 1) * 256], start=(k == 0), stop=(k == 7)),
                          reads=[("hTb", hb), "wo"], writes=[("pO", 3)])
                P.add("scalar", lambda e, t=t: e.activation(out=OG[:, t, :], in_=pO[3][:, 0:256], func=AF.Sigmoid),
                      reads=[("pO", 3)], writes=[("OG", t)])
        gfk = [("gf", c) for c in range(NCk)]
        gik = [("gi", c) for c in range(NCk)]
        P.add("scalar", lambda e: e.activation(out=gf[:], in_=gf[:], func=AF.Exp, scale=-1.0), reads=gfk, writes=["gf2"])
        P.add("scalar", lambda e: e.activation(out=gf[:], in_=gf[:], func=AF.Ln, bias=1.0, scale=1.0), reads=["gf2"], writes=["gf2"])
        P.add("vector", lambda e: e.tensor_scalar(out=gf[:], in0=gf[:], scalar1=-1.0, scalar2=None, op0=ALU.mult), reads=["gf2"], writes=["gf2"])
        for c in range(NCk):
            csl = slice(c * 512, (c + 1) * 512)
            ini = 0.0 if c == 0 else gf[:, c * 512 - 1:c * 512]
            P.add("vector", lambda e, csl=csl, ini=ini: e.tensor_tensor_scan(out=gf[:, csl], data0=ones2[:], data1=gf[:, csl], initial=ini, op0=ALU.mult, op1=ALU.add),
                  reads=["gf2", "ones2"], writes=["gf2"])
        P.add("vector", lambda e: e.tensor_tensor(out=gi[:], in0=gi[:], in1=gf[:], op=ALU.subtract), reads=gik + ["gf2"], writes=["gi2"])
        for t in range(NT):
            P.add("tensor", lambda e, t=t: e.transpose(out=pB[:, t * 2:(t + 1) * 2], in_=gi[:, t * 128:(t + 1) * 128], identity=ident[0:2, 0:2]),
                  reads=["gi2", "ident"], writes=["pB"])
        P.add("vector", lambda e: e.tensor_copy(out=nbT[:].rearrange("p t h -> p (t h)"), in_=pB[:, 0:NT * 2]), reads=["pB"], writes=["nbT"])
        for hl in range(2):
            h = hp * 2 + hl
            for j in range(NCk):
                P.add("tensor", lambda e, hl=hl, j=j: e.matmul(pA[:], lhsT=selF[:, hl, :], rhs=gf[:, j * 512:(j + 1) * 512], start=True, stop=True),
                      reads=["gf2", "selF"], writes=["pA"])
                for i in range(4 * j + 4):
                    r = i - 4 * j
                    q0 = 128 * r if r > 0 else 0
                    sb_ = cnt % 2
                    cnt += 1
                    ps = pS[sb_]
                    P.add("tensor", lambda e, hl=hl, i=i, j=j, q0=q0, ps=ps: e.matmul(ps[:, q0:512], lhsT=KT[:, hl, i * 128:(i + 1) * 128],
                                                                                     rhs=QT[:, hl, j * 512 + q0:(j + 1) * 512], start=True, stop=True),
                          reads=[("wqo", hl, j), ("wko", hl, i // 4)], writes=["pS%d" % sb_])
                    P.add("scalar", lambda e, hl=hl, i=i, q0=q0, sb_=sb_: e.activation(out=E[:, sb_, q0:512], in_=pA[:, q0:512], func=AF.Exp,
                                                                                      bias=nbT[:, i, hl:hl + 1], scale=1.0),
                          reads=["pA", "nbT"], writes=[("E", sb_)])
                    P.add("vector", lambda e, q0=q0, sb_=sb_, ps=ps: e.scalar_tensor_tensor(out=W[:, sb_, q0:512], in0=ps[:, q0:512], scalar=SC, in1=E[:, sb_, q0:512],
                                                                                           op0=ALU.mult, op1=ALU.mult),
                          reads=["pS%d" % sb_, ("E", sb_)], writes=[("W", sb_)])
                    if r >= 0:
                        P.add("gpsimd", lambda e, sb_=sb_, q0=q0: e.affine_select(out=W[:, sb_, q0:q0 + 128], in_=W[:, sb_, q0:q0 + 128], pattern=[[1, 128]],
                                                                                   compare_op=ALU.is_ge, fill=0.0, base=0, channel_multiplier=-1),
                              reads=[("W", sb_)], writes=[("W", sb_)])
                    for u in range(max(r, 0), 4):
                        P.add("tensor", lambda e, hl=hl, i=i, u=u, sb_=sb_, j=j: e.matmul(pO[u][:, 0:129], lhsT=W[:, sb_, u * 128:(u + 1) * 128],
                                                                                         rhs=V[:, i, hl, 0:129], start=(i == 0), stop=(i == 4 * j + u)),
                              reads=[("W", sb_), ("V", i), "V1"], writes=[("pO", u)])
                        if i == 4 * j + u:
                            tt = j * 4 + u
                            ob = tt % 2
                            P.add("vector", lambda e, u=u: e.tensor_copy(out=otmp[:, u, :], in_=pO[u][:, 0:129]), reads=[("pO", u)], writes=[("otmp", u)])
                            P.add("scalar", lambda e, u=u: e.activation(out=rec[:, u:u + 1], in_=otmp[:, u, 128:129], func=AF.Abs),
                                  reads=[("otmp", u)], writes=[("rec", u)])
                            P.add("vector", lambda e, u=u: e.tensor_scalar(out=rec[:, u:u + 1], in0=rec[:, u:u + 1], scalar1=1.0, scalar2=None, op0=ALU.max),
                                  reads=[("rec", u)], writes=[("rec", u)])
                            P.add("vector", lambda e, u=u: e.reciprocal(out=rec[:, u:u + 1], in_=rec[:, u:u + 1]), reads=[("rec", u)], writes=[("rec", u)])
                            P.add("vector", lambda e, u=u, ob=ob, hl=hl, tt=tt: e.scalar_tensor_tensor(out=ost[:, ob, :], in0=otmp[:, u, 0:128], scalar=rec[:, u:u + 1],
                                                                                                      in1=OG[:, tt, hl * 128:(hl + 1) * 128], op0=ALU.mult, op1=ALU.mult),
                                  reads=[("otmp", u), ("rec", u), ("OG", tt)], writes=[("ost", ob)])
                            P.add("sync", lambda e, ob=ob, h=h, tt=tt: e.dma_start(out=mixg[tt * 128:(tt + 1) * 128, h * 128:(h + 1) * 128], in_=ost[:, ob, :]),
                                  reads=[("ost", ob)], dma=True, out=True)
    print("odd ops", P.nops())
    return kb.finish()


def _even_inputs(hT, j, hh, d):
    w_in = d['even_w_in'][j]
    hs = slice(hh * 256, (hh + 1) * 256)
    g0 = hh * 16
    c = np.ascontiguousarray

    def pl(a):
        return c(a[g0:g0 + 16].reshape(8, 2, 64).transpose(1, 2, 0).reshape(128, 8))

    def plb(a):
        return c(a[g0:g0 + 16].reshape(8, 2, 64, 16).transpose(1, 2, 0, 3).reshape(128, 8, 16))

    def plc(a):
        return c(a[g0:g0 + 16].reshape(8, 2, 16, 64).transpose(1, 3, 0, 2).reshape(128, 8, 16))
    ldt = np.repeat(d['s5_log_dt'][j][:, None], 64, 1)
    fox = dict(hT=hT, wq=c(w_in[:, 0:512][:, hs]), wk=c(w_in[:, 512:1024][:, hs]), wv=c(w_in[:, 1024:1536][:, hs]),
               wf=c(w_in[:, 1536 + hh * 4:1536 + hh * 4 + 4]), fbias=c(d['fox_f_bias'][j][hh * 4:hh * 4 + 4, None]))
    s5 = dict(hT=hT, wu=c(w_in[:, 1544:][:, hs]), are=pl(d['s5_a_re'][j]), aim=pl(d['s5_a_im'][j]), ldt=pl(ldt),
              bre=plb(d['s5_b_re'][j]), bim=plb(d['s5_b_im'][j]), cre=plc(d['s5_c_re'][j]), cim=plc(d['s5_c_im'][j]),
              dsk=c(d['s5_d'][j][g0:g0 + 16].reshape(2, 128).T), jrow=np.tile(np.arange(512, dtype=np.float32), (128, 1)))
    return fox, s5


def _odd_inputs(hT, j, hh, d):
    c = np.ascontiguousarray
    w_in = d['odd_w_in'][j]
    cs = slice(hh * 512, (hh + 1) * 512)
    cwf = d['mlstm_conv_w'][j]
    cbf = d['mlstm_conv_b'][j]
    qcols = np.arange(hh * 512, (hh + 1) * 512)
    cols = np.concatenate([qcols, 1024 + qcols])
    cw = c(cwf[:, cols].reshape(4, 8, 128).transpose(2, 1, 0))
    cb = c(cbf[cols].reshape(8, 128).T)
    return dict(hT=hT, wq=c(w_in[:, 0:1024][:, cs]), wk=c(w_in[:, 1024:2048][:, cs]), wv=c(w_in[:, 2048:3072][:, cs]),
                wo=c(w_in[:, 3072:4096][:, cs]), wi=c(w_in[:, 4096 + hh * 4:4096 + hh * 4 + 4]),
                wf=c(w_in[:, 4104 + hh * 4:4104 + hh * 4 + 4]), cw=cw, cb=cb,
                ibias=c(d['mlstm_i_bias'][j][hh * 4:hh * 4 + 4, None]), fbias=c(d['mlstm_f_bias'][j][hh * 4:hh * 4 + 4, None]))


def kernel(**d):
    d = {k: np.asarray(v) for k, v in d.items()}
    x = d['x']
    B, S, D = x.shape
    cores = list(range(8))
    c = np.ascontiguousarray
    h = x.reshape(B * S, D).astype(np.float32)
    for layer in range(4):
        j = layer // 2
        even = (layer % 2 == 0)
        hb = h.reshape(B, S, D)
        if even:
            fins, sins = [], []
            for core in cores:
                b, hh = core // 2, core % 2
                f_, s_ = _even_inputs(c(hb[b].T), j, hh, d)
                fins.append(f_); sins.append(s_)
            rf = run_bass_kernel_spmd(build_even(S, 'fox'), fins, core_ids=cores).results
            rs = run_bass_kernel_spmd(build_even(S, 's5'), sins, core_ids=cores).results
            att = np.zeros((B, S, 512), np.float32)
            ys = np.zeros((B, S, 512), np.float32)
            for core in cores:
                b, hh = core // 2, core % 2
                att[b, :, hh * 256:(hh + 1) * 256] = rf[core]['att']
                ys[b, :, hh * 256:(hh + 1) * 256] = rs[core]['ysT'].T
            att = att.reshape(B * S, 512); ys = ys.reshape(B * S, 512)
        else:
            oins = []
            for core in cores:
                b, hh = core // 2, core % 2
                oins.append(_odd_inputs(c(hb[b].T), j, hh, d))
            ro = run_bass_kernel_spmd(build_odd(S), oins, core_ids=cores).results
            mix = np.zeros((B, S, 1024), np.float32)
            for core in cores:
                b, hh = core // 2, core % 2
                mix[b, :, hh * 512:(hh + 1) * 512] = ro[core]['mixg']
            mix = mix.reshape(B * S, 1024)
        wr = c(np.concatenate([d['moe_w_group'][layer], d['moe_w_expert'][layer].transpose(1, 0, 2).reshape(1024, 16)], 1))
        br = c(np.concatenate([d['moe_b_group'][layer], d['moe_b_expert'][layer].reshape(16)]))
        lnp = c(np.stack([d['ln_g'][layer, 0], d['ln_b'][layer, 0], d['ln_g'][layer, 1], d['ln_b'][layer, 1]]))
        pins = []
        for core in cores:
            ts = slice(core * TOK, (core + 1) * TOK)
            im = dict(hT=c(h[ts].T), lnp=lnp, wr=wr, br=br, wg=d['moe_w_gate'][layer], wu=d['moe_w_up'][layer], wd=d['moe_w_down'][layer])
            if even:
                im.update(attT=c(att[ts].T), ysT=c(ys[ts].T), w_glu=d['s5_w_glu'][j], b_glu=d['s5_b_glu'][j], w_out=d['even_w_out'][j])
            else:
                im.update(mixT=c(mix[ts].T), w_out=d['odd_w_out'][j])
            pins.append(im)
        rp = run_bass_kernel_spmd(build_post(even, TOK), pins, core_ids=cores).results
        h = np.concatenate([rp[core]['houtT'].T for core in cores], 0)
    return c(h.reshape(B, S, D).astype(np.float32))
```

```python
import numpy as np
import concourse.bass as bass
import concourse.mybir as mybir
from concourse.bass_utils import run_bass_kernel_spmd

F32 = mybir.dt.float32
BF16 = mybir.dt.bfloat16
ALU = mybir.AluOpType
AF = mybir.ActivationFunctionType
AX = mybir.AxisListType

ENGS = ["sync", "scalar", "vector", "gpsimd", "tensor"]
NDSEM = 24


class Op:
    __slots__ = ("eng", "fn", "idx", "waits", "dwaits", "signal", "sval", "is_dma", "dnum", "guard", "is_cc", "ccnum", "ccwaits", "phase")

    def __init__(self, eng, fn, is_dma):
        self.eng = eng
        self.fn = fn
        self.waits = []
        self.dwaits = []
        self.signal = False
        self.sval = 0
        self.is_dma = is_dma
        self.dnum = -1
        self.guard = None
        self.is_cc = False
        self.ccnum = -1
        self.ccwaits = []
        self.phase = 0


class Prog:
    def __init__(self, nc):
        self.nc = nc
        self.ops = {e: [] for e in ENGS}
        self.last_writer = {}
        self.readers = {}
        self.seen = {e: {f: -1 for f in ENGS} for e in ENGS}
        self.seen_dma = {e: set() for e in ENGS}
        self.ndma = 0
        self.ncc = 0
        self.seen_cc = {e: -1 for e in ENGS}
        self.out_dmas = []
        self.dma_ops = []
        self.cc_ops = []
        self.use_pid = False
        self.pidval = None
        self.phase = 0
        self.last_compute = {e: None for e in ENGS}

    def barrier(self, wait_cc=True):
        for e in ENGS:
            b = Op(e, None, False)
            b.idx = len(self.ops[e])
            b.phase = self.phase
            for f in ENGS:
                lc = self.last_compute[f]
                if f == e or lc is None:
                    continue
                if lc.phase == self.phase and self.seen[e][f] >= lc.idx:
                    continue
                lc.signal = True
                b.waits.append(lc)
            for d in self.dma_ops[-NDSEM:]:
                if d.dnum not in self.seen_dma[e]:
                    b.dwaits.append(d)
            if wait_cc and self.cc_ops and self.seen_cc[e] < self.cc_ops[-1].ccnum:
                b.ccwaits.append(self.cc_ops[-1])
            self.ops[e].append(b)
        self.phase += 1
        for e in ENGS:
            for f in ENGS:
                self.seen[e][f] = -1
            self.seen_dma[e] = set(range(self.ndma))
            if wait_cc:
                self.seen_cc[e] = self.ncc - 1
        self.last_writer = {} if wait_cc else {k: v for k, v in self.last_writer.items() if v.is_cc}
        self.readers = {}
        self.last_compute = {e: None for e in ENGS}

    def add(self, eng, fn, reads=(), writes=(), dma=False, out=False, cc=False):
        op = Op(eng, fn, dma)
        op.is_cc = cc
        op.idx = len(self.ops[eng])
        op.phase = self.phase
        deps = []
        for k in reads:
            w = self.last_writer.get(k)
            if w is not None:
                deps.append((w, "raw"))
        for k in writes:
            w = self.last_writer.get(k)
            if w is not None:
                deps.append((w, "waw"))
            for r in self.readers.get(k, ()):
                deps.append((r, "war"))
        for d, kind in deps:
            if d is op:
                continue
            if d.is_cc:
                if self.seen_cc[eng] >= d.ccnum:
                    continue
                self.seen_cc[eng] = d.ccnum
                op.ccwaits.append(d)
            elif d.is_dma:
                if d.dnum in self.seen_dma[eng]:
                    continue
                self.seen_dma[eng].add(d.dnum)
                op.dwaits.append(d)
            else:
                if d.eng == eng and (eng == "tensor" or kind == "war"):
                    continue
                if self.seen[eng][d.eng] >= d.idx:
                    continue
                self.seen[eng][d.eng] = d.idx
                d.signal = True
                op.waits.append(d)
        if cc:
            op.ccnum = self.ncc
            self.ncc += 1
            self.cc_ops.append(op)
            self.seen_cc[eng] = max(self.seen_cc[eng], op.ccnum - 1)
        if dma:
            op.dnum = self.ndma
            self.ndma += 1
            self.dma_ops.append(op)
            if op.dnum >= NDSEM:
                op.guard = op.dnum - NDSEM
                self.seen_dma[eng].add(op.guard)
            if out:
                self.out_dmas.append(op)
        for k in reads:
            self.readers.setdefault(k, []).append(op)
        for k in writes:
            self.last_writer[k] = op
            self.readers[k] = []
        if not dma and not cc:
            self.last_compute[eng] = op
        self.ops[eng].append(op)
        return op

    def emit(self):
        nc = self.nc
        fin = Op("sync", None, False)
        fin.idx = len(self.ops["sync"])
        fin.dwaits = [d for d in self.out_dmas]
        self.ops["sync"].append(fin)
        fin.phase = self.phase
        for e in ENGS:
            c = {}
            for op in self.ops[e]:
                if op.signal:
                    c[op.phase] = c.get(op.phase, 0) + 1
                    op.sval = c[op.phase]
        import contextlib
        with contextlib.ExitStack() as st:
            need = sorted({(op.phase, e) for e in ENGS for op in self.ops[e] if op.signal})
            esem = {(ph, e): st.enter_context(nc.semaphore("s%d_%s" % (ph, e))) for (ph, e) in need}
            dsem = [st.enter_context(nc.semaphore("d_%d" % i)) for i in range(NDSEM)]
            ccsem = st.enter_context(nc.semaphore("ccs"))
            block = st.enter_context(nc.Block())

            def mk(e):
                def body(eng):
                    self.pidval = eng.partition_id() if (self.use_pid and e in ("sync", "gpsimd")) else None
                    for op in self.ops[e]:
                        for d in op.waits:
                            eng.wait_ge(esem[(d.phase, d.eng)], d.sval)
                        for d in op.dwaits:
                            eng.wait_ge(dsem[d.dnum % NDSEM], 16 * (d.dnum // NDSEM + 1))
                        for d in op.ccwaits:
                            eng.wait_ge(ccsem, d.ccnum + 1)
                        if op.is_cc and op.ccnum > 0:
                            eng.wait_ge(ccsem, op.ccnum)
                        if op.guard is not None:
                            g = op.guard
                            eng.wait_ge(dsem[g % NDSEM], 16 * (g // NDSEM + 1))
                        if op.fn is None:
                            continue
                        ins = op.fn(eng)
                        if op.is_cc:
                            ins.then_inc(ccsem, 1)
                        elif op.is_dma:
                            ins.then_inc(dsem[op.dnum % NDSEM], 16)
                        elif op.signal:
                            ins.then_inc(esem[(op.phase, e)], 1)
                return body

            block.sync(mk("sync"))
            block.scalar(mk("scalar"))
            block.vector(mk("vector"))
            block.gpsimd(mk("gpsimd"))
            block.tensor(mk("tensor"))

    def nops(self):
        return {e: len(v) for e, v in self.ops.items()}

import contextlib
import math

DN_ALPHA = 8 ** 0.25
LN_EPS = 1e-5
TOK = 2048
CH = 512
ARENA_WORDS = 52000
I32 = mybir.dt.int32
PI = math.pi


def S_(x, e):
    return x(e) if callable(x) else x


class KB:
    def __init__(self, arena_words=ARENA_WORDS):
        self.nc = bass.Bass("TRN2", target_bir_lowering=False)
        self.st = contextlib.ExitStack()
        self.P = Prog(self.nc)
        self.prefix = ""
        self.over = {}
        self.dram = {}
        self.cache = {}
        self.arena = self.st.enter_context(self.nc.sbuf_tensor("arena", [128, arena_words], F32))
        self.words = arena_words
        self.aoff = 0
        self.amark = 0
        self.banks = [self.st.enter_context(self.nc.psum_tensor("bank%d" % i, [128, 512], F32)) for i in range(8)]
        self.bank_i = 0
        self.hmode = ("ext", None)
        self.peak = 0

    def _dram(self, name, shape, dt, kind):
        if name in self.over:
            return self.over[name]
        full = name if kind == "Internal" else self.prefix + name
        if full not in self.dram:
            self.dram[full] = self.nc.dram_tensor(full, list(shape), dt, kind=kind).ap()
        return self.dram[full]

    def din(self, name, shape, dt=F32):
        return self._dram(name, shape, dt, "ExternalInput")

    def dout(self, name, shape, dt=F32):
        return self._dram(name, shape, dt, "ExternalOutput")

    def dint(self, name, shape, dt=F32):
        return self._dram(name, shape, dt, "Internal")

    def sb(self, name, shape, dt=F32):
        n = 1
        for s in shape[1:]:
            n *= s
        esz = 2 if dt == BF16 else 4
        words = (n * esz + 3) // 4
        words = (words + 7) // 8 * 8
        assert self.aoff + words <= self.words, ("SBUF arena overflow", name, self.aoff, words)
        v = self.arena[0:shape[0], self.aoff:self.aoff + words]
        self.aoff += words
        self.peak = max(self.peak, self.aoff)
        if dt != F32:
            v = v.bitcast(dt)
        v = v[:, 0:n]
        if len(shape) == 3:
            v = v.rearrange("p (a b) -> p a b", a=shape[1])
        elif len(shape) == 4:
            v = v.rearrange("p (a b c) -> p a b c", a=shape[1], b=shape[2])
        return v

    def ps(self, name, shape, dt=F32):
        b = self.banks[self.bank_i]
        self.bank_i += 1
        return b

    def mark(self):
        self.amark = self.aoff

    def phase(self, wait_cc=True):
        self.P.barrier(wait_cc)
        self.aoff = self.amark
        self.bank_i = 0

    def finish(self):
        self.P.emit()
        self.st.close()
        return self.nc


def make_ident(kb, name="ident", dt=F32):
    if name in kb.cache:
        return kb.cache[name]
    P = kb.P
    ident = kb.sb(name, [128, 128], dt)
    P.add("gpsimd", lambda e: e.memset(ident[:], 0.0), writes=[name])
    P.add("gpsimd", lambda e: e.affine_select(out=ident[:], in_=ident[:], pattern=[[-1, 128]], compare_op=ALU.not_equal,
                                              fill=1.0, base=0, channel_multiplier=1), reads=[name], writes=[name])
    kb.cache[name] = ident
    return ident


def load_hT(kb, hTb, hb, c, hT):
    P = kb.P
    mode, src = kb.hmode
    if mode == "ext":
        csl = slice(c * 512, (c + 1) * 512)
        P.add("gpsimd", lambda e: e.dma_start(out=hTb[:, hb], in_=hT.rearrange("(m p) t -> p m t", p=128)[:, :, csl]),
              writes=[("hTb", hb)], dma=True)
    else:
        q, lc = c // 4, c % 4
        t2, tl = lc // 2, (lc % 2) * 512
        P.add("sync", lambda e: e.dma_start(out=hTb[:, hb], in_=src[t2, q * 1024:(q + 1) * 1024, tl:tl + 512].rearrange("(m p) t -> p m t", p=128)),
              reads=["hg"], writes=[("hTb", hb)], dma=True)

def ln_feature_major(kb, pfx, r, rk, g_col, b_col, gk, onesf, pm, pq, tmp, outs, n=CH):
    P = kb.P
    sq, mean, rstd, t = tmp["sq"], tmp["mean"], tmp["rstd"], tmp["t"]
    for m in range(8):
        P.add("tensor", lambda e, m=m: e.matmul(pm[:, :n], lhsT=onesf[:], rhs=r[:, m, :n], start=(m == 0), stop=(m == 7)),
              reads=[rk(m), "onesf"], writes=["pm"])
    for m in range(8):
        P.add("scalar", lambda e, m=m: e.activation(out=sq[:, m % 2, :n], in_=r[:, m, :n], func=AF.Square),
              reads=[rk(m)], writes=[(pfx + "sq", m % 2)])
        P.add("tensor", lambda e, m=m: e.matmul(pq[:, :n], lhsT=onesf[:], rhs=sq[:, m % 2, :n], start=(m == 0), stop=(m == 7)),
              reads=[(pfx + "sq", m % 2), "onesf"], writes=["pq"])
    P.add("scalar", lambda e: e.activation(out=mean[:, :n], in_=pm[:, :n], func=AF.Identity), reads=["pm"], writes=[pfx + "mean"])
    P.add("scalar", lambda e: e.activation(out=rstd[:, :n], in_=pm[:, :n], func=AF.Square), reads=["pm"], writes=[pfx + "rstd"])
    P.add("vector", lambda e: e.tensor_tensor(out=rstd[:, :n], in0=pq[:, :n], in1=rstd[:, :n], op=ALU.subtract),
          reads=["pq", pfx + "rstd"], writes=[pfx + "rstd"])
    P.add("scalar", lambda e: e.activation(out=rstd[:, :n], in_=rstd[:, :n], func=AF.Sqrt, bias=tmp["eps"][:], scale=1.0),
          reads=[pfx + "rstd", "eps"], writes=[pfx + "rstd"])
    P.add("vector", lambda e: e.reciprocal(out=rstd[:, :n], in_=rstd[:, :n]), reads=[pfx + "rstd"], writes=[pfx + "rstd"])
    for m in range(8):
        P.add("vector", lambda e, m=m: e.tensor_tensor(out=t[:, m % 2, :n], in0=r[:, m, :n], in1=mean[:, :n], op=ALU.subtract),
              reads=[rk(m), pfx + "mean"], writes=[(pfx + "t", m % 2)])
        P.add("vector", lambda e, m=m: e.tensor_tensor(out=t[:, m % 2, :n], in0=t[:, m % 2, :n], in1=rstd[:, :n], op=ALU.mult),
              reads=[(pfx + "t", m % 2), pfx + "rstd"], writes=[(pfx + "t", m % 2)])
        for oent in outs:
            (ot, okf, oeng) = oent[:3]
            g_c, b_c = (oent[3], oent[4]) if len(oent) > 3 else (g_col, b_col)
            if oeng == "scalar":
                P.add("scalar", lambda e, m=m, ot=ot, g_c=g_c, b_c=b_c: e.activation(out=ot[:, m, :n], in_=t[:, m % 2, :n], func=AF.Identity,
                                                                    bias=b_c(m), scale=g_c(m)),
                      reads=[(pfx + "t", m % 2), gk], writes=[okf(m)])
            else:
                P.add(oeng, lambda e, m=m, ot=ot, g_c=g_c, b_c=b_c: e.tensor_scalar(out=ot[:, m, :n], in0=t[:, m % 2, :n], scalar1=g_c(m), scalar2=b_c(m),
                                                                   op0=ALU.mult, op1=ALU.add),
                      reads=[(pfx + "t", m % 2), gk], writes=[okf(m)])


def emit_post(kb, even, tok=TOK):
    nc, P = kb.nc, kb.P
    nch = tok // CH
    hT = kb.din("hT", [1024, tok])
    if even:
        attT = kb.din("attT", [512, tok])
        ysT = kb.din("ysT", [512, tok])
        w_glu = kb.din("w_glu", [512, 512])
        b_glu = kb.din("b_glu", [512])
    else:
        mixTd = kb.din("mixT", [1024, tok])
    w_out = kb.din("w_out", [1024, 1024])
    lnp = kb.din("lnp", [4, 1024])
    wr = kb.din("wr", [1024, 20])
    br = kb.din("br", [20])
    wg = kb.din("wg", [16, 1024, 256])
    wu = kb.din("wu", [16, 1024, 256])
    wd = kb.din("wd", [16, 256, 1024])
    houtT = kb.dout("houtT", [1024, tok])
    hbT = kb.over.get("hbT")
    out_final = "houtT" not in kb.over
    xk = list(kb.over.get("xkeys", []))
    ident = make_ident(kb)

    x1b_all = kb.sb("x1b_all", [128, nch, 8, CH], BF16)
    acc_all = kb.sb("acc_all", [128, nch, 8, CH])
    combT_all = kb.sb("combT_all", [16, nch * CH])
    lnc = kb.sb("lnc", [128, 4, 8])
    lnca_t = kb.sb("lnca", [128, 2, 8])
    wr_sb = kb.sb("wr_sb", [128, 8, 20])
    br_sb = kb.sb("br_sb", [128, 20])
    onesf = kb.sb("onesf", [128, 128])
    eps = kb.sb("eps", [128, 1])
    sel = kb.sb("sel", [16, 16, 128])
    sub_mark = kb.aoff
    po = [kb.ps("po0", [128, CH]), kb.ps("po1", [128, CH])]
    pm = kb.ps("pm", [128, CH])
    pq = kb.ps("pq", [128, CH])
    pg = [kb.ps("pg0", [128, CH]), kb.ps("pg1", [128, CH])]
    pu = [kb.ps("pu0", [128, CH]), kb.ps("pu1", [128, CH])]

    def subphase():
        P.barrier()
        kb.aoff = sub_mark

    wout_sb = kb.sb("wout_sb", [128, 8, 1024], BF16)
    mixT = kb.sb("mixT_sb", [128, 8, CH], BF16)
    r = kb.sb("r", [128, 8, CH])
    tmp = dict(sq=kb.sb("sq", [128, 2, CH]), mean=kb.sb("mean", [128, CH]), rstd=kb.sb("rstd", [128, CH]),
               t=kb.sb("lt", [128, 2, CH]), eps=eps)
    lgb = kb.sb("lgb", [128, 4, 20])
    gm = kb.sb("gm", [128, 4]); gv = kb.sb("gv", [128, 4]); dd = kb.sb("dd", [128, 4]); w1 = kb.sb("w1", [128, 4]); w2 = kb.sb("w2", [128, 4])
    gk = kb.sb("gk", [128, 4, 4]); gx = kb.sb("gx", [128, 4, 4])
    em = kb.sb("em", [128, 4, 16]); m1k = kb.sb("m1k", [128, 4, 16]); m2k = kb.sb("m2k", [128, 4, 16]); combb = kb.sb("combb", [128, 4, 16])
    top = kb.sb("top", [128, 4, 8])
    lnrow = kb.sb("lnrow", [32, 128])
    if even:
        ys = kb.sb("ys", [128, 4, CH])
        yt = kb.sb("yt", [128, 2, CH])
        ygb = kb.sb("ygb", [128, 4, CH], BF16)
        wglu_sb = kb.sb("wglu_sb", [128, 4, 512], BF16)
        bglu_sb = kb.sb("bglu_sb", [128, 4])
        bgrow = kb.sb("bgrow", [4, 128])

    P.add("gpsimd", lambda e: e.dma_start(out=wout_sb[:], in_=w_out.rearrange("(k p) n -> p k n", p=128)), writes=["wout"], dma=True)
    P.add("sync", lambda e: e.dma_start(out=lnrow[:], in_=lnp.rearrange("i (m p) -> (i m) p", p=128)), writes=["lnrow"], dma=True)
    P.add("tensor", lambda e: e.transpose(out=pq[:, 0:32], in_=lnrow[:], identity=ident[0:32, 0:32]), reads=["lnrow", "ident"], writes=["pq"])
    P.add("vector", lambda e: e.tensor_copy(out=lnc[:].rearrange("p i m -> p (i m)"), in_=pq[:, 0:32]), reads=["pq"], writes=["lnc"])
    P.add("sync", lambda e: e.dma_start(out=wr_sb[:], in_=wr.rearrange("(k p) n -> p k n", p=128)), writes=["wr"], dma=True)
    P.add("sync", lambda e: e.dma_start(out=br_sb[:], in_=br.rearrange("(o n) -> o n", o=1).to_broadcast([128, 20])), writes=["br"], dma=True)
    P.add("vector", lambda e: e.memset(onesf[:], 1.0 / 1024.0), writes=["onesf"])
    P.add("vector", lambda e: e.memset(eps[:], LN_EPS), writes=["eps"])
    P.add("gpsimd", lambda e: e.memset(sel[:], 1.0), writes=["sel"])
    P.add("gpsimd", lambda e: e.affine_select(out=sel[:], in_=sel[:], pattern=[[-1, 16], [0, 128]], compare_op=ALU.is_equal,
                                              fill=0.0, base=0, channel_multiplier=1), reads=["sel"], writes=["sel"])
    if even:
        P.add("gpsimd", lambda e: e.dma_start(out=wglu_sb[:], in_=w_glu.rearrange("(k p) n -> p k n", p=128)), writes=["wglu"], dma=True)
        P.add("sync", lambda e: e.dma_start(out=bgrow[:], in_=b_glu.rearrange("(n p) -> n p", p=128)), writes=["bgrow"], dma=True)
        P.add("tensor", lambda e: e.transpose(out=pq[:, 0:4], in_=bgrow[:], identity=ident[0:4, 0:4]), reads=["bgrow", "ident"], writes=["pq"])
        P.add("vector", lambda e: e.tensor_copy(out=bglu_sb[:], in_=pq[:, 0:4]), reads=["pq"], writes=["bglu"])

    gcol = lambda i: (lambda m: lnc[:, i, m:m + 1])
    lnca = kb.sb("lnca", [128, 2, 8]) if False else lnca_t
    P.add("vector", lambda e: e.tensor_scalar(out=lnca[:], in0=lnc[:, 0:2, :], scalar1=DN_ALPHA, scalar2=None, op0=ALU.mult), reads=["lnc"], writes=["lnc"])
    gcola = lambda i: (lambda m: lnca[:, i, m:m + 1])

    def chunkA(c, part):
        csl = slice(c * CH, (c + 1) * CH)
        accv = acc_all[:, c]
        x1bv = x1b_all[:, c]
        if part == 1:
            return chunkA_router(c, accv)
        P.add("sync", lambda e, csl=csl, accv=accv: e.dma_start(out=accv, in_=S_(hT, e).rearrange("(m p) t -> p m t", p=128)[:, :, csl]),
              writes=[("acc", c, m) for m in range(8)], dma=True)
        if even:
            P.add("gpsimd", lambda e, csl=csl: e.dma_start(out=mixT[:, 0:4, :], in_=attT.rearrange("(m p) t -> p m t", p=128)[:, :, csl]),
                  reads=xk, writes=[("mixT", m) for m in range(4)], dma=True)
            P.add("sync", lambda e, csl=csl: e.dma_start(out=ys[:], in_=ysT.rearrange("(m p) t -> p m t", p=128)[:, :, csl]),
                  reads=xk, writes=[("ys", m) for m in range(4)], dma=True)
            for m in range(4):
                b = m % 2
                P.add("scalar", lambda e, m=m, b=b: e.activation(out=yt[:, b, :], in_=ys[:, m, :], func=AF.Square),
                      reads=[("ys", m)], writes=[("yt", b)])
                P.add("vector", lambda e, b=b: e.tensor_scalar(out=yt[:, b, :], in0=yt[:, b, :], scalar1=0.044715, scalar2=1.0,
                                                               op0=ALU.mult, op1=ALU.add), reads=[("yt", b)], writes=[("yt", b)])
                P.add("vector", lambda e, m=m, b=b: e.tensor_tensor(out=yt[:, b, :], in0=yt[:, b, :], in1=ys[:, m, :], op=ALU.mult),
                      reads=[("yt", b), ("ys", m)], writes=[("yt", b)])
                P.add("scalar", lambda e, b=b: e.activation(out=yt[:, b, :], in_=yt[:, b, :], func=AF.Sigmoid, scale=1.5957691216),
                      reads=[("yt", b)], writes=[("yt", b)])
                P.add("vector", lambda e, m=m, b=b: e.tensor_tensor(out=ygb[:, m, :], in0=yt[:, b, :], in1=ys[:, m, :], op=ALU.mult),
                      reads=[("yt", b), ("ys", m)], writes=[("ygb", m)])
            for n in range(4):
                pb = po[n % 2]
                for k in range(4):
                    P.add("tensor", lambda e, n=n, k=k, pb=pb: e.matmul(pb[:], lhsT=wglu_sb[:, k, n * 128:(n + 1) * 128], rhs=ygb[:, k, :],
                                                                         start=(k == 0), stop=(k == 3)),
                          reads=[("ygb", k), "wglu"], writes=["po%d" % (n % 2)])
                b = n % 2
                P.add("scalar", lambda e, n=n, pb=pb, b=b: e.activation(out=yt[:, b, :], in_=pb[:], func=AF.Sigmoid, bias=bglu_sb[:, n:n + 1], scale=1.0),
                      reads=["po%d" % (n % 2), "bglu"], writes=[("yt", b)])
                P.add("vector", lambda e, n=n, b=b: e.tensor_tensor(out=mixT[:, 4 + n, :], in0=yt[:, b, :], in1=ygb[:, n, :], op=ALU.mult),
                      reads=[("yt", b), ("ygb", n)], writes=[("mixT", 4 + n)])
        else:
            P.add("gpsimd", lambda e, csl=csl: e.dma_start(out=mixT[:], in_=mixTd.rearrange("(m p) t -> p m t", p=128)[:, :, csl]),
                  reads=xk, writes=[("mixT", m) for m in range(8)], dma=True)
        for m in range(8):
            pb = [po[0], po[1], pg[0], pg[1], pu[0], pu[1]][m % 6]
            pk = ["po0", "po1", "pg0", "pg1", "pu0", "pu1"][m % 6]
            for k in range(8):
                P.add("tensor", lambda e, m=m, k=k, pb=pb: e.matmul(pb[:], lhsT=wout_sb[:, k, m * 128:(m + 1) * 128], rhs=mixT[:, k, :],
                                                                     start=(k == 0), stop=(k == 7)),
                      reads=[("mixT", k), "wout"], writes=[pk])
            P.add("vector", lambda e, m=m, pb=pb, accv=accv: e.scalar_tensor_tensor(out=r[:, m, :], in0=accv[:, m, :], scalar=DN_ALPHA, in1=pb[:],
                                                                                    op0=ALU.mult, op1=ALU.add),
                  reads=[("acc", c, m), pk], writes=[("r", m)])
        ln_feature_major(kb, "l1", r, lambda m: ("r", m), gcol(0), gcol(1), "lnc", onesf, pm, pq, tmp,
                         [(accv, lambda m, c=c: ("acc", c, m), "scalar", gcola(0), gcola(1)), (x1bv, lambda m, c=c: ("x1b", c, m), "vector")])

    def chunkA_router(c, accv):
        RK = "rt"
        for tt in range(4):
            tsl = slice(tt * 128, (tt + 1) * 128)
            for m in range(8):
                P.add("tensor", lambda e, m=m, tsl=tsl, tt=tt: e.matmul(pm[:, tt * 20:(tt + 1) * 20], lhsT=accv[:, m, tsl], rhs=wr_sb[:, m, :],
                                                                         start=(m == 0), stop=(m == 7)),
                      reads=[("acc", c, m), "wr"], writes=["pm"])
        B3 = lambda ap, n: ap.to_broadcast([128, 4, n])
        P.add("vector", lambda e: e.scalar_tensor_tensor(out=lgb[:], in0=pm[:, 0:80].rearrange("p (t n) -> p t n", t=4), scalar=1.0 / DN_ALPHA,
                                                         in1=br_sb[:].rearrange("p (o n) -> p o n", o=1).to_broadcast([128, 4, 20]), op0=ALU.mult, op1=ALU.add),
              reads=["pm", "br"], writes=[RK])
        P.add("vector", lambda e: e.tensor_reduce(out=gm[:], in_=lgb[:, :, 0:4], axis=AX.X, op=ALU.max), reads=[RK], writes=[RK])
        P.add("vector", lambda e: e.tensor_tensor(out=gk[:], in0=lgb[:, :, 0:4], in1=B3(gm[:].rearrange("p (t o) -> p t o", o=1), 4), op=ALU.is_equal),
              reads=[RK], writes=[RK])
        P.add("vector", lambda e: e.tensor_tensor(out=gx[:], in0=lgb[:, :, 0:4], in1=B3(gm[:].rearrange("p (t o) -> p t o", o=1), 4), op=ALU.subtract),
              reads=[RK], writes=[RK])
        P.add("scalar", lambda e: e.activation(out=gx[:], in_=gx[:], func=AF.Exp), reads=[RK], writes=[RK])
        P.add("vector", lambda e: e.tensor_reduce(out=gv[:], in_=gx[:], axis=AX.X, op=ALU.add), reads=[RK], writes=[RK])
        P.add("vector", lambda e: e.reciprocal(out=gv[:], in_=gv[:]), reads=[RK], writes=[RK])
        P.add("vector", lambda e: e.tensor_scalar(out=gk[:], in0=gk[:], scalar1=-1.0, scalar2=1e30, op0=ALU.add, op1=ALU.mult), reads=[RK], writes=[RK])
        P.add("vector", lambda e: e.tensor_tensor(out=em[:].rearrange("p t (g x) -> p t g x", g=4),
                                                  in0=lgb[:, :, 4:20].rearrange("p t (g x) -> p t g x", g=4),
                                                  in1=gk[:].rearrange("p t (g o) -> p t g o", o=1).to_broadcast([128, 4, 4, 4]), op=ALU.add),
              reads=[RK], writes=[RK])
        for tt in range(4):
            P.add("vector", lambda e, tt=tt: e.max(out=top[:, tt, :], in_=em[:, tt, :]), reads=[RK], writes=[RK])
        P.add("vector", lambda e: e.tensor_tensor(out=m1k[:], in0=em[:], in1=B3(top[:, :, 0:1], 16), op=ALU.is_equal), reads=[RK], writes=[RK])
        P.add("vector", lambda e: e.tensor_tensor(out=m2k[:], in0=em[:], in1=B3(top[:, :, 1:2], 16), op=ALU.is_equal), reads=[RK], writes=[RK])
        P.add("vector", lambda e: e.tensor_tensor(out=dd[:].rearrange("p (t o) -> p t o", o=1), in0=top[:, :, 1:2], in1=top[:, :, 0:1], op=ALU.subtract),
              reads=[RK], writes=[RK])
        P.add("scalar", lambda e: e.activation(out=dd[:], in_=dd[:], func=AF.Exp), reads=[RK], writes=[RK])
        P.add("vector", lambda e: e.tensor_scalar(out=w1[:], in0=dd[:], scalar1=1.0, scalar2=None, op0=ALU.add), reads=[RK], writes=[RK])
        P.add("vector", lambda e: e.reciprocal(out=w1[:], in_=w1[:]), reads=[RK], writes=[RK])
        P.add("vector", lambda e: e.tensor_tensor(out=w1[:], in0=w1[:], in1=gv[:], op=ALU.mult), reads=[RK], writes=[RK])
        P.add("vector", lambda e: e.tensor_tensor(out=w2[:], in0=w1[:], in1=dd[:], op=ALU.mult), reads=[RK], writes=[RK])
        P.add("vector", lambda e: e.tensor_tensor(out=m1k[:], in0=m1k[:], in1=B3(w1[:].rearrange("p (t o) -> p t o", o=1), 16), op=ALU.mult),
              reads=[RK], writes=[RK])
        P.add("vector", lambda e: e.tensor_tensor(out=m2k[:], in0=m2k[:], in1=B3(w2[:].rearrange("p (t o) -> p t o", o=1), 16), op=ALU.mult),
              reads=[RK], writes=[RK])
        P.add("vector", lambda e: e.tensor_tensor(out=combb[:], in0=m1k[:], in1=m2k[:], op=ALU.add), reads=[RK], writes=["combb"])
        for tt in range(4):
            P.add("tensor", lambda e, tt=tt: e.transpose(out=pq[0:16, tt * 128:(tt + 1) * 128], in_=combb[:, tt, :], identity=ident[:]),
                  reads=["combb", "ident"], writes=["pq"])
        P.add("scalar", lambda e: e.activation(out=combT_all[:, c * 512:(c + 1) * 512], in_=pq[0:16, 0:512], func=AF.Identity),
              reads=["pq"], writes=[("combT", c, i) for i in range(4)])

    chunkA(0, 0)
    for c in range(nch):
        if c + 1 < nch:
            chunkA(c + 1, 0)
        chunkA(c, 1)

    comb_d = kb.dint("comb_d", [16, nch * CH], F32)
    P.add("sync", lambda e: e.dma_start(out=comb_d[:, :], in_=combT_all[:]), reads=[("combT", c, i) for c in range(nch) for i in range(4)], dma=True)
    subphase()
    NWB = 4
    wgs = kb.sb("wgs", [128, NWB, 8, 256], BF16)
    wus = kb.sb("wus", [128, NWB, 8, 256], BF16)
    wds = kb.sb("wds", [128, NWB, 2, 1024], BF16)
    cbs = kb.sb("cbs", [128, 4, CH])
    sgs = kb.sb("sgs", [128, 2, CH])
    tts = kb.sb("tts", [128, 2, CH])
    hid2 = kb.sb("hid2", [128, 2, 2, CH], BF16)
    units = []
    for ex in range(16):
        wb = ex % NWB
        for c in range(nch):
            un = ex * nch + c
            hbuf = un % 2

            cbuf = un % 4

            def fGU(ex=ex, wb=wb, c=c, hbuf=hbuf, cbuf=cbuf):
                csl = slice(c * CH, (c + 1) * CH)
                if c == 0:
                    P.add("gpsimd", lambda e: e.dma_start(out=wgs[:, wb], in_=wg[ex].rearrange("(k p) n -> p k n", p=128)), writes=[("wgs", wb)], dma=True)
                    P.add("gpsimd", lambda e: e.dma_start(out=wus[:, wb], in_=wu[ex].rearrange("(k p) n -> p k n", p=128)), writes=[("wus", wb)], dma=True)
                    P.add("gpsimd", lambda e: e.dma_start(out=wds[:, wb], in_=wd[ex].rearrange("(fh p) d -> p fh d", p=128)), writes=[("wds", wb)], dma=True)
                P.add("sync", lambda e: e.dma_start(out=cbs[:, cbuf, :], in_=comb_d[ex:ex + 1, csl].to_broadcast([128, CH])), writes=[("cbs", cbuf)], dma=True)
                for fh in range(2):
                    pgb, pub = pg[fh], pu[fh]
                    for k in range(8):
                        P.add("tensor", lambda e, k=k, fh=fh, pgb=pgb: e.matmul(pgb[:], lhsT=wgs[:, wb, k, fh * 128:(fh + 1) * 128], rhs=x1b_all[:, c, k, :],
                                                                                 start=(k == 0), stop=(k == 7)),
                              reads=[("x1b", c, k), ("wgs", wb)], writes=["pg%d" % fh])
                    for k in range(8):
                        P.add("tensor", lambda e, k=k, fh=fh, pub=pub: e.matmul(pub[:], lhsT=wus[:, wb, k, fh * 128:(fh + 1) * 128], rhs=x1b_all[:, c, k, :],
                                                                                 start=(k == 0), stop=(k == 7)),
                              reads=[("x1b", c, k), ("wus", wb)], writes=["pu%d" % fh])
                    P.add("scalar", lambda e, fh=fh, pgb=pgb: e.activation(out=sgs[:, fh, :], in_=pgb[:], func=AF.Silu), reads=["pg%d" % fh], writes=[("sgs", fh)])
                    P.add("vector", lambda e, fh=fh, pub=pub: e.tensor_tensor(out=tts[:, fh, :], in0=sgs[:, fh, :], in1=pub[:], op=ALU.mult),
                          reads=[("sgs", fh), "pu%d" % fh], writes=[("tts", fh)])
                    P.add("gpsimd", lambda e, fh=fh: e.tensor_tensor(out=hid2[:, hbuf, fh, :], in0=tts[:, fh, :], in1=cbs[:, cbuf, :], op=ALU.mult),
                          reads=[("tts", fh), ("cbs", cbuf)], writes=[("hid2", hbuf, fh)])

            def fD(ex=ex, wb=wb, c=c, hbuf=hbuf):
                for m in range(8):
                    pb = [po[0], po[1], pm, pq][m % 4]
                    pk = ["po0", "po1", "pm", "pq"][m % 4]
                    for fh in range(2):
                        P.add("tensor", lambda e, m=m, fh=fh, pb=pb: e.matmul(pb[:], lhsT=wds[:, wb, fh, m * 128:(m + 1) * 128], rhs=hid2[:, hbuf, fh, :],
                                                                               start=(fh == 0), stop=(fh == 1)),
                              reads=[("hid2", hbuf, fh), ("wds", wb)], writes=[pk])
                    P.add("vector", lambda e, m=m, pb=pb: e.tensor_tensor(out=acc_all[:, c, m, :], in0=pb[:], in1=acc_all[:, c, m, :], op=ALU.add),
                          reads=[pk, ("acc", c, m)], writes=[("acc", c, m)])
            units.append((fGU, fD))
    for n in range(len(units)):
        if n == 0:
            units[0][0]()
        if n + 1 < len(units):
            units[n + 1][0]()
        units[n][1]()

    subphase()
    tmp = dict(sq=kb.sb("sq", [128, 2, CH]), mean=kb.sb("mean", [128, CH]), rstd=kb.sb("rstd", [128, CH]),
               t=kb.sb("lt", [128, 2, CH]), eps=eps)
    hTs = kb.sb("hTs", [128, 2, 8, CH])
    hbs = kb.sb("hbs", [128, 2, 8, CH], BF16)
    for c in range(nch):
        csl = slice(c * CH, (c + 1) * CH)
        ob = c % 2
        outs2 = [(hTs[:, ob], lambda m, ob=ob: ("hTs", ob, m), "scalar")]
        if hbT is not None:
            outs2.append((hbs[:, ob], lambda m, ob=ob: ("hbs", ob, m), "vector"))
        ln_feature_major(kb, "l2", acc_all[:, c], lambda m, c=c: ("acc", c, m), gcol(2), gcol(3), "lnc", onesf, pm, pq, tmp, outs2)
        P.add("sync", lambda e, csl=csl, ob=ob: e.dma_start(out=S_(houtT, e).rearrange("(m p) t -> p m t", p=128)[:, :, csl], in_=hTs[:, ob]),
              reads=[("hTs", ob, m) for m in range(8)], dma=True, out=out_final)
        if hbT is not None:
            t2, tl2 = c // 2, (c % 2) * CH
            P.add("sync", lambda e, ob=ob, t2=t2, tl2=tl2: e.dma_start(out=hbT[t2].rearrange("(m p) t -> p m t", p=128)[:, :, tl2:tl2 + CH], in_=hbs[:, ob]),
                  reads=[("hbs", ob, m) for m in range(8)], writes=[("hbd", c)], dma=True)
            if c % 2 == 1 and "hb_cb" in kb.over:
                kb.over["hb_cb"](t2, [("hbd", c - 1), ("hbd", c)])


def build_post(even, tok=TOK):
    kb = KB()
    make_ident(kb)
    kb.mark()
    emit_post(kb, even, tok)
    print("post ops", kb.P.nops(), "peak words", kb.peak)
    return kb.finish()

def sincos(P, x, sin_o, cos_o, t1, t2, ti, rd, wr_s, wr_c, K):
    P.add("vector", lambda e: e.tensor_scalar(out=t1, in0=x, scalar1=1.0 / (2 * PI), scalar2=None, op0=ALU.mult), reads=list(rd) + [K], writes=[K])
    P.add("vector", lambda e: e.tensor_copy(out=ti, in_=t1), reads=[K], writes=[K])
    P.add("vector", lambda e: e.tensor_copy(out=t1, in_=ti), reads=[K], writes=[K])
    P.add("vector", lambda e: e.scalar_tensor_tensor(out=t1, in0=t1, scalar=-2 * PI, in1=x, op0=ALU.mult, op1=ALU.add), reads=list(rd) + [K], writes=[K])
    P.add("scalar", lambda e: e.activation(out=t2, in_=t1, func=AF.Sin, scale=0.25), reads=[K], writes=[K])
    P.add("scalar", lambda e: e.activation(out=t1, in_=t1, func=AF.Sin, scale=0.5), reads=[K], writes=[K])
    P.add("vector", lambda e: e.tensor_tensor(out=t2, in0=t2, in1=t2, op=ALU.mult), reads=[K], writes=[K])
    P.add("vector", lambda e: e.tensor_scalar(out=t2, in0=t2, scalar1=-2.0, scalar2=1.0, op0=ALU.mult, op1=ALU.add), reads=[K], writes=[K])
    P.add("vector", lambda e: e.scalar_tensor_tensor(out=sin_o, in0=t1, scalar=2.0, in1=t2, op0=ALU.mult, op1=ALU.mult), reads=[K], writes=list(wr_s) + [K])
    P.add("vector", lambda e: e.tensor_tensor(out=t1, in0=t1, in1=t1, op=ALU.mult), reads=[K], writes=[K])
    P.add("vector", lambda e: e.tensor_scalar(out=cos_o, in0=t1, scalar1=-2.0, scalar2=1.0, op0=ALU.mult, op1=ALU.add), reads=[K], writes=list(wr_c) + [K])


def emit_even(kb, S=4096, mode='fox'):
    FOX = (mode == 'fox'); S5 = not FOX
    nc, P = kb.nc, kb.P
    fm = ('attT_d' in kb.over) if FOX else ('ysT_d' in kb.over)
    HS = S // 2; NH = (S // 512) // 2
    NCk = S // 512
    NT = S // 128
    hT = kb.din("hT", [1024, S])
    if FOX:
        wq = kb.din("wq", [1024, 256]); wk = kb.din("wk", [1024, 256]); wv = kb.din("wv", [1024, 256])
        wf = kb.din("wf", [1024, 4])
        fbias = kb.din("fbias", [4, 1])
    else:
        wu = kb.din("wu", [1024, 256])
        are = kb.din("are", [128, 8]); aim = kb.din("aim", [128, 8]); ldt = kb.din("ldt", [128, 8])
        bre = kb.din("bre", [128, 8, 16]); bim = kb.din("bim", [128, 8, 16])
        cre = kb.din("cre", [128, 8, 16]); cim = kb.din("cim", [128, 8, 16])
        dsk = kb.din("dsk", [128, 2])
        jrow_d = kb.din("jrow", [128, 512])
    att = kb.dout("att", [S, 256]) if (FOX and not fm) else None
    ysT = kb.dout("ysT", [256, S]) if (S5 and not fm) else None
    attT_d = kb.over.get("attT_d"); ysT_d = kb.over.get("ysT_d")

    ident = make_ident(kb)
    hTb = kb.sb("hTb", [128, 2, 8, 512], BF16)
    if FOX:
        wq_sb = kb.sb("wq_sb", [128, 8, 256], BF16); wk_sb = kb.sb("wk_sb", [128, 8, 256], BF16)
        wv_sb = kb.sb("wv_sb", [128, 8, 256], BF16)
        wf_sb = kb.sb("wf_sb", [128, 8, 4], BF16)
        fb_sb = kb.sb("fb_sb", [4, 1])
        wl = ((wq_sb, wq, "wq"), (wk_sb, wk, "wk"), (wv_sb, wv, "wv"), (wf_sb, wf, "wf"))
    else:
        wu_sb = kb.sb("wu_sb", [128, 8, 256], BF16)
        wl = ((wu_sb, wu, "wu"),)
    if FOX:
        QA = kb.sb("QA", [65, 4, S], BF16)
        KA = kb.sb("KA", [65, 4, S], BF16)
        V = kb.sb("V", [128, NT, 4, 72], BF16)
        fl = kb.sb("fl", [4, S])
        cc = fl
        ones4 = kb.sb("ones4", [4, 512])
    else:
        uTb = kb.sb("uTb", [128, 2, S], BF16)
    selc = kb.sb("selc", [4, 4, 65])
    negc = kb.sb("negc", [128, NT, 4])
    PT = kb.sb("PT", [128, 3, 512], BF16)
    ost = kb.sb("ost", [128, 2, 256])
    rec = kb.sb("rec", [128, 4])
    otmp = kb.sb("otmp", [128, 4, 65])
    attst = kb.sb("attst", [64, 2, 512], BF16)
    pA = kb.ps("pA", [128, 512]); pB = kb.ps("pB", [128, 512])
    pS = [kb.ps("pS0", [128, 512]), kb.ps("pS1", [128, 512])]
    pO = [kb.ps("pO0", [128, 512]), kb.ps("pO1", [128, 512])]
    pC = kb.ps("pC", [128, 512]); pD = kb.ps("pD", [128, 512])

    for (wsb, wdr, nm) in wl:
        P.add("gpsimd", lambda e, wsb=wsb, wdr=wdr: e.dma_start(out=wsb[:], in_=wdr.rearrange("(k p) n -> p k n", p=128)), writes=[nm], dma=True)
    if FOX:
        P.add("sync", lambda e: e.dma_start(out=fb_sb[:], in_=fbias[:, :]), writes=["fb"], dma=True)
        P.add("vector", lambda e: e.memset(ones4[:], 1.0), writes=["ones4"])
        P.add("vector", lambda e: e.memset(KA[64:65, :, :], 1.0), writes=["KA1"])
        P.add("vector", lambda e: e.memset(V[:, :, :, 64:65], 1.0), writes=["V1"])
    P.add("gpsimd", lambda e: e.memset(selc[:], 1.0), writes=["selc"])
    P.add("gpsimd", lambda e: e.affine_select(out=selc[:], in_=selc[:], pattern=[[-1, 4], [0, 65]], compare_op=ALU.is_equal, fill=0.0, base=0,
                                              channel_multiplier=1), reads=["selc"], writes=["selc"])
    P.add("gpsimd", lambda e: e.affine_select(out=selc[:], in_=selc[:], pattern=[[0, 4], [1, 65]], compare_op=ALU.is_equal, fill=0.0, base=-64,
                                              channel_multiplier=0), reads=["selc"], writes=["selc"])

    for c in range(NCk):
        hb = c % 2
        csl = slice(c * 512, (c + 1) * 512)
        load_hT(kb, hTb, hb, c, hT)
        if FOX:
            for h in range(4):
                for (wsb, wn, dst, pb, pk, sc) in ((wq_sb, "wq", QA, [pA, pS[0]][h % 2], ["pA", "pS0"][h % 2], 0.125),
                                                   (wk_sb, "wk", KA, [pB, pS[1]][h % 2], ["pB", "pS1"][h % 2], 1.0)):
                    for k in range(8):
                        P.add("tensor", lambda e, k=k, h=h, wsb=wsb, pb=pb, hb=hb: e.matmul(pb[0:64, :], lhsT=wsb[:, k, h * 64:(h + 1) * 64], rhs=hTb[:, hb, k, :],
                                                                                           start=(k == 0), stop=(k == 7)),
                              reads=[("hTb", hb), wn], writes=[pk])
                    P.add("scalar", lambda e, h=h, dst=dst, pb=pb, sc=sc, csl=csl: e.activation(out=dst[0:64, h, csl], in_=pb[0:64, :], func=AF.Copy, scale=sc),
                          reads=[pk], writes=[(wn + "o", h, c)])
            for k in range(8):
                P.add("tensor", lambda e, k=k, hb=hb: e.matmul(pD[0:4, :], lhsT=wf_sb[:, k, :], rhs=hTb[:, hb, k, :], start=(k == 0), stop=(k == 7)),
                      reads=[("hTb", hb), "wf"], writes=["pD"])
            P.add("scalar", lambda e, csl=csl: e.activation(out=fl[:, csl], in_=pD[0:4, :], func=AF.Identity, bias=fb_sb[:], scale=1.0),
                  reads=["pD", "fb"], writes=[("fl", c)])
            for t4 in range(4):
                t = c * 4 + t4
                for k in range(8):
                    P.add("tensor", lambda e, k=k, t4=t4, hb=hb: e.matmul(pD[:, 256:512], lhsT=hTb[:, hb, k, t4 * 128:(t4 + 1) * 128], rhs=wv_sb[:, k, :],
                                                                           start=(k == 0), stop=(k == 7)),
                          reads=[("hTb", hb), "wv"], writes=["pD"])
                P.add("vector", lambda e, t=t: e.tensor_copy(out=V[:, t, :, 0:64], in_=pD[:, 256:512].rearrange("p (h d) -> p h d", h=4)),
                      reads=["pD", "V1"], writes=[("V", t)])
        else:
            for ut in range(2):
                for k in range(8):
                    P.add("tensor", lambda e, k=k, ut=ut, hb=hb: e.matmul(pC[:], lhsT=wu_sb[:, k, ut * 128:(ut + 1) * 128], rhs=hTb[:, hb, k, :],
                                                                           start=(k == 0), stop=(k == 7)),
                          reads=[("hTb", hb), "wu"], writes=["pC"])
                P.add("vector", lambda e, ut=ut, csl=csl: e.tensor_copy(out=uTb[:, ut, csl], in_=pC[:]), reads=["pC"], writes=[("uTb", ut, c)])
    STAGE = 9; KK = 65
    if FOX and STAGE >= 2:
        flk = [("fl", c) for c in range(NCk)]
        P.add("scalar", lambda e: e.activation(out=fl[:], in_=fl[:], func=AF.Exp, scale=-1.0), reads=flk, writes=["fl2"])
        P.add("scalar", lambda e: e.activation(out=fl[:], in_=fl[:], func=AF.Ln, bias=1.0, scale=1.0), reads=["fl2"], writes=["fl3"])
        P.add("vector", lambda e: e.tensor_scalar(out=fl[:], in0=fl[:], scalar1=-1.0, scalar2=None, op0=ALU.mult), reads=["fl3"], writes=["fl4"])
        for c in range(NCk):
            csl = slice(c * 512, (c + 1) * 512)
            ini = 0.0 if c == 0 else cc[:, c * 512 - 1:c * 512]
            P.add("vector", lambda e, csl=csl, ini=ini: e.tensor_tensor_scan(out=cc[:, csl], data0=ones4[:], data1=fl[:, csl], initial=ini, op0=ALU.mult, op1=ALU.add),
                  reads=["fl4", "ones4", "cc"], writes=["cc"])
        for c in range(NCk):
            csl = slice(c * 512, (c + 1) * 512)
            for h in range(4):
                P.add("tensor", lambda e, h=h, csl=csl: e.matmul(pA[0:65, :], lhsT=selc[:, h, :], rhs=cc[:, csl], start=True, stop=True),
                      reads=["cc", "selc"], writes=["pA"])
                P.add("scalar", lambda e, h=h, csl=csl: e.activation(out=QA[64:65, h, csl], in_=pA[64:65, :], func=AF.Copy),
                      reads=["pA"], writes=[("QAc", h, c)])
        for t in range(NT):
            P.add("tensor", lambda e, t=t: e.transpose(out=pB[:, t * 4:(t + 1) * 4], in_=cc[:, t * 128:(t + 1) * 128], identity=ident[0:4, 0:4]),
                  reads=["cc", "ident"], writes=["pB"])
        P.add("scalar", lambda e: e.activation(out=negc[:].rearrange("p t h -> p (t h)"), in_=pB[:, 0:NT * 4], func=AF.Copy, scale=-1.0),
              reads=["pB"], writes=["negc"])

        cnt = 0
        tiles = []
        for h in range(4 if STAGE >= 3 else 0):
            for j in range(NCk):
                qk_reads = [("wqo", h, j), ("QAc", h, j)]
                for i in range(4 * j + 4):
                    r = i - 4 * j
                    q0 = 128 * r if r > 0 else 0
                    sb_ = cnt % 3
                    cnt += 1
                    ps = [pS[0], pS[1], pB][sb_]
                    def fS(h=h, i=i, j=j, q0=q0, ps=ps, sb_=sb_, qk_reads=qk_reads):
                        P.add("tensor", lambda e, h=h, i=i, j=j, q0=q0, ps=ps: e.matmul(ps[:, q0:512], lhsT=KA[0:KK, h, i * 128:(i + 1) * 128],
                                                                                        rhs=QA[0:KK, h, j * 512 + q0:(j + 1) * 512], start=True, stop=True),
                              reads=qk_reads + [("wko", h, i // 4), "KA1"], writes=[["pS0", "pS1", "pB"][sb_]])
                    def fR(h=h, i=i, j=j, r=r, q0=q0, ps=ps, sb_=sb_):
                        P.add("scalar", lambda e, h=h, i=i, q0=q0, ps=ps, sb_=sb_: e.activation(out=PT[:, sb_, q0:512], in_=ps[:, q0:512], func=AF.Exp,
                                                                                               bias=negc[:, i, h:h + 1], scale=1.0),
                              reads=[["pS0", "pS1", "pB"][sb_], "negc"], writes=[("PT", sb_)])
                        AL = 9
                        if r >= 0 and AL >= 2:
                            P.add("gpsimd", lambda e, sb_=sb_, q0=q0: e.affine_select(out=PT[:, sb_, q0:q0 + 128], in_=PT[:, sb_, q0:q0 + 128], pattern=[[1, 128]],
                                                                                       compare_op=ALU.is_ge, fill=0.0, base=0, channel_multiplier=-1),
                                  reads=[("PT", sb_)], writes=[("PT", sb_)])
                        for u in range(max(r, 0), 4 if AL >= 3 else 0):
                            po_ = [pO[0], pO[1], pC, pD][u]
                            o0 = 0
                            okey = [('pO', 0), ('pO', 1), 'pC', 'pD'][u]
                            P.add("tensor", lambda e, h=h, i=i, u=u, sb_=sb_, po_=po_, o0=o0, j=j: e.matmul(po_[:, o0:o0 + 65], lhsT=PT[:, sb_, u * 128:(u + 1) * 128],
                                                                                                             rhs=V[:, i, h, 0:65], start=(i == 0), stop=(i == 4 * j + u)),
                                  reads=[("PT", sb_), ("V", i), "V1"], writes=[okey])
                            if i == 4 * j + u and AL >= 4:
                                ob = (j * 4 + u) % 2
                                tt = j * 4 + u
                                P.add("vector", lambda e, po_=po_, o0=o0, u=u: e.tensor_copy(out=otmp[:, u, :], in_=po_[:, o0:o0 + 65]),
                                      reads=[okey], writes=[("otmp", u)])
                                P.add("vector", lambda e, u=u: e.reciprocal(out=rec[:, u:u + 1], in_=otmp[:, u, 64:65]),
                                      reads=[("otmp", u)], writes=[("rec", u)])
                                P.add("vector", lambda e, u=u, ob=ob, h=h: e.tensor_scalar(out=ost[:, ob, h * 64:(h + 1) * 64], in0=otmp[:, u, 0:64],
                                                                                         scalar1=rec[:, u:u + 1], scalar2=None, op0=ALU.mult),
                                      reads=[("otmp", u), ("rec", u)], writes=[("ost", ob, h)])
                                if not fm:
                                    P.add("sync", lambda e, ob=ob, h=h, tt=tt: e.dma_start(out=att[tt * 128:(tt + 1) * 128, h * 64:(h + 1) * 64], in_=ost[:, ob, h * 64:(h + 1) * 64]),
                                          reads=[("ost", ob, h)], dma=True, out=True)
                                else:
                                    def fin(h=h, j=j, u=u, ob=ob):
                                        stb = (h * NCk + j) % 2
                                        P.add("tensor", lambda e, ob=ob, h=h, u=u: e.transpose(out=pA[0:64, u * 128:(u + 1) * 128], in_=ost[:, ob, h * 64:(h + 1) * 64], identity=ident[:]),
                                              reads=[("ost", ob, h), "ident"], writes=["pA"])
                                        P.add("scalar", lambda e, u=u, stb=stb: e.activation(out=attst[:, stb, u * 128:(u + 1) * 128], in_=pA[0:64, u * 128:(u + 1) * 128], func=AF.Copy),
                                              reads=["pA"], writes=[("attst", stb)])
                                        if u == 3:
                                            half, tl = j // NH, (j % NH) * 512
                                            P.add("sync", lambda e, stb=stb, h=h, half=half, tl=tl: e.dma_start(out=attT_d[half, h * 64:(h + 1) * 64, tl:tl + 512], in_=attst[:, stb, :]),
                                                  reads=[("attst", stb)], dma=True)
                                    deferred.append(fin)
                    tiles.append((fS, fR))
        deferred = []
        for n in range(len(tiles)):
            if n == 0:
                tiles[0][0]()
                if len(tiles) > 1:
                    tiles[1][0]()
            if n + 2 < len(tiles):
                tiles[n + 2][0]()
            pend = list(deferred)
            del deferred[:]
            tiles[n][1]()
            for f in pend:
                f()
        for f in deferred:
            f()

    if S5:
        prm = kb.sb("prm", [128, 24, 8])
        prmi = kb.sb("prmi", [128, 1, 8], I32)
        angi = kb.sb("angi", [128, 512], I32)
        bb = kb.sb("bb", [128, 2, 8, 16])
        cs = kb.sb("cs", [128, 2, 8, 16])
        btmp = kb.sb("btmp", [128, 2, 16])
        Xp = kb.sb("Xp", [128, 2, 128])
        Blhs = kb.sb("Blhs", [128, 8, 2, 128], BF16)
        Cl = kb.sb("Cl", [128, 8, 2, 128], BF16)
        Dl = kb.sb("Dl", [128, 2, 128], BF16)
        dsk_sb = kb.sb("dsk_sb", [128, 2])
        jrow = kb.sb("jrow_sb", [128, 512])
        cosT = kb.sb("cosT", [128, 8, 512]); sinT = kb.sb("sinT", [128, 8, 512]); rT = kb.sb("rT", [128, 8, 512])
        ang = kb.sb("ang", [128, 512])
        wsets = [[kb.sb("w%d_%d" % (i, u), [128, 512]) for i in range(6)] for u in range(3)]
        w = wsets[0]
        sb2s = [[kb.sb("sre%d" % u, [128, 512]), kb.sb("sim%d" % u, [128, 512])] for u in range(3)]
        wb4s = [[kb.sb("wb4_%d_%d" % (u, q), [128, 512], BF16) for q in range(4)] for u in range(3)]
        Cln = kb.sb("Cln", [128, 8, 2, 128], BF16)
        init = kb.sb("init", [128, 8, 2])
        yst = kb.sb("yst", [128, 2, 512])
        pi_c = kb.sb("pi_c", [128, 1])

        def col(i):
            return prm[:, i, :]
        for (i, dr) in ((0, are), (1, aim), (2, ldt)):
            P.add("sync", lambda e, i=i, dr=dr: e.dma_start(out=prm[:, i, :], in_=dr[:, :]), writes=[("prm", i)], dma=True)
        P.add("sync", lambda e: e.dma_start(out=bb[:, 0], in_=bre[:, :, :]), writes=["bre"], dma=True)
        P.add("sync", lambda e: e.dma_start(out=bb[:, 1], in_=bim[:, :, :]), writes=["bim"], dma=True)
        P.add("sync", lambda e: e.dma_start(out=cs[:, 0], in_=cre[:, :, :]), writes=["cre"], dma=True)
        P.add("sync", lambda e: e.dma_start(out=cs[:, 1], in_=cim[:, :, :]), writes=["cim"], dma=True)
        P.add("sync", lambda e: e.dma_start(out=dsk_sb[:], in_=dsk[:, :]), writes=["dsk"], dma=True)
        P.add("sync", lambda e: e.dma_start(out=jrow[:], in_=jrow_d[:, :]), writes=["jrow"], dma=True)
        P.add("vector", lambda e: e.memset(pi_c[:], -PI), writes=["pi_c"])
        P.add("vector", lambda e: e.memset(init[:], 0.0), writes=[("init", pr) for pr in range(8)])
        K = "prmall"
        A = lambda fn, rd=(), eng="vector": P.add(eng, fn, reads=list(rd) + [K], writes=[K])
        TT = lambda o, a, b, op: A(lambda e: e.tensor_tensor(out=col(o), in0=col(a), in1=col(b), op=op))
        P.add("scalar", lambda e: e.activation(out=col(3), in_=col(2), func=AF.Exp), reads=[("prm", 0), ("prm", 1), ("prm", 2)], writes=[K])
        TT(4, 0, 3, ALU.mult)
        TT(5, 1, 3, ALU.mult)
        A(lambda e: e.activation(out=col(6), in_=col(4), func=AF.Exp), eng="scalar")
        sincos(P, col(5), col(7), col(8), col(20), col(21), prmi[:, 0, :], [K], [K], [K], K)
        TT(9, 6, 8, ALU.mult)
        TT(10, 6, 7, ALU.mult)
        A(lambda e: e.tensor_scalar(out=col(11), in0=col(9), scalar1=-1.0, scalar2=None, op0=ALU.add))
        TT(12, 0, 0, ALU.mult)
        TT(13, 1, 1, ALU.mult)
        TT(12, 12, 13, ALU.add)
        A(lambda e: e.reciprocal(out=col(12), in_=col(12)))
        TT(13, 11, 0, ALU.mult); TT(14, 10, 1, ALU.mult); TT(13, 13, 14, ALU.add); TT(13, 13, 12, ALU.mult)
        TT(14, 10, 0, ALU.mult); TT(15, 11, 1, ALU.mult); TT(14, 14, 15, ALU.subtract); TT(14, 14, 12, ALU.mult)
        A(lambda e: e.tensor_scalar(out=col(15), in0=col(14), scalar1=-1.0, scalar2=None, op0=ALU.mult))
        A(lambda e: e.tensor_scalar(out=col(16), in0=col(5), scalar1=512.0, scalar2=None, op0=ALU.mult))
        sincos(P, col(16), col(17), col(18), col(20), col(21), prmi[:, 0, :], [K], [K], [K], K)
        A(lambda e: e.tensor_scalar(out=col(19), in0=col(17), scalar1=-1.0, scalar2=None, op0=ALU.mult))
        for pr in range(8):
            zr = prm[:, 13, pr:pr + 1]; zi = prm[:, 14, pr:pr + 1]; nzi = prm[:, 15, pr:pr + 1]
            P.add("vector", lambda e, pr=pr, zr=zr: e.tensor_scalar(out=btmp[:, 0, :], in0=bb[:, 0, pr, :], scalar1=zr, scalar2=None, op0=ALU.mult),
                  reads=[K, "bre"], writes=["btmp0"])
            P.add("vector", lambda e, pr=pr, zi=zi: e.tensor_scalar(out=btmp[:, 1, :], in0=bb[:, 0, pr, :], scalar1=zi, scalar2=None, op0=ALU.mult),
                  reads=[K, "bre"], writes=["btmp1"])
            P.add("vector", lambda e, pr=pr, nzi=nzi: e.scalar_tensor_tensor(out=bb[:, 0, pr, :], in0=bb[:, 1, pr, :], scalar=nzi, in1=btmp[:, 0, :], op0=ALU.mult, op1=ALU.add),
                  reads=[K, "bim", "btmp0", "bre", "btmp1"], writes=["bre"])
            P.add("vector", lambda e, pr=pr, zr=zr: e.scalar_tensor_tensor(out=bb[:, 1, pr, :], in0=bb[:, 1, pr, :], scalar=zr, in1=btmp[:, 1, :], op0=ALU.mult, op1=ALU.add),
                  reads=[K, "bim", "btmp1", "bre"], writes=["bim"])
        P.add("gpsimd", lambda e: e.memset(Cl[:], 0.0), writes=["Cl"])
        for pr in range(8):
            pi_ = pr % 4
            for part in range(2):
                xb = part
                P.add("gpsimd", lambda e, xb=xb: e.memset(Xp[:, xb, :], 0.0), writes=[("Xp", xb)])
                P.add("vector", lambda e, xb=xb, pr=pr, part=part, pi_=pi_: e.tensor_copy(out=Xp[0:64, xb, 32 * pi_:32 * pi_ + 16], in_=bb[0:64, part, pr, :]),
                      reads=["bre", "bim", ("Xp", xb)], writes=[("Xp", xb)])
                P.add("vector", lambda e, xb=xb, pr=pr, part=part, pi_=pi_: e.tensor_copy(out=Xp[64:128, xb, 32 * pi_ + 16:32 * pi_ + 32], in_=bb[64:128, part, pr, :]),
                      reads=["bre", "bim", ("Xp", xb)], writes=[("Xp", xb)])
                P.add("tensor", lambda e, xb=xb: e.transpose(out=pD[:, xb * 128:(xb + 1) * 128], in_=Xp[:, xb, :], identity=ident[:]),
                      reads=[("Xp", xb), "ident"], writes=[("pDx", xb)])
                P.add("scalar", lambda e, xb=xb, pr=pr, part=part: e.activation(out=Blhs[:, pr, part, :], in_=pD[:, xb * 128:(xb + 1) * 128], func=AF.Copy),
                      reads=[("pDx", xb)], writes=[("Blhs", pr)])
                P.add("vector", lambda e, pr=pr, part=part, pi_=pi_: e.tensor_copy(out=Cl[0:64, pr, part, 32 * pi_:32 * pi_ + 16], in_=cs[0:64, part, pr, :]),
                      reads=["cre", "cim", "Cl"], writes=["Cl"])
                P.add("vector", lambda e, pr=pr, part=part, pi_=pi_: e.tensor_copy(out=Cl[64:128, pr, part, 32 * pi_ + 16:32 * pi_ + 32], in_=cs[64:128, part, pr, :]),
                      reads=["cre", "cim", "Cl"], writes=["Cl"])
        P.add("vector", lambda e: e.tensor_scalar(out=Cln[:].rearrange("p a b c -> p (a b c)"), in0=Cl[:].rearrange("p a b c -> p (a b c)"),
                                                  scalar1=-1.0, scalar2=None, op0=ALU.mult), reads=["Cl"], writes=["Cl"])
        for ut in range(2):
            P.add("vector", lambda e, ut=ut: e.tensor_scalar(out=Dl[:, ut, :], in0=ident[:], scalar1=dsk_sb[:, ut:ut + 1], scalar2=None, op0=ALU.mult),
                  reads=["ident", "dsk"], writes=["Dl"])
        for pr in range(8):
            th = prm[:, 5, pr:pr + 1]
            P.add("vector", lambda e, th=th: e.tensor_scalar(out=ang[:], in0=jrow[:], scalar1=th, scalar2=None, op0=ALU.mult), reads=["jrow", K], writes=["ang"])
            sincos(P, ang[:], sinT[:, pr, :], cosT[:, pr, :], w[0][:], w[1][:], angi[:], ["ang"], [("sinT", pr)], [("cosT", pr)], "w01")
            P.add("gpsimd", lambda e, pr=pr: e.memset(rT[:, pr, :], 1.0), writes=[("rT", pr)])
            P.add("gpsimd", lambda e, pr=pr: e.tensor_scalar(out=rT[:, pr, :], in0=rT[:, pr, :], scalar1=prm[:, 6, pr:pr + 1], scalar2=None, op0=ALU.mult),
                  reads=[("rT", pr), K], writes=[("rT", pr)])
        def s5_unit(c, ut, pi_, pr, csl, w, sb2, wb4, pbr, pbi, kbr, kbi, ws, ub):
            sre, sim = sb2
            P.add("tensor", lambda e: e.matmul(pbr[:], lhsT=Blhs[:, pr, 0, :], rhs=uTb[:, ut, csl], start=True, stop=True),
                  reads=[("Blhs", pr), ("uTb", ut, c)], writes=[kbr])
            P.add("tensor", lambda e: e.matmul(pbi[:], lhsT=Blhs[:, pr, 1, :], rhs=uTb[:, ut, csl], start=True, stop=True),
                  reads=[("Blhs", pr), ("uTb", ut, c)], writes=[kbi])
            P.add("scalar", lambda e: e.activation(out=sre[:], in_=pbr[:], func=AF.Copy), reads=[kbr], writes=[("sre", ub)])
            P.add("scalar", lambda e: e.activation(out=sim[:], in_=pbi[:], func=AF.Copy), reads=[kbi], writes=[("sim", ub)])
            ck, sk = ("cosT", pr), ("sinT", pr)
            P.add("vector", lambda e: e.tensor_tensor(out=w[0][:], in0=cosT[:, pr, :], in1=sre[:], op=ALU.mult), reads=[ck, ("sre", ub)], writes=[ws[0]])
            P.add("vector", lambda e: e.tensor_tensor(out=w[1][:], in0=sinT[:, pr, :], in1=sim[:], op=ALU.mult), reads=[sk, ("sim", ub)], writes=[ws[1]])
            P.add("vector", lambda e: e.tensor_tensor(out=w[2][:], in0=cosT[:, pr, :], in1=sim[:], op=ALU.mult), reads=[ck, ("sim", ub)], writes=[ws[2]])
            P.add("vector", lambda e: e.tensor_tensor(out=w[3][:], in0=sinT[:, pr, :], in1=sre[:], op=ALU.mult), reads=[sk, ("sre", ub)], writes=[ws[3]])
            P.add("vector", lambda e: e.tensor_tensor(out=w[0][:], in0=w[0][:], in1=w[1][:], op=ALU.add), reads=[ws[0], ws[1]], writes=[ws[0]])
            P.add("vector", lambda e: e.tensor_tensor(out=w[2][:], in0=w[2][:], in1=w[3][:], op=ALU.subtract), reads=[ws[2], ws[3]], writes=[ws[2]])
            P.add("vector", lambda e: e.tensor_tensor_scan(out=w[4][:], data0=rT[:, pr, :], data1=w[0][:], initial=init[:, pr, 0:1], op0=ALU.mult, op1=ALU.add),
                  reads=[("rT", pr), ws[0], ("init", pr)], writes=[ws[4]])
            P.add("vector", lambda e: e.tensor_tensor_scan(out=w[5][:], data0=rT[:, pr, :], data1=w[2][:], initial=init[:, pr, 1:2], op0=ALU.mult, op1=ALU.add),
                  reads=[("rT", pr), ws[2], ("init", pr)], writes=[ws[5]])
            cL = prm[:, 18, pr:pr + 1]; sL = prm[:, 17, pr:pr + 1]; nsL = prm[:, 19, pr:pr + 1]
            P.add("vector", lambda e: e.tensor_scalar(out=init[:, pr, 0:1], in0=w[4][:, 511:512], scalar1=cL, scalar2=None, op0=ALU.mult),
                  reads=[ws[4], K, ("init", pr)], writes=[("init", pr)])
            P.add("vector", lambda e: e.scalar_tensor_tensor(out=init[:, pr, 0:1], in0=w[5][:, 511:512], scalar=nsL, in1=init[:, pr, 0:1], op0=ALU.mult, op1=ALU.add),
                  reads=[ws[5], K, ("init", pr)], writes=[("init", pr)])
            P.add("vector", lambda e: e.tensor_scalar(out=init[:, pr, 1:2], in0=w[4][:, 511:512], scalar1=sL, scalar2=None, op0=ALU.mult),
                  reads=[ws[4], K, ("init", pr)], writes=[("init", pr)])
            P.add("vector", lambda e: e.scalar_tensor_tensor(out=init[:, pr, 1:2], in0=w[5][:, 511:512], scalar=cL, in1=init[:, pr, 1:2], op0=ALU.mult, op1=ALU.add),
                  reads=[ws[5], K, ("init", pr)], writes=[("init", pr)])
            srcs = ((cosT, ck, 4), (sinT, sk, 5), (sinT, sk, 4), (cosT, ck, 5))
            lhs = ((Cl, 0), (Cln, 0), (Cln, 1), (Cln, 1))
            for q in range(4):
                tab, tk, wi = srcs[q]
                P.add("gpsimd", lambda e, q=q, tab=tab, wi=wi: e.tensor_tensor(out=wb4[q][:], in0=tab[:, pr, :], in1=w[wi][:], op=ALU.mult),
                      reads=[tk, ws[wi]], writes=[("wb4", ub, q)])
            for q in range(4):
                ct, part = lhs[q]
                P.add("tensor", lambda e, q=q, ct=ct, part=part: e.matmul(pC[:], lhsT=ct[:, pr, part, :], rhs=wb4[q][:], start=(pi_ == 0 and q == 0), stop=False),
                      reads=["Cl", ("wb4", ub, q)], writes=["pC"])
        for c in range(NCk):
            csl = slice(c * 512, (c + 1) * 512)
            for ut in range(2):
                for pi_ in range(4):
                    pr = ut * 4 + pi_
                    ub = (c * 8 + pr) % 3
                    s5_unit(c, ut, pi_, pr, csl, wsets[ub], sb2s[ub], wb4s[ub], [pA, pS[0], pO[0]][ub], [pB, pS[1], pO[1]][ub],
                            ["pA", "pS0", ("pO", 0)][ub], ["pB", "pS1", ("pO", 1)][ub], ["w%d_%d" % (i, ub) for i in range(6)], ub)
                P.add("tensor", lambda e, ut=ut, csl=csl: e.matmul(pC[:], lhsT=Dl[:, ut, :], rhs=uTb[:, ut, csl], start=False, stop=True),
                      reads=["Dl", ("uTb", ut, c)], writes=["pC"])
                ob = (c * 2 + ut) % 2
                P.add("scalar", lambda e, ob=ob: e.activation(out=yst[:, ob, :], in_=pC[:], func=AF.Copy), reads=["pC"], writes=[("yst", ob)])
                if not fm:
                    P.add("sync", lambda e, ob=ob, ut=ut, csl=csl: e.dma_start(out=ysT[ut * 128:(ut + 1) * 128, csl], in_=yst[:, ob, :]),
                          reads=[("yst", ob)], dma=True, out=True)
                else:
                    half, tl = c // NH, (c % NH) * 512
                    P.add("sync", lambda e, ob=ob, ut=ut, half=half, tl=tl: e.dma_start(out=ysT_d[half, ut * 128:(ut + 1) * 128, tl:tl + 512], in_=yst[:, ob, :]),
                          reads=[("yst", ob)], writes=[("ysd", c, ut)], dma=True)
                    if ut == 1 and (c + 1) % NH == 0 and "ys_cb" in kb.over:
                        kb.over["ys_cb"](half, [("ysd", cc, u) for cc in range(half * NH, (half + 1) * NH) for u in range(2)])


def build_even(S=4096, mode='fox'):
    kb = KB()
    make_ident(kb)
    kb.mark()
    emit_even(kb, S, mode)
    print("even ops", kb.P.nops(), "peak words", kb.peak)
    return kb.finish()

import math
def emit_odd(kb, S=4096):
    nc, P = kb.nc, kb.P
    fm = 'mixT_d' in kb.over
    mixT_d = kb.over.get('mixT_d')
    NH = (S // 512) // 2
    NCk = S // 512
    NT = S // 128
    SC = 128 ** -0.5
    LNSC = math.log(SC)
    hT = kb.din("hT", [1024, S])
    wq = kb.din("wq", [1024, 512]); wk = kb.din("wk", [1024, 512]); wv = kb.din("wv", [1024, 512]); wo = kb.din("wo", [1024, 512])
    wi = kb.din("wi", [1024, 4]); wf = kb.din("wf", [1024, 4])
    cw = kb.din("cw", [128, 8, 4]); cb = kb.din("cb", [128, 8])
    ibias = kb.din("ibias", [4, 1]); fbias = kb.din("fbias", [4, 1])
    mixg = kb.dout("mixg", [S, 512]) if not fm else None

    ident = make_ident(kb)
    hTb = kb.sb("hTb", [128, 2, 8, 512], BF16)
    wq_sb = kb.sb("wq_sb", [128, 8, 512], BF16); wk_sb = kb.sb("wk_sb", [128, 8, 512], BF16)
    wv_sb = kb.sb("wv_sb", [128, 8, 512], BF16); wo_sb = kb.sb("wo_sb", [128, 8, 512], BF16)
    wi_sb = kb.sb("wi_sb", [128, 8, 4], BF16); wf_sb = kb.sb("wf_sb", [128, 8, 4], BF16)
    cw_sb = kb.sb("cw_sb", [128, 8, 4]); cb_sb = kb.sb("cb_sb", [128, 8])
    ib_sb = [kb.sb("ib0", [2, 1]), kb.sb("ib1", [2, 1])]
    fb_sb = [kb.sb("fb0", [2, 1]), kb.sb("fb1", [2, 1])]
    QT = kb.sb("QT", [128, 2, S], BF16)
    KT = kb.sb("KT", [128, 2, S], BF16)
    V = kb.sb("V", [128, NT, 2, 132], BF16)
    OG = kb.sb("OG", [128, NT, 256], BF16)
    gi = kb.sb("gi", [2, S]); gf = kb.sb("gf", [2, S])
    ones2 = kb.sb("ones2", [2, 512])
    selF = kb.sb("selF", [2, 2, 128])
    nbT = kb.sb("nbT", [128, NT, 2])
    pre = kb.sb("pre", [128, 4, 515])
    acc = kb.sb("acc", [128, 4, 512])
    FT = kb.sb("FT", [128, NT, 2])
    fr = kb.sb("fr", [2, NCk])
    frefP = kb.sb("frefP", [128, 2, NCk]); frefN = kb.sb("frefN", [128, 2, NCk])
    RF = kb.sb("RF", [128, 2, NT])
    cfc = kb.sb("cfc", [128, 4])
    W = kb.sb("W", [128, 3, 512], BF16)
    otmp = kb.sb("otmp", [128, 4, 129])
    rec = kb.sb("rec", [128, 4])
    ost = kb.sb("ost", [128, 2, 128])
    mst = kb.sb("mst", [128, 2, 512], BF16)
    pA = kb.ps("pA", [128, 512]); pB = kb.ps("pB", [128, 512])
    pS = [kb.ps("pS0", [128, 512]), kb.ps("pS1", [128, 512])]
    pO = [kb.ps("pO%d" % u, [128, 512]) for u in range(4)]

    for (wsb, wdr, nm) in ((wq_sb, wq, "wq"), (wk_sb, wk, "wk"), (wv_sb, wv, "wv"), (wo_sb, wo, "wo"), (wi_sb, wi, "wi"), (wf_sb, wf, "wf")):
        P.add("gpsimd", lambda e, wsb=wsb, wdr=wdr: e.dma_start(out=wsb[:], in_=wdr.rearrange("(k p) n -> p k n", p=128)), writes=[nm], dma=True)
    P.add("sync", lambda e: e.dma_start(out=cw_sb[:], in_=cw[:, :, :]), writes=["cw"], dma=True)
    P.add("sync", lambda e: e.dma_start(out=cb_sb[:], in_=cb[:, :]), writes=["cb"], dma=True)
    for hp in range(2):
        P.add("sync", lambda e, hp=hp: e.dma_start(out=ib_sb[hp][:], in_=ibias[hp * 2:hp * 2 + 2, :]), writes=[("ib", hp)], dma=True)
        P.add("sync", lambda e, hp=hp: e.dma_start(out=fb_sb[hp][:], in_=fbias[hp * 2:hp * 2 + 2, :]), writes=[("fb", hp)], dma=True)
    P.add("vector", lambda e: e.memset(ones2[:], 1.0), writes=["ones2"])
    P.add("vector", lambda e: e.memset(V[:, :, :, 128:129], 1.0), writes=["V1"])
    P.add("gpsimd", lambda e: e.memset(selF[:], 1.0), writes=["selF"])
    P.add("gpsimd", lambda e: e.affine_select(out=selF[:], in_=selF[:], pattern=[[-1, 2], [0, 128]], compare_op=ALU.is_equal, fill=0.0, base=0,
                                              channel_multiplier=1), reads=["selF"], writes=["selF"])
    cnt = 0
    for hp in range(2):
        P.add("vector", lambda e: e.memset(pre[:, :, 0:3], 0.0), reads=[("pre", i) for i in range(4)], writes=[("pre", i) for i in range(4)])
        for c in range(NCk):
            hb = c % 2
            csl = slice(c * 512, (c + 1) * 512)
            load_hT(kb, hTb, hb, c, hT)
            for hl in range(2):
                h = hp * 2 + hl
                for qk, (wsb, wn, dst, pb, pk) in enumerate(((wq_sb, "wq", QT, [pA, pS[0]][hl], ["pA", "pS0"][hl]),
                                                              (wk_sb, "wk", KT, [pB, pS[1]][hl], ["pB", "pS1"][hl]))):
                    idx = qk * 2 + hl
                    ci = qk * 4 + h
                    ab = qk * 2 + hl
                    for k in range(8):
                        P.add("tensor", lambda e, k=k, h=h, wsb=wsb, pb=pb, hb=hb: e.matmul(pb[:], lhsT=wsb[:, k, h * 128:(h + 1) * 128], rhs=hTb[:, hb, k, :],
                                                                                           start=(k == 0), stop=(k == 7)),
                              reads=[("hTb", hb), wn], writes=[pk])
                    P.add("scalar", lambda e, idx=idx, pb=pb: e.activation(out=pre[:, idx, 3:515], in_=pb[:], func=AF.Copy),
                          reads=[pk, ("pre", idx)], writes=[("pre", idx)])
                    P.add("vector", lambda e, idx=idx, ci=ci, ab=ab: e.tensor_scalar(out=acc[:, ab, :], in0=pre[:, idx, 0:512], scalar1=cw_sb[:, ci, 0:1],
                                                                                     scalar2=None, op0=ALU.mult),
                          reads=[("pre", idx), "cw"], writes=[("acc", ab)])
                    for jj in range(1, 4):
                        P.add("vector", lambda e, idx=idx, ci=ci, ab=ab, jj=jj: e.scalar_tensor_tensor(out=acc[:, ab, :], in0=pre[:, idx, jj:jj + 512],
                                                                                                      scalar=cw_sb[:, ci, jj:jj + 1], in1=acc[:, ab, :],
                                                                                                      op0=ALU.mult, op1=ALU.add),
                              reads=[("pre", idx), "cw", ("acc", ab)], writes=[("acc", ab)])
                    P.add("scalar", lambda e, dst=dst, hl=hl, ab=ab, ci=ci, csl=csl: e.activation(out=dst[:, hl, csl], in_=acc[:, ab, :], func=AF.Silu,
                                                                                                 bias=cb_sb[:, ci:ci + 1], scale=1.0),
                          reads=[("acc", ab), "cb"], writes=[(wn + "o", hl, c)])
                    P.add("vector", lambda e, idx=idx: e.tensor_copy(out=pre[:, idx, 0:3], in_=pre[:, idx, 512:515]),
                          reads=[("pre", idx)], writes=[("pre", idx)])
            for (wsb, wn, gt, gk, bs, bk, u) in ((wi_sb, "wi", gi, "gi", ib_sb[hp], ("ib", hp), 0), (wf_sb, "wf", gf, "gf", fb_sb[hp], ("fb", hp), 1)):
                for k in range(8):
                    P.add("tensor", lambda e, k=k, wsb=wsb, u=u, hb=hb, hp=hp: e.matmul(pO[u][0:2, :], lhsT=wsb[:, k, hp * 2:hp * 2 + 2], rhs=hTb[:, hb, k, :],
                                                                                start=(k == 0), stop=(k == 7)),
                          reads=[("hTb", hb), wn], writes=[("pO", u)])
                P.add("scalar", lambda e, gt=gt, bs=bs, u=u, csl=csl: e.activation(out=gt[:, csl], in_=pO[u][0:2, :], func=AF.Identity, bias=bs[:], scale=1.0),
                      reads=[("pO", u), bk], writes=[(gk, c), gk + "2"])
            for t4 in range(4):
                t = c * 4 + t4
                for k in range(8):
                    P.add("tensor", lambda e, k=k, t4=t4, hb=hb, hp=hp: e.matmul(pO[2][:, 0:256], lhsT=hTb[:, hb, k, t4 * 128:(t4 + 1) * 128],
                                                                           rhs=wv_sb[:, k, hp * 256:(hp + 1) * 256], start=(k == 0), stop=(k == 7)),
                          reads=[("hTb", hb), "wv"], writes=[("pO", 2)])
                P.add("vector", lambda e, t=t: e.tensor_copy(out=V[:, t, :, 0:128], in_=pO[2][:, 0:256].rearrange("p (h d) -> p h d", h=2)),
                      reads=[("pO", 2)], writes=[("V", t)])
                for k in range(8):
                    P.add("tensor", lambda e, k=k, t4=t4, hb=hb, hp=hp: e.matmul(pO[3][:, 0:256], lhsT=hTb[:, hb, k, t4 * 128:(t4 + 1) * 128],
                                                                           rhs=wo_sb[:, k, hp * 256:(hp + 1) * 256], start=(k == 0), stop=(k == 7)),
                          reads=[("hTb", hb), "wo"], writes=[("pO", 3)])
                P.add("scalar", lambda e, t=t: e.activation(out=OG[:, t, :], in_=pO[3][:, 0:256], func=AF.Sigmoid),
                      reads=[("pO", 3)], writes=[("OG", t)])
        gfk = [("gf", c) for c in range(NCk)]
        gik = [("gi", c) for c in range(NCk)]
        P.add("scalar", lambda e: e.activation(out=gf[:], in_=gf[:], func=AF.Exp, scale=-1.0), reads=gfk, writes=["gf2"])
        P.add("scalar", lambda e: e.activation(out=gf[:], in_=gf[:], func=AF.Ln, bias=1.0, scale=1.0), reads=["gf2"], writes=["gf2"])
        P.add("vector", lambda e: e.tensor_scalar(out=gf[:], in0=gf[:], scalar1=-1.0, scalar2=None, op0=ALU.mult), reads=["gf2"], writes=["gf2"])
        for c in range(NCk):
            csl = slice(c * 512, (c + 1) * 512)
            ini = 0.0 if c == 0 else gf[:, c * 512 - 1:c * 512]
            P.add("vector", lambda e, csl=csl, ini=ini: e.tensor_tensor_scan(out=gf[:, csl], data0=ones2[:], data1=gf[:, csl], initial=ini, op0=ALU.mult, op1=ALU.add),
                  reads=["gf2", "ones2"], writes=["gf2"])
        P.add("vector", lambda e: e.tensor_tensor(out=gi[:], in0=gi[:], in1=gf[:], op=ALU.subtract), reads=gik + ["gf2"], writes=["gi2"])
        for t in range(NT):
            P.add("tensor", lambda e, t=t: e.transpose(out=pB[:, t * 2:(t + 1) * 2], in_=gi[:, t * 128:(t + 1) * 128], identity=ident[0:2, 0:2]),
                  reads=["gi2", "ident"], writes=["pB"])
        P.add("vector", lambda e: e.tensor_copy(out=nbT[:].rearrange("p t h -> p (t h)"), in_=pB[:, 0:NT * 2]), reads=["pB"], writes=["nbT"])
        for t in range(NT):
            P.add("tensor", lambda e, t=t: e.transpose(out=pA[:, t * 2:(t + 1) * 2], in_=gf[:, t * 128:(t + 1) * 128], identity=ident[0:2, 0:2]),
                  reads=["gf2", "ident"], writes=["pA"])
        P.add("vector", lambda e: e.tensor_copy(out=FT[:].rearrange("p t h -> p (t h)"), in_=pA[:, 0:NT * 2]), reads=["pA"], writes=["FT"])
        P.add("vector", lambda e: e.memset(fr[:], 0.0), writes=["fr"])
        if NCk > 1:
            P.add("vector", lambda e: e.tensor_copy(out=fr[:, 1:NCk], in_=gf[:].rearrange("p (c t) -> p c t", t=512)[:, 0:NCk - 1, 511]),
                  reads=["gf2", "fr"], writes=["fr"])
        for hl in range(2):
            P.add("tensor", lambda e, hl=hl: e.matmul(pA[:, 256 + hl * 16:256 + hl * 16 + NCk], lhsT=selF[:, hl, :], rhs=fr[:], start=True, stop=True),
                  reads=["fr", "selF"], writes=["pA"])
            P.add("vector", lambda e, hl=hl: e.tensor_scalar(out=frefP[:, hl, :], in0=pA[:, 256 + hl * 16:256 + hl * 16 + NCk], scalar1=LNSC, scalar2=None, op0=ALU.add),
                  reads=["pA"], writes=["fref"])
            P.add("vector", lambda e, hl=hl: e.tensor_scalar(out=frefN[:, hl, :], in0=pA[:, 256 + hl * 16:256 + hl * 16 + NCk], scalar1=-1.0, scalar2=None, op0=ALU.mult),
                  reads=["pA"], writes=["fref"])
        tiles = []
        for hl in range(2):
            h = hp * 2 + hl
            for j in range(NCk):
                for i in range(4 * j + 4):
                    r = i - 4 * j
                    q0 = 128 * r if r > 0 else 0
                    sb_ = cnt % 3
                    cnt += 1
                    ps = [pS[0], pS[1], pA][sb_]
                    def fS(hl=hl, i=i, j=j, q0=q0, ps=ps, sb_=sb_):
                        P.add("tensor", lambda e, hl=hl, i=i, j=j, q0=q0, ps=ps: e.matmul(ps[:, q0:512], lhsT=KT[:, hl, i * 128:(i + 1) * 128],
                                                                                         rhs=QT[:, hl, j * 512 + q0:(j + 1) * 512], start=True, stop=True),
                              reads=[("wqo", hl, j), ("wko", hl, i // 4)], writes=[["pS0", "pS1", "pA"][sb_]])
                    def fR(hl=hl, h=h, i=i, j=j, r=r, q0=q0, ps=ps, sb_=sb_):
                        rb = (hl * NCk + j) % 2
                        if i == 0:
                            nt = 4 * j + 4
                            P.add("scalar", lambda e, hl=hl, j=j, nt=nt, rb=rb: e.activation(out=RF[:, rb, 0:nt], in_=nbT[:, 0:nt, hl], func=AF.Exp,
                                                                                            bias=frefP[:, hl, j:j + 1], scale=1.0),
                                  reads=["nbT", "fref"], writes=[("RF", rb)])
                        P.add("scalar", lambda e, i=i, q0=q0, sb_=sb_, ps=ps, rb=rb: e.activation(out=W[:, sb_, q0:512], in_=ps[:, q0:512], func=AF.Copy,
                                                                                                 scale=RF[:, rb, i:i + 1]),
                              reads=[["pS0", "pS1", "pA"][sb_], ("RF", rb)], writes=[("W", sb_)])
                        if r >= 0:
                            P.add("gpsimd", lambda e, sb_=sb_, q0=q0: e.affine_select(out=W[:, sb_, q0:q0 + 128], in_=W[:, sb_, q0:q0 + 128], pattern=[[1, 128]],
                                                                                       compare_op=ALU.is_ge, fill=0.0, base=0, channel_multiplier=-1),
                                  reads=[("W", sb_)], writes=[("W", sb_)])
                        for u in range(max(r, 0), 4):
                            P.add("tensor", lambda e, hl=hl, i=i, u=u, sb_=sb_, j=j: e.matmul(pO[u][:, 0:129], lhsT=W[:, sb_, u * 128:(u + 1) * 128],
                                                                                             rhs=V[:, i, hl, 0:129], start=(i == 0), stop=(i == 4 * j + u)),
                                  reads=[("W", sb_), ("V", i), "V1"], writes=[("pO", u)])
                            if i == 4 * j + u:
                                tt = j * 4 + u
                                ob = tt % 2
                                P.add("scalar", lambda e, u=u, tt=tt, hl=hl, j=j: e.activation(out=cfc[:, u:u + 1], in_=FT[:, tt, hl:hl + 1], func=AF.Exp,
                                                                                              bias=frefN[:, hl, j:j + 1], scale=1.0),
                                      reads=["FT", "fref"], writes=[("cfc", u)])
                                P.add("vector", lambda e, u=u: e.tensor_scalar(out=otmp[:, u, :], in0=pO[u][:, 0:129], scalar1=cfc[:, u:u + 1], scalar2=None, op0=ALU.mult),
                                      reads=[("pO", u), ("cfc", u)], writes=[("otmp", u)])
                                P.add("scalar", lambda e, u=u: e.activation(out=rec[:, u:u + 1], in_=otmp[:, u, 128:129], func=AF.Abs),
                                      reads=[("otmp", u)], writes=[("rec", u)])
                                P.add("vector", lambda e, u=u: e.tensor_scalar(out=rec[:, u:u + 1], in0=rec[:, u:u + 1], scalar1=1.0, scalar2=None, op0=ALU.max),
                                      reads=[("rec", u)], writes=[("rec", u)])
                                P.add("vector", lambda e, u=u: e.reciprocal(out=rec[:, u:u + 1], in_=rec[:, u:u + 1]), reads=[("rec", u)], writes=[("rec", u)])
                                P.add("vector", lambda e, u=u, ob=ob, hl=hl, tt=tt: e.scalar_tensor_tensor(out=ost[:, ob, :], in0=otmp[:, u, 0:128], scalar=rec[:, u:u + 1],
                                                                                                          in1=OG[:, tt, hl * 128:(hl + 1) * 128], op0=ALU.mult, op1=ALU.mult),
                                      reads=[("otmp", u), ("rec", u), ("OG", tt)], writes=[("ost", ob)])
                                if not fm:
                                    P.add("sync", lambda e, ob=ob, h=h, tt=tt: e.dma_start(out=mixg[tt * 128:(tt + 1) * 128, h * 128:(h + 1) * 128], in_=ost[:, ob, :]),
                                          reads=[("ost", ob)], dma=True, out=True)
                                else:
                                    def fin(h=h, j=j, u=u, ob=ob):
                                        stb = (h * NCk + j) % 2
                                        P.add("tensor", lambda e, ob=ob, u=u: e.transpose(out=pB[:, u * 128:(u + 1) * 128], in_=ost[:, ob, :], identity=ident[:]),
                                              reads=[("ost", ob), "ident"], writes=["pB"])
                                        P.add("scalar", lambda e, u=u, stb=stb: e.activation(out=mst[:, stb, u * 128:(u + 1) * 128], in_=pB[:, u * 128:(u + 1) * 128], func=AF.Copy),
                                              reads=["pB"], writes=[("mst", stb)])
                                        if u == 3:
                                            half, tl = j // NH, (j % NH) * 512
                                            P.add("sync", lambda e, stb=stb, h=h, half=half, tl=tl: e.dma_start(out=mixT_d[half, h * 128:(h + 1) * 128, tl:tl + 512], in_=mst[:, stb, :]),
                                                  reads=[("mst", stb)], dma=True)
                                    deferred.append(fin)
                    tiles.append((fS, fR))
        deferred = []
        for n in range(len(tiles)):
            if n == 0:
                tiles[0][0]()
                if len(tiles) > 1:
                    tiles[1][0]()
            if n + 2 < len(tiles):
                tiles[n + 2][0]()
            pend = list(deferred)
            del deferred[:]
            tiles[n][1]()
            for f in pend:
                f()
        for f in deferred:
            f()


def build_odd(S=4096):
    kb = KB()
    make_ident(kb)
    kb.mark()
    emit_odd(kb, S)
    print("odd ops", kb.P.nops(), "peak words", kb.peak)
    return kb.finish()


GROUPS = [[0, 1], [2, 3], [4, 5], [6, 7]]
SEQ = 4096


def _ag(kb, src, dst, rd=(), wr=()):
    kb.P.add("gpsimd", lambda e: e.collective_compute("AllGather", ALU.bypass, replica_groups=GROUPS, ins=[src], outs=[dst]),
             reads=list(rd), writes=list(wr), cc=True)


def build_fused():
    kb = KB()
    make_ident(kb)
    kb.mark()
    S, H = SEQ, SEQ // 2
    xT = kb.din("xT", [1024, S])
    xh = kb.din("xh", [1024, H])
    attT_d = kb.dint("attT_d", [2, 256, H], BF16)
    attG_d = kb.dint("attG_d", [1024, H], BF16)
    ysT_d = kb.dint("ysT_d", [2, 256, H], F32)
    ysG_d = kb.dint("ysG_d", [1024, H], F32)
    mixT_d = kb.dint("mixT_d", [2, 512, H], BF16)
    mixG_d = kb.dint("mixG_d", [2048, H], BF16)
    hres_d = kb.dint("hres_d", [1024, H], F32)
    hb_d = kb.dint("hb_d", [2, 1024, H // 2], BF16)
    hg_d = kb.dint("hg_d", [2, 2048, H // 2], BF16)
    att_m = kb.dint("att_m", [512, H], BF16)
    ys_m = kb.dint("ys_m", [512, H], F32)
    mix_m = kb.dint("mix_m", [1024, H], BF16)

    kb.P.use_pid = True

    def rk(e):
        return kb.P.pidval % 2

    for layer in range(4):
        even = (layer % 2 == 0)
        if layer == 0:
            kb.hmode = ("ext", None)
            hsrc = xT
        else:
            kb.hmode = ("gath", hg_d)
            hsrc = None
        kb.phase(wait_cc=False)
        if even:
            kb.prefix = "L%df_" % layer
            kb.over = {"hT": hsrc, "attT_d": attT_d}
            emit_even(kb, S, 'fox')
            kb.phase()
            for k in range(2):
                _ag(kb, attT_d[k], attG_d[k * 512:(k + 1) * 512, :], wr=["attG"])
            kb.prefix = "L%ds_" % layer
            kb.over = {"hT": hsrc, "ysT_d": ysT_d,
                       "ys_cb": lambda half, keys: _ag(kb, ysT_d[half], ysG_d[half * 512:(half + 1) * 512, :], rd=keys, wr=["ysG"])}
            emit_even(kb, S, 's5')
            kb.phase(wait_cc=False)
            kb.P.add("sync", lambda e: e.dma_start(out=att_m[:, :], in_=attG_d[bass.ds(rk(e) * 512, 512), :]), reads=["attG"], writes=["xsel"], dma=True)
            kb.P.add("sync", lambda e: e.dma_start(out=ys_m[:, :], in_=ysG_d[bass.ds(rk(e) * 512, 512), :]), reads=["ysG"], writes=["xsel2"], dma=True)
        else:
            kb.prefix = "L%dm_" % layer
            kb.over = {"hT": hsrc, "mixT_d": mixT_d}
            emit_odd(kb, S)
            kb.phase()
            for k in range(2):
                _ag(kb, mixT_d[k], mixG_d[k * 1024:(k + 1) * 1024, :], wr=["mixG"])
            kb.P.add("sync", lambda e: e.dma_start(out=mix_m[:, :], in_=mixG_d[bass.ds(rk(e) * 1024, 1024), :]), reads=["mixG"], writes=["xsel"], dma=True)
        kb.prefix = "L%dp_" % layer
        ov = {"hT": xh if layer == 0 else hres_d, "xkeys": ["xsel", "xsel2"] if even else ["xsel"]}
        if even:
            ov["attT"] = att_m
            ov["ysT"] = ys_m
        else:
            ov["mixT"] = mix_m
        if layer < 3:
            ov["houtT"] = hres_d
            ov["hbT"] = hb_d
            ov["hb_cb"] = lambda t2, keys: _ag(kb, hb_d[t2], hg_d[t2], rd=keys, wr=["hg"])
        kb.over = ov
        emit_post(kb, even, TOK)
    print("fused ops", kb.P.nops(), "peak words", kb.peak, "phases", kb.P.phase + 1)
    return kb.finish()


def _even_inputs(j, hh, d):
    w_in = d['even_w_in'][j]
    hs = slice(hh * 256, (hh + 1) * 256)
    g0 = hh * 16
    c = np.ascontiguousarray

    def pl(a):
        return c(a[g0:g0 + 16].reshape(8, 2, 64).transpose(1, 2, 0).reshape(128, 8))

    def plb(a):
        return c(a[g0:g0 + 16].reshape(8, 2, 64, 16).transpose(1, 2, 0, 3).reshape(128, 8, 16))

    def plc(a):
        return c(a[g0:g0 + 16].reshape(8, 2, 16, 64).transpose(1, 3, 0, 2).reshape(128, 8, 16))
    ldt = np.repeat(d['s5_log_dt'][j][:, None], 64, 1)
    fox = dict(wq=c(w_in[:, 0:512][:, hs]), wk=c(w_in[:, 512:1024][:, hs]), wv=c(w_in[:, 1024:1536][:, hs]),
               wf=c(w_in[:, 1536 + hh * 4:1536 + hh * 4 + 4]), fbias=c(d['fox_f_bias'][j][hh * 4:hh * 4 + 4, None]))
    s5 = dict(wu=c(w_in[:, 1544:][:, hs]), are=pl(d['s5_a_re'][j]), aim=pl(d['s5_a_im'][j]), ldt=pl(ldt),
              bre=plb(d['s5_b_re'][j]), bim=plb(d['s5_b_im'][j]), cre=plc(d['s5_c_re'][j]), cim=plc(d['s5_c_im'][j]),
              dsk=c(d['s5_d'][j][g0:g0 + 16].reshape(2, 128).T), jrow=np.tile(np.arange(512, dtype=np.float32), (128, 1)))
    return fox, s5


def _odd_inputs(j, hh, d):
    c = np.ascontiguousarray
    w_in = d['odd_w_in'][j]
    cs = slice(hh * 512, (hh + 1) * 512)
    cwf = d['mlstm_conv_w'][j]
    cbf = d['mlstm_conv_b'][j]
    qcols = np.arange(hh * 512, (hh + 1) * 512)
    cols = np.concatenate([qcols, 1024 + qcols])
    cw = c(cwf[:, cols].reshape(4, 8, 128).transpose(2, 1, 0))
    cb = c(cbf[cols].reshape(8, 128).T)
    return dict(wq=c(w_in[:, 0:1024][:, cs]), wk=c(w_in[:, 1024:2048][:, cs]), wv=c(w_in[:, 2048:3072][:, cs]),
                wo=c(w_in[:, 3072:4096][:, cs]), wi=c(w_in[:, 4096 + hh * 4:4096 + hh * 4 + 4]),
                wf=c(w_in[:, 4104 + hh * 4:4104 + hh * 4 + 4]), cw=cw, cb=cb,
                ibias=c(d['mlstm_i_bias'][j][hh * 4:hh * 4 + 4, None]), fbias=c(d['mlstm_f_bias'][j][hh * 4:hh * 4 + 4, None]))


def kernel(**d):
    d = {k: np.asarray(v) for k, v in d.items()}
    x = d['x'].astype(np.float32)
    B, S, D = x.shape
    cores = list(range(8))
    c = np.ascontiguousarray
    shared = {}
    for layer in range(4):
        j = layer // 2
        wr = c(np.concatenate([d['moe_w_group'][layer], d['moe_w_expert'][layer].transpose(1, 0, 2).reshape(1024, 16)], 1))
        br = c(np.concatenate([d['moe_b_group'][layer], d['moe_b_expert'][layer].reshape(16)]))
        lnp = c(np.stack([d['ln_g'][layer, 0], d['ln_b'][layer, 0], d['ln_g'][layer, 1], d['ln_b'][layer, 1]]))
        p = dict(lnp=lnp, wr=wr, br=br, wg=d['moe_w_gate'][layer], wu=d['moe_w_up'][layer], wd=d['moe_w_down'][layer])
        if layer % 2 == 0:
            p.update(w_glu=d['s5_w_glu'][j], b_glu=d['s5_b_glu'][j], w_out=d['even_w_out'][j])
        else:
            p.update(w_out=d['odd_w_out'][j])
        for k, v in p.items():
            shared["L%dp_%s" % (layer, k)] = c(v)
    in_maps = []
    for core in cores:
        b, r = core // 2, core % 2
        im = dict(shared)
        im["xT"] = c(x[b].T)
        im["xh"] = c(x[b, r * (S // 2):(r + 1) * (S // 2)].T)
        for layer in range(4):
            j = layer // 2
            if layer % 2 == 0:
                f_, s_ = _even_inputs(j, r, d)
                for k, v in f_.items():
                    im["L%df_%s" % (layer, k)] = v
                for k, v in s_.items():
                    im["L%ds_%s" % (layer, k)] = v
            else:
                for k, v in _odd_inputs(j, r, d).items():
                    im["L%dm_%s" % (layer, k)] = v
        in_maps.append(im)
    res = run_bass_kernel_spmd(build_fused(), in_maps, core_ids=cores).results
    out = np.zeros((B, S, D), np.float32)
    for core in cores:
        b, r = core // 2, core % 2
        out[b, r * (S // 2):(r + 1) * (S // 2), :] = res[core]["L3p_houtT"].T
    return out
```

```python
import numpy as np
import concourse.bass as bass
import concourse.mybir as mybir
from concourse.bass_utils import run_bass_kernel_spmd

F32 = mybir.dt.float32
BF16 = mybir.dt.bfloat16
ALU = mybir.AluOpType
AF = mybir.ActivationFunctionType
AX = mybir.AxisListType

ENGS = ["sync", "scalar", "vector", "gpsimd", "tensor"]
NDSEM = 24


class Op:
    __slots__ = ("eng", "fn", "idx", "waits", "dwaits", "signal", "sval", "is_dma", "dnum", "guard", "is_cc", "ccnum", "ccwaits", "phase")

    def __init__(self, eng, fn, is_dma):
        self.eng = eng
        self.fn = fn
        self.waits = []
        self.dwaits = []
        self.signal = False
        self.sval = 0
        self.is_dma = is_dma
        self.dnum = -1
        self.guard = None
        self.is_cc = False
        self.ccnum = -1
        self.ccwaits = []
        self.phase = 0


class Prog:
    def __init__(self, nc):
        self.nc = nc
        self.ops = {e: [] for e in ENGS}
        self.last_writer = {}
        self.readers = {}
        self.seen = {e: {f: -1 for f in ENGS} for e in ENGS}
        self.seen_dma = {e: set() for e in ENGS}
        self.ndma = 0
        self.ncc = 0
        self.seen_cc = {e: -1 for e in ENGS}
        self.out_dmas = []
        self.dma_ops = []
        self.cc_ops = []
        self.use_pid = False
        self.pidval = None
        self.phase = 0
        self.last_compute = {e: None for e in ENGS}

    def barrier(self, wait_cc=True):
        for e in ENGS:
            b = Op(e, None, False)
            b.idx = len(self.ops[e])
            b.phase = self.phase
            for f in ENGS:
                lc = self.last_compute[f]
                if f == e or lc is None:
                    continue
                if lc.phase == self.phase and self.seen[e][f] >= lc.idx:
                    continue
                lc.signal = True
                b.waits.append(lc)
            for d in self.dma_ops[-NDSEM:]:
                if d.dnum not in self.seen_dma[e]:
                    b.dwaits.append(d)
            if wait_cc and self.cc_ops and self.seen_cc[e] < self.cc_ops[-1].ccnum:
                b.ccwaits.append(self.cc_ops[-1])
            self.ops[e].append(b)
        self.phase += 1
        for e in ENGS:
            for f in ENGS:
                self.seen[e][f] = -1
            self.seen_dma[e] = set(range(self.ndma))
            if wait_cc:
                self.seen_cc[e] = self.ncc - 1
        self.last_writer = {} if wait_cc else {k: v for k, v in self.last_writer.items() if v.is_cc}
        self.readers = {}
        self.last_compute = {e: None for e in ENGS}

    def add(self, eng, fn, reads=(), writes=(), dma=False, out=False, cc=False):
        op = Op(eng, fn, dma)
        op.is_cc = cc
        op.idx = len(self.ops[eng])
        op.phase = self.phase
        deps = []
        for k in reads:
            w = self.last_writer.get(k)
            if w is not None:
                deps.append((w, "raw"))
        for k in writes:
            w = self.last_writer.get(k)
            if w is not None:
                deps.append((w, "waw"))
            for r in self.readers.get(k, ()):
                deps.append((r, "war"))
        for d, kind in deps:
            if d is op:
                continue
            if d.is_cc:
                if self.seen_cc[eng] >= d.ccnum:
                    continue
                self.seen_cc[eng] = d.ccnum
                op.ccwaits.append(d)
            elif d.is_dma:
                if d.dnum in self.seen_dma[eng]:
                    continue
                self.seen_dma[eng].add(d.dnum)
                op.dwaits.append(d)
            else:
                if d.eng == eng and (eng == "tensor" or kind == "war"):
                    continue
                if self.seen[eng][d.eng] >= d.idx:
                    continue
                self.seen[eng][d.eng] = d.idx
                d.signal = True
                op.waits.append(d)
        if cc:
            op.ccnum = self.ncc
            self.ncc += 1
            self.cc_ops.append(op)
            self.seen_cc[eng] = max(self.seen_cc[eng], op.ccnum - 1)
        if dma:
            op.dnum = self.ndma
            self.ndma += 1
            self.dma_ops.append(op)
            if op.dnum >= NDSEM:
                op.guard = op.dnum - NDSEM
                self.seen_dma[eng].add(op.guard)
            if out:
                self.out_dmas.append(op)
        for k in reads:
            self.readers.setdefault(k, []).append(op)
        for k in writes:
            self.last_writer[k] = op
            self.readers[k] = []
        if not dma and not cc:
            self.last_compute[eng] = op
        self.ops[eng].append(op)
        return op

    def emit(self):
        nc = self.nc
        fin = Op("sync", None, False)
        fin.idx = len(self.ops["sync"])
        fin.dwaits = [d for d in self.out_dmas]
        self.ops["sync"].append(fin)
        fin.phase = self.phase
        for e in ENGS:
            c = {}
            for op in self.ops[e]:
                if op.signal:
                    c[op.phase] = c.get(op.phase, 0) + 1
                    op.sval = c[op.phase]
        import contextlib
        with contextlib.ExitStack() as st:
            need = sorted({(op.phase, e) for e in ENGS for op in self.ops[e] if op.signal})
            esem = {(ph, e): st.enter_context(nc.semaphore("s%d_%s" % (ph, e))) for (ph, e) in need}
            dsem = [st.enter_context(nc.semaphore("d_%d" % i)) for i in range(NDSEM)]
            ccsem = st.enter_context(nc.semaphore("ccs"))
            block = st.enter_context(nc.Block())

            def mk(e):
                def body(eng):
                    self.pidval = eng.partition_id() if (self.use_pid and e in ("sync", "gpsimd")) else None
                    for op in self.ops[e]:
                        for d in op.waits:
                            eng.wait_ge(esem[(d.phase, d.eng)], d.sval)
                        for d in op.dwaits:
                            eng.wait_ge(dsem[d.dnum % NDSEM], 16 * (d.dnum // NDSEM + 1))
                        for d in op.ccwaits:
                            eng.wait_ge(ccsem, d.ccnum + 1)
                        if op.is_cc and op.ccnum > 0:
                            eng.wait_ge(ccsem, op.ccnum)
                        if op.guard is not None:
                            g = op.guard
                            eng.wait_ge(dsem[g % NDSEM], 16 * (g // NDSEM + 1))
                        if op.fn is None:
                            continue
                        ins = op.fn(eng)
                        if op.is_cc:
                            ins.then_inc(ccsem, 1)
                        elif op.is_dma:
                            ins.then_inc(dsem[op.dnum % NDSEM], 16)
                        elif op.signal:
                            ins.then_inc(esem[(op.phase, e)], 1)
                return body

            block.sync(mk("sync"))
            block.scalar(mk("scalar"))
            block.vector(mk("vector"))
            block.gpsimd(mk("gpsimd"))
            block.tensor(mk("tensor"))

    def nops(self):
        return {e: len(v) for e, v in self.ops.items()}

import contextlib
import math

DN_ALPHA = 8 ** 0.25
LN_EPS = 1e-5
TOK = 2048
CH = 512
ARENA_WORDS = 52000
I32 = mybir.dt.int32
PI = math.pi


def S_(x, e):
    return x(e) if callable(x) else x


class KB:
    def __init__(self, arena_words=ARENA_WORDS):
        self.nc = bass.Bass("TRN2", target_bir_lowering=False)
        self.st = contextlib.ExitStack()
        self.P = Prog(self.nc)
        self.prefix = ""
        self.over = {}
        self.dram = {}
        self.cache = {}
        self.arena = self.st.enter_context(self.nc.sbuf_tensor("arena", [128, arena_words], F32))
        self.words = arena_words
        self.aoff = 0
        self.amark = 0
        self.banks = [self.st.enter_context(self.nc.psum_tensor("bank%d" % i, [128, 512], F32)) for i in range(8)]
        self.bank_i = 0
        self.hmode = ("ext", None)
        self.peak = 0

    def _dram(self, name, shape, dt, kind):
        if name in self.over:
            return self.over[name]
        full = name if kind == "Internal" else self.prefix + name
        if full not in self.dram:
            self.dram[full] = self.nc.dram_tensor(full, list(shape), dt, kind=kind).ap()
        return self.dram[full]

    def din(self, name, shape, dt=F32):
        return self._dram(name, shape, dt, "ExternalInput")

    def dout(self, name, shape, dt=F32):
        return self._dram(name, shape, dt, "ExternalOutput")

    def dint(self, name, shape, dt=F32):
        return self._dram(name, shape, dt, "Internal")

    def sb(self, name, shape, dt=F32):
        n = 1
        for s in shape[1:]:
            n *= s
        esz = 2 if dt == BF16 else 4
        words = (n * esz + 3) // 4
        words = (words + 7) // 8 * 8
        assert self.aoff + words <= self.words, ("SBUF arena overflow", name, self.aoff, words)
        v = self.arena[0:shape[0], self.aoff:self.aoff + words]
        self.aoff += words
        self.peak = max(self.peak, self.aoff)
        if dt != F32:
            v = v.bitcast(dt)
        v = v[:, 0:n]
        if len(shape) == 3:
            v = v.rearrange("p (a b) -> p a b", a=shape[1])
        elif len(shape) == 4:
            v = v.rearrange("p (a b c) -> p a b c", a=shape[1], b=shape[2])
        return v

    def ps(self, name, shape, dt=F32):
        b = self.banks[self.bank_i]
        self.bank_i += 1
        return b

    def mark(self):
        self.amark = self.aoff

    def phase(self, wait_cc=True):
        self.P.barrier(wait_cc)
        self.aoff = self.amark
        self.bank_i = 0

    def finish(self):
        self.P.emit()
        self.st.close()
        return self.nc


def make_ident(kb, name="ident", dt=F32):
    if name in kb.cache:
        return kb.cache[name]
    P = kb.P
    ident = kb.sb(name, [128, 128], dt)
    P.add("gpsimd", lambda e: e.memset(ident[:], 0.0), writes=[name])
    P.add("gpsimd", lambda e: e.affine_select(out=ident[:], in_=ident[:], pattern=[[-1, 128]], compare_op=ALU.not_equal,
                                              fill=1.0, base=0, channel_multiplier=1), reads=[name], writes=[name])
    kb.cache[name] = ident
    return ident


def load_hT(kb, hTb, hb, c, hT):
    P = kb.P
    mode, src = kb.hmode
    if mode == "ext":
        csl = slice(c * 512, (c + 1) * 512)
        P.add("gpsimd", lambda e: e.dma_start(out=hTb[:, hb], in_=hT.rearrange("(m p) t -> p m t", p=128)[:, :, csl]),
              writes=[("hTb", hb)], dma=True)
    else:
        q, lc = c // 4, c % 4
        t2, tl = lc // 2, (lc % 2) * 512
        P.add("sync", lambda e: e.dma_start(out=hTb[:, hb], in_=src[t2, q * 1024:(q + 1) * 1024, tl:tl + 512].rearrange("(m p) t -> p m t", p=128)),
              reads=["hg"], writes=[("hTb", hb)], dma=True)

def ln_feature_major(kb, pfx, r, rk, g_col, b_col, gk, onesf, pm, pq, tmp, outs, n=CH):
    P = kb.P
    sq, mean, rstd, t = tmp["sq"], tmp["mean"], tmp["rstd"], tmp["t"]
    for m in range(8):
        P.add("tensor", lambda e, m=m: e.matmul(pm[:, :n], lhsT=onesf[:], rhs=r[:, m, :n], start=(m == 0), stop=(m == 7)),
              reads=[rk(m), "onesf"], writes=["pm"])
    for m in range(8):
        P.add("scalar", lambda e, m=m: e.activation(out=sq[:, m % 2, :n], in_=r[:, m, :n], func=AF.Square),
              reads=[rk(m)], writes=[(pfx + "sq", m % 2)])
        P.add("tensor", lambda e, m=m: e.matmul(pq[:, :n], lhsT=onesf[:], rhs=sq[:, m % 2, :n], start=(m == 0), stop=(m == 7)),
              reads=[(pfx + "sq", m % 2), "onesf"], writes=["pq"])
    P.add("scalar", lambda e: e.activation(out=mean[:, :n], in_=pm[:, :n], func=AF.Identity), reads=["pm"], writes=[pfx + "mean"])
    P.add("scalar", lambda e: e.activation(out=rstd[:, :n], in_=pm[:, :n], func=AF.Square), reads=["pm"], writes=[pfx + "rstd"])
    P.add("vector", lambda e: e.tensor_tensor(out=rstd[:, :n], in0=pq[:, :n], in1=rstd[:, :n], op=ALU.subtract),
          reads=["pq", pfx + "rstd"], writes=[pfx + "rstd"])
    P.add("scalar", lambda e: e.activation(out=rstd[:, :n], in_=rstd[:, :n], func=AF.Sqrt, bias=tmp["eps"][:], scale=1.0),
          reads=[pfx + "rstd", "eps"], writes=[pfx + "rstd"])
    P.add("vector", lambda e: e.reciprocal(out=rstd[:, :n], in_=rstd[:, :n]), reads=[pfx + "rstd"], writes=[pfx + "rstd"])
    for m in range(8):
        P.add("vector", lambda e, m=m: e.tensor_tensor(out=t[:, m % 2, :n], in0=r[:, m, :n], in1=mean[:, :n], op=ALU.subtract),
              reads=[rk(m), pfx + "mean"], writes=[(pfx + "t", m % 2)])
        P.add("vector", lambda e, m=m: e.tensor_tensor(out=t[:, m % 2, :n], in0=t[:, m % 2, :n], in1=rstd[:, :n], op=ALU.mult),
              reads=[(pfx + "t", m % 2), pfx + "rstd"], writes=[(pfx + "t", m % 2)])
        for oent in outs:
            (ot, okf, oeng) = oent[:3]
            g_c, b_c = (oent[3], oent[4]) if len(oent) > 3 else (g_col, b_col)
            if oeng == "scalar":
                P.add("scalar", lambda e, m=m, ot=ot, g_c=g_c, b_c=b_c: e.activation(out=ot[:, m, :n], in_=t[:, m % 2, :n], func=AF.Identity,
                                                                    bias=b_c(m), scale=g_c(m)),
                      reads=[(pfx + "t", m % 2), gk], writes=[okf(m)])
            else:
                P.add(oeng, lambda e, m=m, ot=ot, g_c=g_c, b_c=b_c: e.tensor_scalar(out=ot[:, m, :n], in0=t[:, m % 2, :n], scalar1=g_c(m), scalar2=b_c(m),
                                                                   op0=ALU.mult, op1=ALU.add),
                      reads=[(pfx + "t", m % 2), gk], writes=[okf(m)])


def emit_post(kb, even, tok=TOK):
    nc, P = kb.nc, kb.P
    nch = tok // CH
    hT = kb.din("hT", [1024, tok])
    if even:
        attT = kb.din("attT", [512, tok])
        ysT = kb.din("ysT", [512, tok])
        w_glu = kb.din("w_glu", [512, 512])
        b_glu = kb.din("b_glu", [512])
    else:
        mixTd = kb.din("mixT", [1024, tok])
    w_out = kb.din("w_out", [1024, 1024])
    lnp = kb.din("lnp", [4, 1024])
    wr = kb.din("wr", [1024, 20])
    br = kb.din("br", [20])
    wg = kb.din("wg", [16, 1024, 256])
    wu = kb.din("wu", [16, 1024, 256])
    wd = kb.din("wd", [16, 256, 1024])
    houtT = kb.dout("houtT", [1024, tok])
    hbT = kb.over.get("hbT")
    out_final = "houtT" not in kb.over
    xk = list(kb.over.get("xkeys", []))
    ident = make_ident(kb)

    x1b_all = kb.sb("x1b_all", [128, nch, 8, CH], BF16)
    acc_all = kb.sb("acc_all", [128, nch, 8, CH])
    combT_all = kb.sb("combT_all", [16, nch * CH])
    lnc = kb.sb("lnc", [128, 4, 8])
    lnca_t = kb.sb("lnca", [128, 2, 8])
    wr_sb = kb.sb("wr_sb", [128, 8, 20])
    br_sb = kb.sb("br_sb", [128, 20])
    onesf = kb.sb("onesf", [128, 128])
    eps = kb.sb("eps", [128, 1])
    sel = kb.sb("sel", [16, 16, 128])
    sub_mark = kb.aoff
    po = [kb.ps("po0", [128, CH]), kb.ps("po1", [128, CH])]
    pm = kb.ps("pm", [128, CH])
    pq = kb.ps("pq", [128, CH])
    pg = [kb.ps("pg0", [128, CH]), kb.ps("pg1", [128, CH])]
    pu = [kb.ps("pu0", [128, CH]), kb.ps("pu1", [128, CH])]

    def subphase():
        P.barrier()
        kb.aoff = sub_mark

    wout_sb = kb.sb("wout_sb", [128, 8, 1024], BF16)
    mixT = kb.sb("mixT_sb", [128, 8, CH], BF16)
    r = kb.sb("r", [128, 8, CH])
    tmp = dict(sq=kb.sb("sq", [128, 2, CH]), mean=kb.sb("mean", [128, CH]), rstd=kb.sb("rstd", [128, CH]),
               t=kb.sb("lt", [128, 2, CH]), eps=eps)
    lgb = kb.sb("lgb", [128, 4, 20])
    gm = kb.sb("gm", [128, 4]); gv = kb.sb("gv", [128, 4]); dd = kb.sb("dd", [128, 4]); w1 = kb.sb("w1", [128, 4]); w2 = kb.sb("w2", [128, 4])
    gk = kb.sb("gk", [128, 4, 4]); gx = kb.sb("gx", [128, 4, 4])
    em = kb.sb("em", [128, 4, 16]); m1k = kb.sb("m1k", [128, 4, 16]); m2k = kb.sb("m2k", [128, 4, 16]); combb = kb.sb("combb", [128, 4, 16])
    top = kb.sb("top", [128, 4, 8])
    lnrow = kb.sb("lnrow", [32, 128])
    if even:
        ys = kb.sb("ys", [128, 4, CH])
        yt = kb.sb("yt", [128, 2, CH])
        ygb = kb.sb("ygb", [128, 4, CH], BF16)
        wglu_sb = kb.sb("wglu_sb", [128, 4, 512], BF16)
        bglu_sb = kb.sb("bglu_sb", [128, 4])
        bgrow = kb.sb("bgrow", [4, 128])

    P.add("gpsimd", lambda e: e.dma_start(out=wout_sb[:], in_=w_out.rearrange("(k p) n -> p k n", p=128)), writes=["wout"], dma=True)
    P.add("sync", lambda e: e.dma_start(out=lnrow[:], in_=lnp.rearrange("i (m p) -> (i m) p", p=128)), writes=["lnrow"], dma=True)
    P.add("tensor", lambda e: e.transpose(out=pq[:, 0:32], in_=lnrow[:], identity=ident[0:32, 0:32]), reads=["lnrow", "ident"], writes=["pq"])
    P.add("vector", lambda e: e.tensor_copy(out=lnc[:].rearrange("p i m -> p (i m)"), in_=pq[:, 0:32]), reads=["pq"], writes=["lnc"])
    P.add("sync", lambda e: e.dma_start(out=wr_sb[:], in_=wr.rearrange("(k p) n -> p k n", p=128)), writes=["wr"], dma=True)
    P.add("sync", lambda e: e.dma_start(out=br_sb[:], in_=br.rearrange("(o n) -> o n", o=1).to_broadcast([128, 20])), writes=["br"], dma=True)
    P.add("vector", lambda e: e.memset(onesf[:], 1.0 / 1024.0), writes=["onesf"])
    P.add("vector", lambda e: e.memset(eps[:], LN_EPS), writes=["eps"])
    P.add("gpsimd", lambda e: e.memset(sel[:], 1.0), writes=["sel"])
    P.add("gpsimd", lambda e: e.affine_select(out=sel[:], in_=sel[:], pattern=[[-1, 16], [0, 128]], compare_op=ALU.is_equal,
                                              fill=0.0, base=0, channel_multiplier=1), reads=["sel"], writes=["sel"])
    if even:
        P.add("gpsimd", lambda e: e.dma_start(out=wglu_sb[:], in_=w_glu.rearrange("(k p) n -> p k n", p=128)), writes=["wglu"], dma=True)
        P.add("sync", lambda e: e.dma_start(out=bgrow[:], in_=b_glu.rearrange("(n p) -> n p", p=128)), writes=["bgrow"], dma=True)
        P.add("tensor", lambda e: e.transpose(out=pq[:, 0:4], in_=bgrow[:], identity=ident[0:4, 0:4]), reads=["bgrow", "ident"], writes=["pq"])
        P.add("vector", lambda e: e.tensor_copy(out=bglu_sb[:], in_=pq[:, 0:4]), reads=["pq"], writes=["bglu"])

    gcol = lambda i: (lambda m: lnc[:, i, m:m + 1])
    lnca = kb.sb("lnca", [128, 2, 8]) if False else lnca_t
    P.add("vector", lambda e: e.tensor_scalar(out=lnca[:], in0=lnc[:, 0:2, :], scalar1=DN_ALPHA, scalar2=None, op0=ALU.mult), reads=["lnc"], writes=["lnc"])
    gcola = lambda i: (lambda m: lnca[:, i, m:m + 1])

    def chunkA(c, part):
        csl = slice(c * CH, (c + 1) * CH)
        accv = acc_all[:, c]
        x1bv = x1b_all[:, c]
        if part == 1:
            return chunkA_router(c, accv)
        P.add("sync", lambda e, csl=csl, accv=accv: e.dma_start(out=accv, in_=S_(hT, e).rearrange("(m p) t -> p m t", p=128)[:, :, csl]),
              writes=[("acc", c, m) for m in range(8)], dma=True)
        if even:
            P.add("gpsimd", lambda e, csl=csl: e.dma_start(out=mixT[:, 0:4, :], in_=attT.rearrange("(m p) t -> p m t", p=128)[:, :, csl]),
                  reads=xk, writes=[("mixT", m) for m in range(4)], dma=True)
            P.add("sync", lambda e, csl=csl: e.dma_start(out=ys[:], in_=ysT.rearrange("(m p) t -> p m t", p=128)[:, :, csl]),
                  reads=xk, writes=[("ys", m) for m in range(4)], dma=True)
            for m in range(4):
                b = m % 2
                P.add("scalar", lambda e, m=m, b=b: e.activation(out=yt[:, b, :], in_=ys[:, m, :], func=AF.Square),
                      reads=[("ys", m)], writes=[("yt", b)])
                P.add("vector", lambda e, b=b: e.tensor_scalar(out=yt[:, b, :], in0=yt[:, b, :], scalar1=0.044715, scalar2=1.0,
                                                               op0=ALU.mult, op1=ALU.add), reads=[("yt", b)], writes=[("yt", b)])
                P.add("vector", lambda e, m=m, b=b: e.tensor_tensor(out=yt[:, b, :], in0=yt[:, b, :], in1=ys[:, m, :], op=ALU.mult),
                      reads=[("yt", b), ("ys", m)], writes=[("yt", b)])
                P.add("scalar", lambda e, b=b: e.activation(out=yt[:, b, :], in_=yt[:, b, :], func=AF.Sigmoid, scale=1.5957691216),
                      reads=[("yt", b)], writes=[("yt", b)])
                P.add("vector", lambda e, m=m, b=b: e.tensor_tensor(out=ygb[:, m, :], in0=yt[:, b, :], in1=ys[:, m, :], op=ALU.mult),
                      reads=[("yt", b), ("ys", m)], writes=[("ygb", m)])
            for n in range(4):
                pb = po[n % 2]
                for k in range(4):
                    P.add("tensor", lambda e, n=n, k=k, pb=pb: e.matmul(pb[:], lhsT=wglu_sb[:, k, n * 128:(n + 1) * 128], rhs=ygb[:, k, :],
                                                                         start=(k == 0), stop=(k == 3)),
                          reads=[("ygb", k), "wglu"], writes=["po%d" % (n % 2)])
                b = n % 2
                P.add("scalar", lambda e, n=n, pb=pb, b=b: e.activation(out=yt[:, b, :], in_=pb[:], func=AF.Sigmoid, bias=bglu_sb[:, n:n + 1], scale=1.0),
                      reads=["po%d" % (n % 2), "bglu"], writes=[("yt", b)])
                P.add("vector", lambda e, n=n, b=b: e.tensor_tensor(out=mixT[:, 4 + n, :], in0=yt[:, b, :], in1=ygb[:, n, :], op=ALU.mult),
                      reads=[("yt", b), ("ygb", n)], writes=[("mixT", 4 + n)])
        else:
            P.add("gpsimd", lambda e, csl=csl: e.dma_start(out=mixT[:], in_=mixTd.rearrange("(m p) t -> p m t", p=128)[:, :, csl]),
                  reads=xk, writes=[("mixT", m) for m in range(8)], dma=True)
        for m in range(8):
            pb = [po[0], po[1], pg[0], pg[1], pu[0], pu[1]][m % 6]
            pk = ["po0", "po1", "pg0", "pg1", "pu0", "pu1"][m % 6]
            for k in range(8):
                P.add("tensor", lambda e, m=m, k=k, pb=pb: e.matmul(pb[:], lhsT=wout_sb[:, k, m * 128:(m + 1) * 128], rhs=mixT[:, k, :],
                                                                     start=(k == 0), stop=(k == 7)),
                      reads=[("mixT", k), "wout"], writes=[pk])
            P.add("vector", lambda e, m=m, pb=pb, accv=accv: e.scalar_tensor_tensor(out=r[:, m, :], in0=accv[:, m, :], scalar=DN_ALPHA, in1=pb[:],
                                                                                    op0=ALU.mult, op1=ALU.add),
                  reads=[("acc", c, m), pk], writes=[("r", m)])
        ln_feature_major(kb, "l1", r, lambda m: ("r", m), gcol(0), gcol(1), "lnc", onesf, pm, pq, tmp,
                         [(accv, lambda m, c=c: ("acc", c, m), "scalar", gcola(0), gcola(1)), (x1bv, lambda m, c=c: ("x1b", c, m), "vector")])

    def chunkA_router(c, accv):
        RK = "rt"
        for tt in range(4):
            tsl = slice(tt * 128, (tt + 1) * 128)
            for m in range(8):
                P.add("tensor", lambda e, m=m, tsl=tsl, tt=tt: e.matmul(pm[:, tt * 20:(tt + 1) * 20], lhsT=accv[:, m, tsl], rhs=wr_sb[:, m, :],
                                                                         start=(m == 0), stop=(m == 7)),
                      reads=[("acc", c, m), "wr"], writes=["pm"])
        B3 = lambda ap, n: ap.to_broadcast([128, 4, n])
        P.add("vector", lambda e: e.scalar_tensor_tensor(out=lgb[:], in0=pm[:, 0:80].rearrange("p (t n) -> p t n", t=4), scalar=1.0 / DN_ALPHA,
                                                         in1=br_sb[:].rearrange("p (o n) -> p o n", o=1).to_broadcast([128, 4, 20]), op0=ALU.mult, op1=ALU.add),
              reads=["pm", "br"], writes=[RK])
        P.add("vector", lambda e: e.tensor_reduce(out=gm[:], in_=lgb[:, :, 0:4], axis=AX.X, op=ALU.max), reads=[RK], writes=[RK])
        P.add("vector", lambda e: e.tensor_tensor(out=gk[:], in0=lgb[:, :, 0:4], in1=B3(gm[:].rearrange("p (t o) -> p t o", o=1), 4), op=ALU.is_equal),
              reads=[RK], writes=[RK])
        P.add("vector", lambda e: e.tensor_tensor(out=gx[:], in0=lgb[:, :, 0:4], in1=B3(gm[:].rearrange("p (t o) -> p t o", o=1), 4), op=ALU.subtract),
              reads=[RK], writes=[RK])
        P.add("scalar", lambda e: e.activation(out=gx[:], in_=gx[:], func=AF.Exp), reads=[RK], writes=[RK])
        P.add("vector", lambda e: e.tensor_reduce(out=gv[:], in_=gx[:], axis=AX.X, op=ALU.add), reads=[RK], writes=[RK])
        P.add("vector", lambda e: e.reciprocal(out=gv[:], in_=gv[:]), reads=[RK], writes=[RK])
        P.add("vector", lambda e: e.tensor_scalar(out=gk[:], in0=gk[:], scalar1=-1.0, scalar2=1e30, op0=ALU.add, op1=ALU.mult), reads=[RK], writes=[RK])
        P.add("vector", lambda e: e.tensor_tensor(out=em[:].rearrange("p t (g x) -> p t g x", g=4),
                                                  in0=lgb[:, :, 4:20].rearrange("p t (g x) -> p t g x", g=4),
                                                  in1=gk[:].rearrange("p t (g o) -> p t g o", o=1).to_broadcast([128, 4, 4, 4]), op=ALU.add),
              reads=[RK], writes=[RK])
        for tt in range(4):
            P.add("vector", lambda e, tt=tt: e.max(out=top[:, tt, :], in_=em[:, tt, :]), reads=[RK], writes=[RK])
        P.add("vector", lambda e: e.tensor_tensor(out=m1k[:], in0=em[:], in1=B3(top[:, :, 0:1], 16), op=ALU.is_equal), reads=[RK], writes=[RK])
        P.add("vector", lambda e: e.tensor_tensor(out=m2k[:], in0=em[:], in1=B3(top[:, :, 1:2], 16), op=ALU.is_equal), reads=[RK], writes=[RK])
        P.add("vector", lambda e: e.tensor_tensor(out=dd[:].rearrange("p (t o) -> p t o", o=1), in0=top[:, :, 1:2], in1=top[:, :, 0:1], op=ALU.subtract),
              reads=[RK], writes=[RK])
        P.add("scalar", lambda e: e.activation(out=dd[:], in_=dd[:], func=AF.Exp), reads=[RK], writes=[RK])
        P.add("vector", lambda e: e.tensor_scalar(out=w1[:], in0=dd[:], scalar1=1.0, scalar2=None, op0=ALU.add), reads=[RK], writes=[RK])
        P.add("vector", lambda e: e.reciprocal(out=w1[:], in_=w1[:]), reads=[RK], writes=[RK])
        P.add("vector", lambda e: e.tensor_tensor(out=w1[:], in0=w1[:], in1=gv[:], op=ALU.mult), reads=[RK], writes=[RK])
        P.add("vector", lambda e: e.tensor_tensor(out=w2[:], in0=w1[:], in1=dd[:], op=ALU.mult), reads=[RK], writes=[RK])
        P.add("vector", lambda e: e.tensor_tensor(out=m1k[:], in0=m1k[:], in1=B3(w1[:].rearrange("p (t o) -> p t o", o=1), 16), op=ALU.mult),
              reads=[RK], writes=[RK])
        P.add("vector", lambda e: e.tensor_tensor(out=m2k[:], in0=m2k[:], in1=B3(w2[:].rearrange("p (t o) -> p t o", o=1), 16), op=ALU.mult),
              reads=[RK], writes=[RK])
        P.add("vector", lambda e: e.tensor_tensor(out=combb[:], in0=m1k[:], in1=m2k[:], op=ALU.add), reads=[RK], writes=["combb"])
        for tt in range(4):
            P.add("tensor", lambda e, tt=tt: e.transpose(out=pq[0:16, tt * 128:(tt + 1) * 128], in_=combb[:, tt, :], identity=ident[:]),
                  reads=["combb", "ident"], writes=["pq"])
        P.add("scalar", lambda e: e.activation(out=combT_all[:, c * 512:(c + 1) * 512], in_=pq[0:16, 0:512], func=AF.Identity),
              reads=["pq"], writes=[("combT", c, i) for i in range(4)])

    chunkA(0, 0)
    for c in range(nch):
        if c + 1 < nch:
            chunkA(c + 1, 0)
        chunkA(c, 1)

    comb_d = kb.dint("comb_d", [16, nch * CH], F32)
    P.add("sync", lambda e: e.dma_start(out=comb_d[:, :], in_=combT_all[:]), reads=[("combT", c, i) for c in range(nch) for i in range(4)], dma=True)
    subphase()
    NWB = 4
    wgs = kb.sb("wgs", [128, NWB, 8, 256], BF16)
    wus = kb.sb("wus", [128, NWB, 8, 256], BF16)
    wds = kb.sb("wds", [128, NWB, 2, 1024], BF16)
    cbs = kb.sb("cbs", [128, 4, CH])
    sgs = kb.sb("sgs", [128, 2, CH])
    tts = kb.sb("tts", [128, 2, CH])
    hid2 = kb.sb("hid2", [128, 2, 2, CH], BF16)
    units = []
    for ex in range(16):
        wb = ex % NWB
        for c in range(nch):
            un = ex * nch + c
            hbuf = un % 2

            cbuf = un % 4

            def fGU(ex=ex, wb=wb, c=c, hbuf=hbuf, cbuf=cbuf):
                csl = slice(c * CH, (c + 1) * CH)
                if c == 0:
                    P.add("gpsimd", lambda e: e.dma_start(out=wgs[:, wb], in_=wg[ex].rearrange("(k p) n -> p k n", p=128)), writes=[("wgs", wb)], dma=True)
                    P.add("gpsimd", lambda e: e.dma_start(out=wus[:, wb], in_=wu[ex].rearrange("(k p) n -> p k n", p=128)), writes=[("wus", wb)], dma=True)
                    P.add("gpsimd", lambda e: e.dma_start(out=wds[:, wb], in_=wd[ex].rearrange("(fh p) d -> p fh d", p=128)), writes=[("wds", wb)], dma=True)
                P.add("sync", lambda e: e.dma_start(out=cbs[:, cbuf, :], in_=comb_d[ex:ex + 1, csl].to_broadcast([128, CH])), writes=[("cbs", cbuf)], dma=True)
                for fh in range(2):
                    pgb, pub = pg[fh], pu[fh]
                    for k in range(8):
                        P.add("tensor", lambda e, k=k, fh=fh, pgb=pgb: e.matmul(pgb[:], lhsT=wgs[:, wb, k, fh * 128:(fh + 1) * 128], rhs=x1b_all[:, c, k, :],
                                                                                 start=(k == 0), stop=(k == 7)),
                              reads=[("x1b", c, k), ("wgs", wb)], writes=["pg%d" % fh])
                    for k in range(8):
                        P.add("tensor", lambda e, k=k, fh=fh, pub=pub: e.matmul(pub[:], lhsT=wus[:, wb, k, fh * 128:(fh + 1) * 128], rhs=x1b_all[:, c, k, :],
                                                                                 start=(k == 0), stop=(k == 7)),
                              reads=[("x1b", c, k), ("wus", wb)], writes=["pu%d" % fh])
                    P.add("scalar", lambda e, fh=fh, pgb=pgb: e.activation(out=sgs[:, fh, :], in_=pgb[:], func=AF.Silu), reads=["pg%d" % fh], writes=[("sgs", fh)])
                    P.add("vector", lambda e, fh=fh, pub=pub: e.tensor_tensor(out=tts[:, fh, :], in0=sgs[:, fh, :], in1=pub[:], op=ALU.mult),
                          reads=[("sgs", fh), "pu%d" % fh], writes=[("tts", fh)])
                    P.add("gpsimd", lambda e, fh=fh: e.tensor_tensor(out=hid2[:, hbuf, fh, :], in0=tts[:, fh, :], in1=cbs[:, cbuf, :], op=ALU.mult),
                          reads=[("tts", fh), ("cbs", cbuf)], writes=[("hid2", hbuf, fh)])

            def fD(ex=ex, wb=wb, c=c, hbuf=hbuf):
                for m in range(8):
                    pb = [po[0], po[1], pm, pq][m % 4]
                    pk = ["po0", "po1", "pm", "pq"][m % 4]
                    for fh in range(2):
                        P.add("tensor", lambda e, m=m, fh=fh, pb=pb: e.matmul(pb[:], lhsT=wds[:, wb, fh, m * 128:(m + 1) * 128], rhs=hid2[:, hbuf, fh, :],
                                                                               start=(fh == 0), stop=(fh == 1)),
                              reads=[("hid2", hbuf, fh), ("wds", wb)], writes=[pk])
                    P.add("vector", lambda e, m=m, pb=pb: e.tensor_tensor(out=acc_all[:, c, m, :], in0=pb[:], in1=acc_all[:, c, m, :], op=ALU.add),
                          reads=[pk, ("acc", c, m)], writes=[("acc", c, m)])
            units.append((fGU, fD))
    for n in range(len(units)):
        if n == 0:
            units[0][0]()
        if n + 1 < len(units):
            units[n + 1][0]()
        units[n][1]()

    subphase()
    tmp = dict(sq=kb.sb("sq", [128, 2, CH]), mean=kb.sb("mean", [128, CH]), rstd=kb.sb("rstd", [128, CH]),
               t=kb.sb("lt", [128, 2, CH]), eps=eps)
    hTs = kb.sb("hTs", [128, 2, 8, CH])
    hbs = kb.sb("hbs", [128, 2, 8, CH], BF16)
    for c in range(nch):
        csl = slice(c * CH, (c + 1) * CH)
        ob = c % 2
        outs2 = [(hTs[:, ob], lambda m, ob=ob: ("hTs", ob, m), "scalar")]
        if hbT is not None:
            outs2.append((hbs[:, ob], lambda m, ob=ob: ("hbs", ob, m), "vector"))
        ln_feature_major(kb, "l2", acc_all[:, c], lambda m, c=c: ("acc", c, m), gcol(2), gcol(3), "lnc", onesf, pm, pq, tmp, outs2)
        P.add("sync", lambda e, csl=csl, ob=ob: e.dma_start(out=S_(houtT, e).rearrange("(m p) t -> p m t", p=128)[:, :, csl], in_=hTs[:, ob]),
              reads=[("hTs", ob, m) for m in range(8)], dma=True, out=out_final)
        if hbT is not None:
            t2, tl2 = c // 2, (c % 2) * CH
            P.add("sync", lambda e, ob=ob, t2=t2, tl2=tl2: e.dma_start(out=hbT[t2].rearrange("(m p) t -> p m t", p=128)[:, :, tl2:tl2 + CH], in_=hbs[:, ob]),
                  reads=[("hbs", ob, m) for m in range(8)], writes=[("hbd", c)], dma=True)
            if c % 2 == 1 and "hb_cb" in kb.over:
                kb.over["hb_cb"](t2, [("hbd", c - 1), ("hbd", c)])


def build_post(even, tok=TOK):
    kb = KB()
    make_ident(kb)
    kb.mark()
    emit_post(kb, even, tok)
    print("post ops", kb.P.nops(), "peak words", kb.peak)
    return kb.finish()

def sincos(P, x, sin_o, cos_o, t1, t2, ti, rd, wr_s, wr_c, K):
    P.add("vector", lambda e: e.tensor_scalar(out=t1, in0=x, scalar1=1.0 / (2 * PI), scalar2=None, op0=ALU.mult), reads=list(rd) + [K], writes=[K])
    P.add("vector", lambda e: e.tensor_copy(out=ti, in_=t1), reads=[K], writes=[K])
    P.add("vector", lambda e: e.tensor_copy(out=t1, in_=ti), reads=[K], writes=[K])
    P.add("vector", lambda e: e.scalar_tensor_tensor(out=t1, in0=t1, scalar=-2 * PI, in1=x, op0=ALU.mult, op1=ALU.add), reads=list(rd) + [K], writes=[K])
    P.add("scalar", lambda e: e.activation(out=t2, in_=t1, func=AF.Sin, scale=0.25), reads=[K], writes=[K])
    P.add("scalar", lambda e: e.activation(out=t1, in_=t1, func=AF.Sin, scale=0.5), reads=[K], writes=[K])
    P.add("vector", lambda e: e.tensor_tensor(out=t2, in0=t2, in1=t2, op=ALU.mult), reads=[K], writes=[K])
    P.add("vector", lambda e: e.tensor_scalar(out=t2, in0=t2, scalar1=-2.0, scalar2=1.0, op0=ALU.mult, op1=ALU.add), reads=[K], writes=[K])
    P.add("vector", lambda e: e.scalar_tensor_tensor(out=sin_o, in0=t1, scalar=2.0, in1=t2, op0=ALU.mult, op1=ALU.mult), reads=[K], writes=list(wr_s) + [K])
    P.add("vector", lambda e: e.tensor_tensor(out=t1, in0=t1, in1=t1, op=ALU.mult), reads=[K], writes=[K])
    P.add("vector", lambda e: e.tensor_scalar(out=cos_o, in0=t1, scalar1=-2.0, scalar2=1.0, op0=ALU.mult, op1=ALU.add), reads=[K], writes=list(wr_c) + [K])


def emit_even(kb, S=4096, mode='fox'):
    FOX = (mode == 'fox'); S5 = not FOX
    nc, P = kb.nc, kb.P
    fm = ('attT_d' in kb.over) if FOX else ('ysT_d' in kb.over)
    HS = S // 2; NH = (S // 512) // 2
    NCk = S // 512
    NT = S // 128
    hT = kb.din("hT", [1024, S])
    if FOX:
        wq = kb.din("wq", [1024, 256]); wk = kb.din("wk", [1024, 256]); wv = kb.din("wv", [1024, 256])
        wf = kb.din("wf", [1024, 4])
        fbias = kb.din("fbias", [4, 1])
    else:
        wu = kb.din("wu", [1024, 256])
        are = kb.din("are", [128, 8]); aim = kb.din("aim", [128, 8]); ldt = kb.din("ldt", [128, 8])
        bre = kb.din("bre", [128, 8, 16]); bim = kb.din("bim", [128, 8, 16])
        cre = kb.din("cre", [128, 8, 16]); cim = kb.din("cim", [128, 8, 16])
        dsk = kb.din("dsk", [128, 2])
        jrow_d = kb.din("jrow", [128, 512])
    att = kb.dout("att", [S, 256]) if (FOX and not fm) else None
    ysT = kb.dout("ysT", [256, S]) if (S5 and not fm) else None
    attT_d = kb.over.get("attT_d"); ysT_d = kb.over.get("ysT_d")

    ident = make_ident(kb)
    hTb = kb.sb("hTb", [128, 2, 8, 512], BF16)
    if FOX:
        wq_sb = kb.sb("wq_sb", [128, 8, 256], BF16); wk_sb = kb.sb("wk_sb", [128, 8, 256], BF16)
        wv_sb = kb.sb("wv_sb", [128, 8, 256], BF16)
        wf_sb = kb.sb("wf_sb", [128, 8, 4], BF16)
        fb_sb = kb.sb("fb_sb", [4, 1])
        wl = ((wq_sb, wq, "wq"), (wk_sb, wk, "wk"), (wv_sb, wv, "wv"), (wf_sb, wf, "wf"))
    else:
        wu_sb = kb.sb("wu_sb", [128, 8, 256], BF16)
        wl = ((wu_sb, wu, "wu"),)
    if FOX:
        QA = kb.sb("QA", [65, 4, S], BF16)
        KA = kb.sb("KA", [65, 4, S], BF16)
        V = kb.sb("V", [128, NT, 4, 72], BF16)
        fl = kb.sb("fl", [4, S])
        cc = fl
        ones4 = kb.sb("ones4", [4, 512])
    else:
        uTb = kb.sb("uTb", [128, 2, S], BF16)
    selc = kb.sb("selc", [4, 4, 65])
    negc = kb.sb("negc", [128, NT, 4])
    PT = kb.sb("PT", [128, 3, 512], BF16)
    ost = kb.sb("ost", [128, 2, 256])
    rec = kb.sb("rec", [128, 4])
    otmp = kb.sb("otmp", [128, 4, 65])
    attst = kb.sb("attst", [64, 2, 512], BF16)
    pA = kb.ps("pA", [128, 512]); pB = kb.ps("pB", [128, 512])
    pS = [kb.ps("pS0", [128, 512]), kb.ps("pS1", [128, 512])]
    pO = [kb.ps("pO0", [128, 512]), kb.ps("pO1", [128, 512])]
    pC = kb.ps("pC", [128, 512]); pD = kb.ps("pD", [128, 512])

    for (wsb, wdr, nm) in wl:
        P.add("gpsimd", lambda e, wsb=wsb, wdr=wdr: e.dma_start(out=wsb[:], in_=wdr.rearrange("(k p) n -> p k n", p=128)), writes=[nm], dma=True)
    if FOX:
        P.add("sync", lambda e: e.dma_start(out=fb_sb[:], in_=fbias[:, :]), writes=["fb"], dma=True)
        P.add("vector", lambda e: e.memset(ones4[:], 1.0), writes=["ones4"])
        P.add("vector", lambda e: e.memset(KA[64:65, :, :], 1.0), writes=["KA1"])
        P.add("vector", lambda e: e.memset(V[:, :, :, 64:65], 1.0), writes=["V1"])
    P.add("gpsimd", lambda e: e.memset(selc[:], 1.0), writes=["selc"])
    P.add("gpsimd", lambda e: e.affine_select(out=selc[:], in_=selc[:], pattern=[[-1, 4], [0, 65]], compare_op=ALU.is_equal, fill=0.0, base=0,
                                              channel_multiplier=1), reads=["selc"], writes=["selc"])
    P.add("gpsimd", lambda e: e.affine_select(out=selc[:], in_=selc[:], pattern=[[0, 4], [1, 65]], compare_op=ALU.is_equal, fill=0.0, base=-64,
                                              channel_multiplier=0), reads=["selc"], writes=["selc"])

    for c in range(NCk):
        hb = c % 2
        csl = slice(c * 512, (c + 1) * 512)
        load_hT(kb, hTb, hb, c, hT)
        if FOX:
            for h in range(4):
                for (wsb, wn, dst, pb, pk, sc) in ((wq_sb, "wq", QA, [pA, pS[0]][h % 2], ["pA", "pS0"][h % 2], 0.125),
                                                   (wk_sb, "wk", KA, [pB, pS[1]][h % 2], ["pB", "pS1"][h % 2], 1.0)):
                    for k in range(8):
                        P.add("tensor", lambda e, k=k, h=h, wsb=wsb, pb=pb, hb=hb: e.matmul(pb[0:64, :], lhsT=wsb[:, k, h * 64:(h + 1) * 64], rhs=hTb[:, hb, k, :],
                                                                                           start=(k == 0), stop=(k == 7)),
                              reads=[("hTb", hb), wn], writes=[pk])
                    P.add("scalar", lambda e, h=h, dst=dst, pb=pb, sc=sc, csl=csl: e.activation(out=dst[0:64, h, csl], in_=pb[0:64, :], func=AF.Copy, scale=sc),
                          reads=[pk], writes=[(wn + "o", h, c)])
            for k in range(8):
                P.add("tensor", lambda e, k=k, hb=hb: e.matmul(pD[0:4, :], lhsT=wf_sb[:, k, :], rhs=hTb[:, hb, k, :], start=(k == 0), stop=(k == 7)),
                      reads=[("hTb", hb), "wf"], writes=["pD"])
            P.add("scalar", lambda e, csl=csl: e.activation(out=fl[:, csl], in_=pD[0:4, :], func=AF.Identity, bias=fb_sb[:], scale=1.0),
                  reads=["pD", "fb"], writes=[("fl", c)])
            for t4 in range(4):
                t = c * 4 + t4
                for k in range(8):
                    P.add("tensor", lambda e, k=k, t4=t4, hb=hb: e.matmul(pD[:, 256:512], lhsT=hTb[:, hb, k, t4 * 128:(t4 + 1) * 128], rhs=wv_sb[:, k, :],
                                                                           start=(k == 0), stop=(k == 7)),
                          reads=[("hTb", hb), "wv"], writes=["pD"])
                P.add("vector", lambda e, t=t: e.tensor_copy(out=V[:, t, :, 0:64], in_=pD[:, 256:512].rearrange("p (h d) -> p h d", h=4)),
                      reads=["pD", "V1"], writes=[("V", t)])
        else:
            for ut in range(2):
                for k in range(8):
                    P.add("tensor", lambda e, k=k, ut=ut, hb=hb: e.matmul(pC[:], lhsT=wu_sb[:, k, ut * 128:(ut + 1) * 128], rhs=hTb[:, hb, k, :],
                                                                           start=(k == 0), stop=(k == 7)),
                          reads=[("hTb", hb), "wu"], writes=["pC"])
                P.add("vector", lambda e, ut=ut, csl=csl: e.tensor_copy(out=uTb[:, ut, csl], in_=pC[:]), reads=["pC"], writes=[("uTb", ut, c)])
    STAGE = 9; KK = 65
    if FOX and STAGE >= 2:
        flk = [("fl", c) for c in range(NCk)]
        P.add("scalar", lambda e: e.activation(out=fl[:], in_=fl[:], func=AF.Exp, scale=-1.0), reads=flk, writes=["fl2"])
        P.add("scalar", lambda e: e.activation(out=fl[:], in_=fl[:], func=AF.Ln, bias=1.0, scale=1.0), reads=["fl2"], writes=["fl3"])
        P.add("vector", lambda e: e.tensor_scalar(out=fl[:], in0=fl[:], scalar1=-1.0, scalar2=None, op0=ALU.mult), reads=["fl3"], writes=["fl4"])
        for c in range(NCk):
            csl = slice(c * 512, (c + 1) * 512)
            ini = 0.0 if c == 0 else cc[:, c * 512 - 1:c * 512]
            P.add("vector", lambda e, csl=csl, ini=ini: e.tensor_tensor_scan(out=cc[:, csl], data0=ones4[:], data1=fl[:, csl], initial=ini, op0=ALU.mult, op1=ALU.add),
                  reads=["fl4", "ones4", "cc"], writes=["cc"])
        for c in range(NCk):
            csl = slice(c * 512, (c + 1) * 512)
            for h in range(4):
                P.add("tensor", lambda e, h=h, csl=csl: e.matmul(pA[0:65, :], lhsT=selc[:, h, :], rhs=cc[:, csl], start=True, stop=True),
                      reads=["cc", "selc"], writes=["pA"])
                P.add("scalar", lambda e, h=h, csl=csl: e.activation(out=QA[64:65, h, csl], in_=pA[64:65, :], func=AF.Copy),
                      reads=["pA"], writes=[("QAc", h, c)])
        for t in range(NT):
            P.add("tensor", lambda e, t=t: e.transpose(out=pB[:, t * 4:(t + 1) * 4], in_=cc[:, t * 128:(t + 1) * 128], identity=ident[0:4, 0:4]),
                  reads=["cc", "ident"], writes=["pB"])
        P.add("scalar", lambda e: e.activation(out=negc[:].rearrange("p t h -> p (t h)"), in_=pB[:, 0:NT * 4], func=AF.Copy, scale=-1.0),
              reads=["pB"], writes=["negc"])

        cnt = 0
        tiles = []
        for h in range(4 if STAGE >= 3 else 0):
            for j in range(NCk):
                qk_reads = [("wqo", h, j), ("QAc", h, j)]
                for i in range(4 * j + 4):
                    r = i - 4 * j
                    q0 = 128 * r if r > 0 else 0
                    sb_ = cnt % 3
                    cnt += 1
                    ps = [pS[0], pS[1], pB][sb_]
                    def fS(h=h, i=i, j=j, q0=q0, ps=ps, sb_=sb_, qk_reads=qk_reads):
                        P.add("tensor", lambda e, h=h, i=i, j=j, q0=q0, ps=ps: e.matmul(ps[:, q0:512], lhsT=KA[0:KK, h, i * 128:(i + 1) * 128],
                                                                                        rhs=QA[0:KK, h, j * 512 + q0:(j + 1) * 512], start=True, stop=True),
                              reads=qk_reads + [("wko", h, i // 4), "KA1"], writes=[["pS0", "pS1", "pB"][sb_]])
                    def fR(h=h, i=i, j=j, r=r, q0=q0, ps=ps, sb_=sb_):
                        P.add("scalar", lambda e, h=h, i=i, q0=q0, ps=ps, sb_=sb_: e.activation(out=PT[:, sb_, q0:512], in_=ps[:, q0:512], func=AF.Exp,
                                                                                               bias=negc[:, i, h:h + 1], scale=1.0),
                              reads=[["pS0", "pS1", "pB"][sb_], "negc"], writes=[("PT", sb_)])
                        AL = 9
                        if r >= 0 and AL >= 2:
                            P.add("gpsimd", lambda e, sb_=sb_, q0=q0: e.affine_select(out=PT[:, sb_, q0:q0 + 128], in_=PT[:, sb_, q0:q0 + 128], pattern=[[1, 128]],
                                                                                       compare_op=ALU.is_ge, fill=0.0, base=0, channel_multiplier=-1),
                                  reads=[("PT", sb_)], writes=[("PT", sb_)])
                        for u in range(max(r, 0), 4 if AL >= 3 else 0):
                            po_ = [pO[0], pO[1], pC, pD][u]
                            o0 = 0
                            okey = [('pO', 0), ('pO', 1), 'pC', 'pD'][u]
                            P.add("tensor", lambda e, h=h, i=i, u=u, sb_=sb_, po_=po_, o0=o0, j=j: e.matmul(po_[:, o0:o0 + 65], lhsT=PT[:, sb_, u * 128:(u + 1) * 128],
                                                                                                             rhs=V[:, i, h, 0:65], start=(i == 0), stop=(i == 4 * j + u)),
                                  reads=[("PT", sb_), ("V", i), "V1"], writes=[okey])
                            if i == 4 * j + u and AL >= 4:
                                ob = (j * 4 + u) % 2
                                tt = j * 4 + u
                                P.add("vector", lambda e, po_=po_, o0=o0, u=u: e.tensor_copy(out=otmp[:, u, :], in_=po_[:, o0:o0 + 65]),
                                      reads=[okey], writes=[("otmp", u)])
                                P.add("vector", lambda e, u=u: e.reciprocal(out=rec[:, u:u + 1], in_=otmp[:, u, 64:65]),
                                      reads=[("otmp", u)], writes=[("rec", u)])
                                P.add("vector", lambda e, u=u, ob=ob, h=h: e.tensor_scalar(out=ost[:, ob, h * 64:(h + 1) * 64], in0=otmp[:, u, 0:64],
                                                                                         scalar1=rec[:, u:u + 1], scalar2=None, op0=ALU.mult),
                                      reads=[("otmp", u), ("rec", u)], writes=[("ost", ob, h)])
                                if not fm:
                                    P.add("sync", lambda e, ob=ob, h=h, tt=tt: e.dma_start(out=att[tt * 128:(tt + 1) * 128, h * 64:(h + 1) * 64], in_=ost[:, ob, h * 64:(h + 1) * 64]),
                                          reads=[("ost", ob, h)], dma=True, out=True)
                                else:
                                    def fin(h=h, j=j, u=u, ob=ob):
                                        stb = (h * NCk + j) % 2
                                        P.add("tensor", lambda e, ob=ob, h=h, u=u: e.transpose(out=pA[0:64, u * 128:(u + 1) * 128], in_=ost[:, ob, h * 64:(h + 1) * 64], identity=ident[:]),
                                              reads=[("ost", ob, h), "ident"], writes=["pA"])
                                        P.add("scalar", lambda e, u=u, stb=stb: e.activation(out=attst[:, stb, u * 128:(u + 1) * 128], in_=pA[0:64, u * 128:(u + 1) * 128], func=AF.Copy),
                                              reads=["pA"], writes=[("attst", stb)])
                                        if u == 3:
                                            half, tl = j // NH, (j % NH) * 512
                                            P.add("sync", lambda e, stb=stb, h=h, half=half, tl=tl: e.dma_start(out=attT_d[half, h * 64:(h + 1) * 64, tl:tl + 512], in_=attst[:, stb, :]),
                                                  reads=[("attst", stb)], dma=True)
                                    deferred.append(fin)
                    tiles.append((fS, fR))
        deferred = []
        for n in range(len(tiles)):
            if n == 0:
                tiles[0][0]()
                if len(tiles) > 1:
                    tiles[1][0]()
            if n + 2 < len(tiles):
                tiles[n + 2][0]()
            pend = list(deferred)
            del deferred[:]
            tiles[n][1]()
            for f in pend:
                f()
        for f in deferred:
            f()

    if S5:
        prm = kb.sb("prm", [128, 24, 8])
        prmi = kb.sb("prmi", [128, 1, 8], I32)
        angi = kb.sb("angi", [128, 512], I32)
        bb = kb.sb("bb", [128, 2, 8, 16])
        cs = kb.sb("cs", [128, 2, 8, 16])
        btmp = kb.sb("btmp", [128, 2, 16])
        Xp = kb.sb("Xp", [128, 2, 128])
        Blhs = kb.sb("Blhs", [128, 8, 2, 128], BF16)
        Cl = kb.sb("Cl", [128, 8, 2, 128], BF16)
        Dl = kb.sb("Dl", [128, 2, 128], BF16)
        dsk_sb = kb.sb("dsk_sb", [128, 2])
        jrow = kb.sb("jrow_sb", [128, 512])
        cosT = kb.sb("cosT", [128, 8, 512]); sinT = kb.sb("sinT", [128, 8, 512]); rT = kb.sb("rT", [128, 8, 512])
        ang = kb.sb("ang", [128, 512])
        wsets = [[kb.sb("w%d_%d" % (i, u), [128, 512]) for i in range(6)] for u in range(3)]
        w = wsets[0]
        sb2s = [[kb.sb("sre%d" % u, [128, 512]), kb.sb("sim%d" % u, [128, 512])] for u in range(3)]
        wb4s = [[kb.sb("wb4_%d_%d" % (u, q), [128, 512], BF16) for q in range(4)] for u in range(3)]
        Cln = kb.sb("Cln", [128, 8, 2, 128], BF16)
        init = kb.sb("init", [128, 8, 2])
        yst = kb.sb("yst", [128, 2, 512])
        pi_c = kb.sb("pi_c", [128, 1])

        def col(i):
            return prm[:, i, :]
        for (i, dr) in ((0, are), (1, aim), (2, ldt)):
            P.add("sync", lambda e, i=i, dr=dr: e.dma_start(out=prm[:, i, :], in_=dr[:, :]), writes=[("prm", i)], dma=True)
        P.add("sync", lambda e: e.dma_start(out=bb[:, 0], in_=bre[:, :, :]), writes=["bre"], dma=True)
        P.add("sync", lambda e: e.dma_start(out=bb[:, 1], in_=bim[:, :, :]), writes=["bim"], dma=True)
        P.add("sync", lambda e: e.dma_start(out=cs[:, 0], in_=cre[:, :, :]), writes=["cre"], dma=True)
        P.add("sync", lambda e: e.dma_start(out=cs[:, 1], in_=cim[:, :, :]), writes=["cim"], dma=True)
        P.add("sync", lambda e: e.dma_start(out=dsk_sb[:], in_=dsk[:, :]), writes=["dsk"], dma=True)
        P.add("sync", lambda e: e.dma_start(out=jrow[:], in_=jrow_d[:, :]), writes=["jrow"], dma=True)
        P.add("vector", lambda e: e.memset(pi_c[:], -PI), writes=["pi_c"])
        P.add("vector", lambda e: e.memset(init[:], 0.0), writes=[("init", pr) for pr in range(8)])
        K = "prmall"
        A = lambda fn, rd=(), eng="vector": P.add(eng, fn, reads=list(rd) + [K], writes=[K])
        TT = lambda o, a, b, op: A(lambda e: e.tensor_tensor(out=col(o), in0=col(a), in1=col(b), op=op))
        P.add("scalar", lambda e: e.activation(out=col(3), in_=col(2), func=AF.Exp), reads=[("prm", 0), ("prm", 1), ("prm", 2)], writes=[K])
        TT(4, 0, 3, ALU.mult)
        TT(5, 1, 3, ALU.mult)
        A(lambda e: e.activation(out=col(6), in_=col(4), func=AF.Exp), eng="scalar")
        sincos(P, col(5), col(7), col(8), col(20), col(21), prmi[:, 0, :], [K], [K], [K], K)
        TT(9, 6, 8, ALU.mult)
        TT(10, 6, 7, ALU.mult)
        A(lambda e: e.tensor_scalar(out=col(11), in0=col(9), scalar1=-1.0, scalar2=None, op0=ALU.add))
        TT(12, 0, 0, ALU.mult)
        TT(13, 1, 1, ALU.mult)
        TT(12, 12, 13, ALU.add)
        A(lambda e: e.reciprocal(out=col(12), in_=col(12)))
        TT(13, 11, 0, ALU.mult); TT(14, 10, 1, ALU.mult); TT(13, 13, 14, ALU.add); TT(13, 13, 12, ALU.mult)
        TT(14, 10, 0, ALU.mult); TT(15, 11, 1, ALU.mult); TT(14, 14, 15, ALU.subtract); TT(14, 14, 12, ALU.mult)
        A(lambda e: e.tensor_scalar(out=col(15), in0=col(14), scalar1=-1.0, scalar2=None, op0=ALU.mult))
        A(lambda e: e.tensor_scalar(out=col(16), in0=col(5), scalar1=512.0, scalar2=None, op0=ALU.mult))
        sincos(P, col(16), col(17), col(18), col(20), col(21), prmi[:, 0, :], [K], [K], [K], K)
        A(lambda e: e.tensor_scalar(out=col(19), in0=col(17), scalar1=-1.0, scalar2=None, op0=ALU.mult))
        for pr in range(8):
            zr = prm[:, 13, pr:pr + 1]; zi = prm[:, 14, pr:pr + 1]; nzi = prm[:, 15, pr:pr + 1]
            P.add("vector", lambda e, pr=pr, zr=zr: e.tensor_scalar(out=btmp[:, 0, :], in0=bb[:, 0, pr, :], scalar1=zr, scalar2=None, op0=ALU.mult),
                  reads=[K, "bre"], writes=["btmp0"])
            P.add("vector", lambda e, pr=pr, zi=zi: e.tensor_scalar(out=btmp[:, 1, :], in0=bb[:, 0, pr, :], scalar1=zi, scalar2=None, op0=ALU.mult),
                  reads=[K, "bre"], writes=["btmp1"])
            P.add("vector", lambda e, pr=pr, nzi=nzi: e.scalar_tensor_tensor(out=bb[:, 0, pr, :], in0=bb[:, 1, pr, :], scalar=nzi, in1=btmp[:, 0, :], op0=ALU.mult, op1=ALU.add),
                  reads=[K, "bim", "btmp0", "bre", "btmp1"], writes=["bre"])
            P.add("vector", lambda e, pr=pr, zr=zr: e.scalar_tensor_tensor(out=bb[:, 1, pr, :], in0=bb[:, 1, pr, :], scalar=zr, in1=btmp[:, 1, :], op0=ALU.mult, op1=ALU.add),
                  reads=[K, "bim", "btmp1", "bre"], writes=["bim"])
        P.add("gpsimd", lambda e: e.memset(Cl[:], 0.0), writes=["Cl"])
        for pr in range(8):
            pi_ = pr % 4
            for part in range(2):
                xb = part
                P.add("gpsimd", lambda e, xb=xb: e.memset(Xp[:, xb, :], 0.0), writes=[("Xp", xb)])
                P.add("vector", lambda e, xb=xb, pr=pr, part=part, pi_=pi_: e.tensor_copy(out=Xp[0:64, xb, 32 * pi_:32 * pi_ + 16], in_=bb[0:64, part, pr, :]),
                      reads=["bre", "bim", ("Xp", xb)], writes=[("Xp", xb)])
                P.add("vector", lambda e, xb=xb, pr=pr, part=part, pi_=pi_: e.tensor_copy(out=Xp[64:128, xb, 32 * pi_ + 16:32 * pi_ + 32], in_=bb[64:128, part, pr, :]),
                      reads=["bre", "bim", ("Xp", xb)], writes=[("Xp", xb)])
                P.add("tensor", lambda e, xb=xb: e.transpose(out=pD[:, xb * 128:(xb + 1) * 128], in_=Xp[:, xb, :], identity=ident[:]),
                      reads=[("Xp", xb), "ident"], writes=[("pDx", xb)])
                P.add("scalar", lambda e, xb=xb, pr=pr, part=part: e.activation(out=Blhs[:, pr, part, :], in_=pD[:, xb * 128:(xb + 1) * 128], func=AF.Copy),
                      reads=[("pDx", xb)], writes=[("Blhs", pr)])
                P.add("vector", lambda e, pr=pr, part=part, pi_=pi_: e.tensor_copy(out=Cl[0:64, pr, part, 32 * pi_:32 * pi_ + 16], in_=cs[0:64, part, pr, :]),
                      reads=["cre", "cim", "Cl"], writes=["Cl"])
                P.add("vector", lambda e, pr=pr, part=part, pi_=pi_: e.tensor_copy(out=Cl[64:128, pr, part, 32 * pi_ + 16:32 * pi_ + 32], in_=cs[64:128, part, pr, :]),
                      reads=["cre", "cim", "Cl"], writes=["Cl"])
        P.add("vector", lambda e: e.tensor_scalar(out=Cln[:].rearrange("p a b c -> p (a b c)"), in0=Cl[:].rearrange("p a b c -> p (a b c)"),
                                                  scalar1=-1.0, scalar2=None, op0=ALU.mult), reads=["Cl"], writes=["Cl"])
        for ut in range(2):
            P.add("vector", lambda e, ut=ut: e.tensor_scalar(out=Dl[:, ut, :], in0=ident[:], scalar1=dsk_sb[:, ut:ut + 1], scalar2=None, op0=ALU.mult),
                  reads=["ident", "dsk"], writes=["Dl"])
        for pr in range(8):
            th = prm[:, 5, pr:pr + 1]
            P.add("vector", lambda e, th=th: e.tensor_scalar(out=ang[:], in0=jrow[:], scalar1=th, scalar2=None, op0=ALU.mult), reads=["jrow", K], writes=["ang"])
            sincos(P, ang[:], sinT[:, pr, :], cosT[:, pr, :], w[0][:], w[1][:], angi[:], ["ang"], [("sinT", pr)], [("cosT", pr)], "w01")
            P.add("gpsimd", lambda e, pr=pr: e.memset(rT[:, pr, :], 1.0), writes=[("rT", pr)])
            P.add("gpsimd", lambda e, pr=pr: e.tensor_scalar(out=rT[:, pr, :], in0=rT[:, pr, :], scalar1=prm[:, 6, pr:pr + 1], scalar2=None, op0=ALU.mult),
                  reads=[("rT", pr), K], writes=[("rT", pr)])
        def s5_unit(c, ut, pi_, pr, csl, w, sb2, wb4, pbr, pbi, kbr, kbi, ws, ub):
            sre, sim = sb2
            P.add("tensor", lambda e: e.matmul(pbr[:], lhsT=Blhs[:, pr, 0, :], rhs=uTb[:, ut, csl], start=True, stop=True),
                  reads=[("Blhs", pr), ("uTb", ut, c)], writes=[kbr])
            P.add("tensor", lambda e: e.matmul(pbi[:], lhsT=Blhs[:, pr, 1, :], rhs=uTb[:, ut, csl], start=True, stop=True),
                  reads=[("Blhs", pr), ("uTb", ut, c)], writes=[kbi])
            P.add("scalar", lambda e: e.activation(out=sre[:], in_=pbr[:], func=AF.Copy), reads=[kbr], writes=[("sre", ub)])
            P.add("scalar", lambda e: e.activation(out=sim[:], in_=pbi[:], func=AF.Copy), reads=[kbi], writes=[("sim", ub)])
            ck, sk = ("cosT", pr), ("sinT", pr)
            P.add("vector", lambda e: e.tensor_tensor(out=w[0][:], in0=cosT[:, pr, :], in1=sre[:], op=ALU.mult), reads=[ck, ("sre", ub)], writes=[ws[0]])
            P.add("vector", lambda e: e.tensor_tensor(out=w[1][:], in0=sinT[:, pr, :], in1=sim[:], op=ALU.mult), reads=[sk, ("sim", ub)], writes=[ws[1]])
            P.add("vector", lambda e: e.tensor_tensor(out=w[2][:], in0=cosT[:, pr, :], in1=sim[:], op=ALU.mult), reads=[ck, ("sim", ub)], writes=[ws[2]])
            P.add("vector", lambda e: e.tensor_tensor(out=w[3][:], in0=sinT[:, pr, :], in1=sre[:], op=ALU.mult), reads=[sk, ("sre", ub)], writes=[ws[3]])
            P.add("vector", lambda e: e.tensor_tensor(out=w[0][:], in0=w[0][:], in1=w[1][:], op=ALU.add), reads=[ws[0], ws[1]], writes=[ws[0]])
            P.add("vector", lambda e: e.tensor_tensor(out=w[2][:], in0=w[2][:], in1=w[3][:], op=ALU.subtract), reads=[ws[2], ws[3]], writes=[ws[2]])
            P.add("vector", lambda e: e.tensor_tensor_scan(out=w[4][:], data0=rT[:, pr, :], data1=w[0][:], initial=init[:, pr, 0:1], op0=ALU.mult, op1=ALU.add),
                  reads=[("rT", pr), ws[0], ("init", pr)], writes=[ws[4]])
            P.add("vector", lambda e: e.tensor_tensor_scan(out=w[5][:], data0=rT[:, pr, :], data1=w[2][:], initial=init[:, pr, 1:2], op0=ALU.mult, op1=ALU.add),
                  reads=[("rT", pr), ws[2], ("init", pr)], writes=[ws[5]])
            cL = prm[:, 18, pr:pr + 1]; sL = prm[:, 17, pr:pr + 1]; nsL = prm[:, 19, pr:pr + 1]
            P.add("vector", lambda e: e.tensor_scalar(out=init[:, pr, 0:1], in0=w[4][:, 511:512], scalar1=cL, scalar2=None, op0=ALU.mult),
                  reads=[ws[4], K, ("init", pr)], writes=[("init", pr)])
            P.add("vector", lambda e: e.scalar_tensor_tensor(out=init[:, pr, 0:1], in0=w[5][:, 511:512], scalar=nsL, in1=init[:, pr, 0:1], op0=ALU.mult, op1=ALU.add),
                  reads=[ws[5], K, ("init", pr)], writes=[("init", pr)])
            P.add("vector", lambda e: e.tensor_scalar(out=init[:, pr, 1:2], in0=w[4][:, 511:512], scalar1=sL, scalar2=None, op0=ALU.mult),
                  reads=[ws[4], K, ("init", pr)], writes=[("init", pr)])
            P.add("vector", lambda e: e.scalar_tensor_tensor(out=init[:, pr, 1:2], in0=w[5][:, 511:512], scalar=cL, in1=init[:, pr, 1:2], op0=ALU.mult, op1=ALU.add),
                  reads=[ws[5], K, ("init", pr)], writes=[("init", pr)])
            srcs = ((cosT, ck, 4), (sinT, sk, 5), (sinT, sk, 4), (cosT, ck, 5))
            lhs = ((Cl, 0), (Cln, 0), (Cln, 1), (Cln, 1))
            for q in range(4):
                tab, tk, wi = srcs[q]
                P.add("gpsimd", lambda e, q=q, tab=tab, wi=wi: e.tensor_tensor(out=wb4[q][:], in0=tab[:, pr, :], in1=w[wi][:], op=ALU.mult),
                      reads=[tk, ws[wi]], writes=[("wb4", ub, q)])
            for q in range(4):
                ct, part = lhs[q]
                P.add("tensor", lambda e, q=q, ct=ct, part=part: e.matmul(pC[:], lhsT=ct[:, pr, part, :], rhs=wb4[q][:], start=(pi_ == 0 and q == 0), stop=False),
                      reads=["Cl", ("wb4", ub, q)], writes=["pC"])
        for c in range(NCk):
            csl = slice(c * 512, (c + 1) * 512)
            for ut in range(2):
                for pi_ in range(4):
                    pr = ut * 4 + pi_
                    ub = (c * 8 + pr) % 3
                    s5_unit(c, ut, pi_, pr, csl, wsets[ub], sb2s[ub], wb4s[ub], [pA, pS[0], pO[0]][ub], [pB, pS[1], pO[1]][ub],
                            ["pA", "pS0", ("pO", 0)][ub], ["pB", "pS1", ("pO", 1)][ub], ["w%d_%d" % (i, ub) for i in range(6)], ub)
                P.add("tensor", lambda e, ut=ut, csl=csl: e.matmul(pC[:], lhsT=Dl[:, ut, :], rhs=uTb[:, ut, csl], start=False, stop=True),
                      reads=["Dl", ("uTb", ut, c)], writes=["pC"])
                ob = (c * 2 + ut) % 2
                P.add("scalar", lambda e, ob=ob: e.activation(out=yst[:, ob, :], in_=pC[:], func=AF.Copy), reads=["pC"], writes=[("yst", ob)])
                if not fm:
                    P.add("sync", lambda e, ob=ob, ut=ut, csl=csl: e.dma_start(out=ysT[ut * 128:(ut + 1) * 128, csl], in_=yst[:, ob, :]),
                          reads=[("yst", ob)], dma=True, out=True)
                else:
                    half, tl = c // NH, (c % NH) * 512
                    P.add("sync", lambda e, ob=ob, ut=ut, half=half, tl=tl: e.dma_start(out=ysT_d[half, ut * 128:(ut + 1) * 128, tl:tl + 512], in_=yst[:, ob, :]),
                          reads=[("yst", ob)], writes=[("ysd", c, ut)], dma=True)
                    if ut == 1 and (c + 1) % NH == 0 and "ys_cb" in kb.over:
                        kb.over["ys_cb"](half, [("ysd", cc, u) for cc in range(half * NH, (half + 1) * NH) for u in range(2)])


def build_even(S=4096, mode='fox'):
    kb = KB()
    make_ident(kb)
    kb.mark()
    emit_even(kb, S, mode)
    print("even ops", kb.P.nops(), "peak words", kb.peak)
    return kb.finish()

import math
def emit_odd(kb, S=4096):
    nc, P = kb.nc, kb.P
    fm = 'mixT_d' in kb.over
    mixT_d = kb.over.get('mixT_d')
    NH = (S // 512) // 2
    NCk = S // 512
    NT = S // 128
    SC = 128 ** -0.5
    LNSC = math.log(SC)
    hT = kb.din("hT", [1024, S])
    wq = kb.din("wq", [1024, 512]); wk = kb.din("wk", [1024, 512]); wv = kb.din("wv", [1024, 512]); wo = kb.din("wo", [1024, 512])
    wi = kb.din("wi", [1024, 4]); wf = kb.din("wf", [1024, 4])
    cw = kb.din("cw", [128, 8, 4]); cb = kb.din("cb", [128, 8])
    ibias = kb.din("ibias", [4, 1]); fbias = kb.din("fbias", [4, 1])
    mixg = kb.dout("mixg", [S, 512]) if not fm else None

    ident = make_ident(kb)
    hTb = kb.sb("hTb", [128, 2, 8, 512], BF16)
    wq_sb = kb.sb("wq_sb", [128, 8, 512], BF16); wk_sb = kb.sb("wk_sb", [128, 8, 512], BF16)
    wv_sb = kb.sb("wv_sb", [128, 8, 512], BF16); wo_sb = kb.sb("wo_sb", [128, 8, 512], BF16)
    wi_sb = kb.sb("wi_sb", [128, 8, 4], BF16); wf_sb = kb.sb("wf_sb", [128, 8, 4], BF16)
    cw_sb = kb.sb("cw_sb", [128, 8, 4]); cb_sb = kb.sb("cb_sb", [128, 8])
    ib_sb = [kb.sb("ib0", [2, 1]), kb.sb("ib1", [2, 1])]
    fb_sb = [kb.sb("fb0", [2, 1]), kb.sb("fb1", [2, 1])]
    QT = kb.sb("QT", [128, 2, S], BF16)
    KT = kb.sb("KT", [128, 2, S], BF16)
    V = kb.sb("V", [128, NT, 2, 132], BF16)
    OG = kb.sb("OG", [128, NT, 256], BF16)
    gi = kb.sb("gi", [2, S]); gf = kb.sb("gf", [2, S])
    ones2 = kb.sb("ones2", [2, 512])
    selF = kb.sb("selF", [2, 2, 128])
    nbT = kb.sb("nbT", [128, NT, 2])
    pre = kb.sb("pre", [128, 4, 515])
    acc = kb.sb("acc", [128, 4, 512])
    FT = kb.sb("FT", [128, NT, 2])
    fr = kb.sb("fr", [2, NCk])
    frefP = kb.sb("frefP", [128, 2, NCk]); frefN = kb.sb("frefN", [128, 2, NCk])
    RF = kb.sb("RF", [128, 2, NT])
    cfc = kb.sb("cfc", [128, 4])
    W = kb.sb("W", [128, 3, 512], BF16)
    otmp = kb.sb("otmp", [128, 4, 129])
    rec = kb.sb("rec", [128, 4])
    ost = kb.sb("ost", [128, 2, 128])
    mst = kb.sb("mst", [128, 2, 512], BF16)
    pA = kb.ps("pA", [128, 512]); pB = kb.ps("pB", [128, 512])
    pS = [kb.ps("pS0", [128, 512]), kb.ps("pS1", [128, 512])]
    pO = [kb.ps("pO%d" % u, [128, 512]) for u in range(4)]

    for (wsb, wdr, nm) in ((wq_sb, wq, "wq"), (wk_sb, wk, "wk"), (wv_sb, wv, "wv"), (wo_sb, wo, "wo"), (wi_sb, wi, "wi"), (wf_sb, wf, "wf")):
        P.add("gpsimd", lambda e, wsb=wsb, wdr=wdr: e.dma_start(out=wsb[:], in_=wdr.rearrange("(k p) n -> p k n", p=128)), writes=[nm], dma=True)
    P.add("sync", lambda e: e.dma_start(out=cw_sb[:], in_=cw[:, :, :]), writes=["cw"], dma=True)
    P.add("sync", lambda e: e.dma_start(out=cb_sb[:], in_=cb[:, :]), writes=["cb"], dma=True)
    for hp in range(2):
        P.add("sync", lambda e, hp=hp: e.dma_start(out=ib_sb[hp][:], in_=ibias[hp * 2:hp * 2 + 2, :]), writes=[("ib", hp)], dma=True)
        P.add("sync", lambda e, hp=hp: e.dma_start(out=fb_sb[hp][:], in_=fbias[hp * 2:hp * 2 + 2, :]), writes=[("fb", hp)], dma=True)
    P.add("vector", lambda e: e.memset(ones2[:], 1.0), writes=["ones2"])
    P.add("vector", lambda e: e.memset(V[:, :, :, 128:129], 1.0), writes=["V1"])
    P.add("gpsimd", lambda e: e.memset(selF[:], 1.0), writes=["selF"])
    P.add("gpsimd", lambda e: e.affine_select(out=selF[:], in_=selF[:], pattern=[[-1, 2], [0, 128]], compare_op=ALU.is_equal, fill=0.0, base=0,
                                              channel_multiplier=1), reads=["selF"], writes=["selF"])
    cnt = 0
    for hp in range(2):
        P.add("vector", lambda e: e.memset(pre[:, :, 0:3], 0.0), reads=[("pre", i) for i in range(4)], writes=[("pre", i) for i in range(4)])
        for c in range(NCk):
            hb = c % 2
            csl = slice(c * 512, (c + 1) * 512)
            load_hT(kb, hTb, hb, c, hT)
            for hl in range(2):
                h = hp * 2 + hl
                for qk, (wsb, wn, dst, pb, pk) in enumerate(((wq_sb, "wq", QT, [pA, pS[0]][hl], ["pA", "pS0"][hl]),
                                                              (wk_sb, "wk", KT, [pB, pS[1]][hl], ["pB", "pS1"][hl]))):
                    idx = qk * 2 + hl
                    ci = qk * 4 + h
                    ab = qk * 2 + hl
                    for k in range(8):
                        P.add("tensor", lambda e, k=k, h=h, wsb=wsb, pb=pb, hb=hb: e.matmul(pb[:], lhsT=wsb[:, k, h * 128:(h + 1) * 128], rhs=hTb[:, hb, k, :],
                                                                                           start=(k == 0), stop=(k == 7)),
                              reads=[("hTb", hb), wn], writes=[pk])
                    P.add("scalar", lambda e, idx=idx, pb=pb: e.activation(out=pre[:, idx, 3:515], in_=pb[:], func=AF.Copy),
                          reads=[pk, ("pre", idx)], writes=[("pre", idx)])
                    P.add("vector", lambda e, idx=idx, ci=ci, ab=ab: e.tensor_scalar(out=acc[:, ab, :], in0=pre[:, idx, 0:512], scalar1=cw_sb[:, ci, 0:1],
                                                                                     scalar2=None, op0=ALU.mult),
                          reads=[("pre", idx), "cw"], writes=[("acc", ab)])
                    for jj in range(1, 4):
                        P.add("vector", lambda e, idx=idx, ci=ci, ab=ab, jj=jj: e.scalar_tensor_tensor(out=acc[:, ab, :], in0=pre[:, idx, jj:jj + 512],
                                                                                                      scalar=cw_sb[:, ci, jj:jj + 1], in1=acc[:, ab, :],
                                                                                                      op0=ALU.mult, op1=ALU.add),
                              reads=[("pre", idx), "cw", ("acc", ab)], writes=[("acc", ab)])
                    P.add("scalar", lambda e, dst=dst, hl=hl, ab=ab, ci=ci, csl=csl: e.activation(out=dst[:, hl, csl], in_=acc[:, ab, :], func=AF.Silu,
                                                                                                 bias=cb_sb[:, ci:ci + 1], scale=1.0),
                          reads=[("acc", ab), "cb"], writes=[(wn + "o", hl, c)])
                    P.add("vector", lambda e, idx=idx: e.tensor_copy(out=pre[:, idx, 0:3], in_=pre[:, idx, 512:515]),
                          reads=[("pre", idx)], writes=[("pre", idx)])
            for (wsb, wn, gt, gk, bs, bk, u) in ((wi_sb, "wi", gi, "gi", ib_sb[hp], ("ib", hp), 0), (wf_sb, "wf", gf, "gf", fb_sb[hp], ("fb", hp), 1)):
                for k in range(8):
                    P.add("tensor", lambda e, k=k, wsb=wsb, u=u, hb=hb, hp=hp: e.matmul(pO[u][0:2, :], lhsT=wsb[:, k, hp * 2:hp * 2 + 2], rhs=hTb[:, hb, k, :],
                                                                                start=(k == 0), stop=(k == 7)),
                          reads=[("hTb", hb), wn], writes=[("pO", u)])
                P.add("scalar", lambda e, gt=gt, bs=bs, u=u, csl=csl: e.activation(out=gt[:, csl], in_=pO[u][0:2, :], func=AF.Identity, bias=bs[:], scale=1.0),
                      reads=[("pO", u), bk], writes=[(gk, c), gk + "2"])
            for t4 in range(4):
                t = c * 4 + t4
                vo = (t % 2) * 256
                for k in range(8):
                    P.add("tensor", lambda e, k=k, t4=t4, hb=hb, hp=hp, vo=vo: e.matmul(pO[2][:, vo:vo + 256], lhsT=hTb[:, hb, k, t4 * 128:(t4 + 1) * 128],
                                                                           rhs=wv_sb[:, k, hp * 256:(hp + 1) * 256], start=(k == 0), stop=(k == 7)),
                          reads=[("hTb", hb), "wv"], writes=[("pO", 2, vo), ("pO", 2)])
                P.add("vector", lambda e, t=t, vo=vo: e.tensor_copy(out=V[:, t, :, 0:128], in_=pO[2][:, vo:vo + 256].rearrange("p (h d) -> p h d", h=2)),
                      reads=[("pO", 2, vo)], writes=[("V", t)])
                for k in range(8):
                    P.add("tensor", lambda e, k=k, t4=t4, hb=hb, hp=hp, vo=vo: e.matmul(pO[3][:, vo:vo + 256], lhsT=hTb[:, hb, k, t4 * 128:(t4 + 1) * 128],
                                                                           rhs=wo_sb[:, k, hp * 256:(hp + 1) * 256], start=(k == 0), stop=(k == 7)),
                          reads=[("hTb", hb), "wo"], writes=[("pO", 3, vo), ("pO", 3)])
                P.add("scalar", lambda e, t=t, vo=vo: e.activation(out=OG[:, t, :], in_=pO[3][:, vo:vo + 256], func=AF.Sigmoid),
                      reads=[("pO", 3, vo)], writes=[("OG", t)])
        gfk = [("gf", c) for c in range(NCk)]
        gik = [("gi", c) for c in range(NCk)]
        P.add("scalar", lambda e: e.activation(out=gf[:], in_=gf[:], func=AF.Exp, scale=-1.0), reads=gfk, writes=["gf2"])
        P.add("scalar", lambda e: e.activation(out=gf[:], in_=gf[:], func=AF.Ln, bias=1.0, scale=1.0), reads=["gf2"], writes=["gf2"])
        P.add("vector", lambda e: e.tensor_scalar(out=gf[:], in0=gf[:], scalar1=-1.0, scalar2=None, op0=ALU.mult), reads=["gf2"], writes=["gf2"])
        for c in range(NCk):
            csl = slice(c * 512, (c + 1) * 512)
            ini = 0.0 if c == 0 else gf[:, c * 512 - 1:c * 512]
            P.add("vector", lambda e, csl=csl, ini=ini: e.tensor_tensor_scan(out=gf[:, csl], data0=ones2[:], data1=gf[:, csl], initial=ini, op0=ALU.mult, op1=ALU.add),
                  reads=["gf2", "ones2"], writes=["gf2"])
        P.add("vector", lambda e: e.tensor_tensor(out=gi[:], in0=gi[:], in1=gf[:], op=ALU.subtract), reads=gik + ["gf2"], writes=["gi2"])
        for t in range(NT):
            P.add("tensor", lambda e, t=t: e.transpose(out=pB[:, t * 2:(t + 1) * 2], in_=gi[:, t * 128:(t + 1) * 128], identity=ident[0:2, 0:2]),
                  reads=["gi2", "ident"], writes=["pB"])
        P.add("vector", lambda e: e.tensor_copy(out=nbT[:].rearrange("p t h -> p (t h)"), in_=pB[:, 0:NT * 2]), reads=["pB"], writes=["nbT"])
        for t in range(NT):
            P.add("tensor", lambda e, t=t: e.transpose(out=pA[:, t * 2:(t + 1) * 2], in_=gf[:, t * 128:(t + 1) * 128], identity=ident[0:2, 0:2]),
                  reads=["gf2", "ident"], writes=["pA"])
        P.add("vector", lambda e: e.tensor_copy(out=FT[:].rearrange("p t h -> p (t h)"), in_=pA[:, 0:NT * 2]), reads=["pA"], writes=["FT"])
        P.add("vector", lambda e: e.memset(fr[:], 0.0), writes=["fr"])
        if NCk > 1:
            P.add("vector", lambda e: e.tensor_copy(out=fr[:, 1:NCk], in_=gf[:].rearrange("p (c t) -> p c t", t=512)[:, 0:NCk - 1, 511]),
                  reads=["gf2", "fr"], writes=["fr"])
        for hl in range(2):
            P.add("tensor", lambda e, hl=hl: e.matmul(pA[:, 256 + hl * 16:256 + hl * 16 + NCk], lhsT=selF[:, hl, :], rhs=fr[:], start=True, stop=True),
                  reads=["fr", "selF"], writes=["pA"])
            P.add("vector", lambda e, hl=hl: e.tensor_scalar(out=frefP[:, hl, :], in0=pA[:, 256 + hl * 16:256 + hl * 16 + NCk], scalar1=LNSC, scalar2=None, op0=ALU.add),
                  reads=["pA"], writes=["fref"])
            P.add("vector", lambda e, hl=hl: e.tensor_scalar(out=frefN[:, hl, :], in0=pA[:, 256 + hl * 16:256 + hl * 16 + NCk], scalar1=-1.0, scalar2=None, op0=ALU.mult),
                  reads=["pA"], writes=["fref"])
        tiles = []
        for hl in range(2):
            h = hp * 2 + hl
            for j in range(NCk):
                for i in range(4 * j + 4):
                    r = i - 4 * j
                    q0 = 128 * r if r > 0 else 0
                    sb_ = cnt % 3
                    cnt += 1
                    ps = [pS[0], pS[1], pA][sb_]
                    def fS(hl=hl, i=i, j=j, q0=q0, ps=ps, sb_=sb_):
                        P.add("tensor", lambda e, hl=hl, i=i, j=j, q0=q0, ps=ps: e.matmul(ps[:, q0:512], lhsT=KT[:, hl, i * 128:(i + 1) * 128],
                                                                                         rhs=QT[:, hl, j * 512 + q0:(j + 1) * 512], start=True, stop=True),
                              reads=[("wqo", hl, j), ("wko", hl, i // 4)], writes=[["pS0", "pS1", "pA"][sb_]])
                    def fR(hl=hl, h=h, i=i, j=j, r=r, q0=q0, ps=ps, sb_=sb_):
                        rb = (hl * NCk + j) % 2
                        if i == 0:
                            nt = 4 * j + 4
                            P.add("scalar", lambda e, hl=hl, j=j, nt=nt, rb=rb: e.activation(out=RF[:, rb, 0:nt], in_=nbT[:, 0:nt, hl], func=AF.Exp,
                                                                                            bias=frefP[:, hl, j:j + 1], scale=1.0),
                                  reads=["nbT", "fref"], writes=[("RF", rb)])
                        P.add("scalar", lambda e, i=i, q0=q0, sb_=sb_, ps=ps, rb=rb: e.activation(out=W[:, sb_, q0:512], in_=ps[:, q0:512], func=AF.Copy,
                                                                                                 scale=RF[:, rb, i:i + 1]),
                              reads=[["pS0", "pS1", "pA"][sb_], ("RF", rb)], writes=[("W", sb_)])
                        if r >= 0:
                            P.add("gpsimd", lambda e, sb_=sb_, q0=q0: e.affine_select(out=W[:, sb_, q0:q0 + 128], in_=W[:, sb_, q0:q0 + 128], pattern=[[1, 128]],
                                                                                       compare_op=ALU.is_ge, fill=0.0, base=0, channel_multiplier=-1),
                                  reads=[("W", sb_)], writes=[("W", sb_)])
                        for u in range(max(r, 0), 4):
                            P.add("tensor", lambda e, hl=hl, i=i, u=u, sb_=sb_, j=j: e.matmul(pO[u][:, 0:129], lhsT=W[:, sb_, u * 128:(u + 1) * 128],
                                                                                             rhs=V[:, i, hl, 0:129], start=(i == 0), stop=(i == 4 * j + u)),
                                  reads=[("W", sb_), ("V", i), "V1"], writes=[("pO", u)])
                            if i == 4 * j + u:
                                tt = j * 4 + u
                                ob = tt % 2
                                P.add("scalar", lambda e, u=u, tt=tt, hl=hl, j=j: e.activation(out=cfc[:, u:u + 1], in_=FT[:, tt, hl:hl + 1], func=AF.Exp,
                                                                                              bias=frefN[:, hl, j:j + 1], scale=1.0),
                                      reads=["FT", "fref"], writes=[("cfc", u)])
                                P.add("vector", lambda e, u=u: e.tensor_scalar(out=otmp[:, u, :], in0=pO[u][:, 0:129], scalar1=cfc[:, u:u + 1], scalar2=None, op0=ALU.mult),
                                      reads=[("pO", u), ("cfc", u)], writes=[("otmp", u)])
                                P.add("scalar", lambda e, u=u: e.activation(out=rec[:, u:u + 1], in_=otmp[:, u, 128:129], func=AF.Abs),
                                      reads=[("otmp", u)], writes=[("rec", u)])
                                P.add("vector", lambda e, u=u: e.tensor_scalar(out=rec[:, u:u + 1], in0=rec[:, u:u + 1], scalar1=1.0, scalar2=None, op0=ALU.max),
                                      reads=[("rec", u)], writes=[("rec", u)])
                                P.add("vector", lambda e, u=u: e.reciprocal(out=rec[:, u:u + 1], in_=rec[:, u:u + 1]), reads=[("rec", u)], writes=[("rec", u)])
                                P.add("vector", lambda e, u=u, ob=ob, hl=hl, tt=tt: e.scalar_tensor_tensor(out=ost[:, ob, :], in0=otmp[:, u, 0:128], scalar=rec[:, u:u + 1],
                                                                                                          in1=OG[:, tt, hl * 128:(hl + 1) * 128], op0=ALU.mult, op1=ALU.mult),
                                      reads=[("otmp", u), ("rec", u), ("OG", tt)], writes=[("ost", ob)])
                                if not fm:
                                    P.add("sync", lambda e, ob=ob, h=h, tt=tt: e.dma_start(out=mixg[tt * 128:(tt + 1) * 128, h * 128:(h + 1) * 128], in_=ost[:, ob, :]),
                                          reads=[("ost", ob)], dma=True, out=True)
                                else:
                                    def fin(h=h, j=j, u=u, ob=ob):
                                        stb = (h * NCk + j) % 2
                                        P.add("tensor", lambda e, ob=ob, u=u: e.transpose(out=pB[:, u * 128:(u + 1) * 128], in_=ost[:, ob, :], identity=ident[:]),
                                              reads=[("ost", ob), "ident"], writes=["pB"])
                                        P.add("scalar", lambda e, u=u, stb=stb: e.activation(out=mst[:, stb, u * 128:(u + 1) * 128], in_=pB[:, u * 128:(u + 1) * 128], func=AF.Copy),
                                              reads=["pB"], writes=[("mst", stb)])
                                        if u == 3:
                                            half, tl = j // NH, (j % NH) * 512
                                            P.add("sync", lambda e, stb=stb, h=h, half=half, tl=tl: e.dma_start(out=mixT_d[half, h * 128:(h + 1) * 128, tl:tl + 512], in_=mst[:, stb, :]),
                                                  reads=[("mst", stb)], dma=True)
                                    deferred.append(fin)
                    tiles.append((fS, fR))
        deferred = []
        for n in range(len(tiles)):
            if n == 0:
                tiles[0][0]()
                if len(tiles) > 1:
                    tiles[1][0]()
            if n + 2 < len(tiles):
                tiles[n + 2][0]()
            pend = list(deferred)
            del deferred[:]
            tiles[n][1]()
            for f in pend:
                f()
        for f in deferred:
            f()


def build_odd(S=4096):
    kb = KB()
    make_ident(kb)
    kb.mark()
    emit_odd(kb, S)
    print("odd ops", kb.P.nops(), "peak words", kb.peak)
    return kb.finish()


GROUPS = [[0, 1], [2, 3], [4, 5], [6, 7]]
SEQ = 4096


def _ag(kb, src, dst, rd=(), wr=()):
    kb.P.add("gpsimd", lambda e: e.collective_compute("AllGather", ALU.bypass, replica_groups=GROUPS, ins=[src], outs=[dst]),
             reads=list(rd), writes=list(wr), cc=True)


def build_fused():
    kb = KB()
    make_ident(kb)
    kb.mark()
    S, H = SEQ, SEQ // 2
    xT = kb.din("xT", [1024, S])
    xh = kb.din("xh", [1024, H])
    attT_d = kb.dint("attT_d", [2, 256, H], BF16)
    attG_d = kb.dint("attG_d", [1024, H], BF16)
    ysT_d = kb.dint("ysT_d", [2, 256, H], F32)
    ysG_d = kb.dint("ysG_d", [1024, H], F32)
    mixT_d = kb.dint("mixT_d", [2, 512, H], BF16)
    mixG_d = kb.dint("mixG_d", [2048, H], BF16)
    hres_d = kb.dint("hres_d", [1024, H], F32)
    hb_d = kb.dint("hb_d", [2, 1024, H // 2], BF16)
    hg_d = kb.dint("hg_d", [2, 2048, H // 2], BF16)
    att_m = kb.dint("att_m", [512, H], BF16)
    ys_m = kb.dint("ys_m", [512, H], F32)
    mix_m = kb.dint("mix_m", [1024, H], BF16)

    kb.P.use_pid = True

    def rk(e):
        return kb.P.pidval % 2

    for layer in range(4):
        even = (layer % 2 == 0)
        if layer == 0:
            kb.hmode = ("ext", None)
            hsrc = xT
        else:
            kb.hmode = ("gath", hg_d)
            hsrc = None
        kb.phase(wait_cc=False)
        if even:
            kb.prefix = "L%df_" % layer
            kb.over = {"hT": hsrc, "attT_d": attT_d}
            emit_even(kb, S, 'fox')
            kb.phase()
            for k in range(2):
                _ag(kb, attT_d[k], attG_d[k * 512:(k + 1) * 512, :], wr=["attG"])
            kb.prefix = "L%ds_" % layer
            kb.over = {"hT": hsrc, "ysT_d": ysT_d,
                       "ys_cb": lambda half, keys: _ag(kb, ysT_d[half], ysG_d[half * 512:(half + 1) * 512, :], rd=keys, wr=["ysG"])}
            emit_even(kb, S, 's5')
            kb.phase(wait_cc=False)
            kb.P.add("sync", lambda e: e.dma_start(out=att_m[:, :], in_=attG_d[bass.ds(rk(e) * 512, 512), :]), reads=["attG"], writes=["xsel"], dma=True)
            kb.P.add("sync", lambda e: e.dma_start(out=ys_m[:, :], in_=ysG_d[bass.ds(rk(e) * 512, 512), :]), reads=["ysG"], writes=["xsel2"], dma=True)
        else:
            kb.prefix = "L%dm_" % layer
            kb.over = {"hT": hsrc, "mixT_d": mixT_d}
            emit_odd(kb, S)
            kb.phase()
            for k in range(2):
                _ag(kb, mixT_d[k], mixG_d[k * 1024:(k + 1) * 1024, :], wr=["mixG"])
            kb.P.add("sync", lambda e: e.dma_start(out=mix_m[:, :], in_=mixG_d[bass.ds(rk(e) * 1024, 1024), :]), reads=["mixG"], writes=["xsel"], dma=True)
        kb.prefix = "L%dp_" % layer
        ov = {"hT": xh if layer == 0 else hres_d, "xkeys": ["xsel", "xsel2"] if even else ["xsel"]}
        if even:
            ov["attT"] = att_m
            ov["ysT"] = ys_m
        else:
            ov["mixT"] = mix_m
        if layer < 3:
            ov["houtT"] = hres_d
            ov["hbT"] = hb_d
            ov["hb_cb"] = lambda t2, keys: _ag(kb, hb_d[t2], hg_d[t2], rd=keys, wr=["hg"])
        kb.over = ov
        emit_post(kb, even, TOK)
    print("fused ops", kb.P.nops(), "peak words", kb.peak, "phases", kb.P.phase + 1)
    return kb.finish()


def _even_inputs(j, hh, d):
    w_in = d['even_w_in'][j]
    hs = slice(hh * 256, (hh + 1) * 256)
    g0 = hh * 16
    c = np.ascontiguousarray

    def pl(a):
        return c(a[g0:g0 + 16].reshape(8, 2, 64).transpose(1, 2, 0).reshape(128, 8))

    def plb(a):
        return c(a[g0:g0 + 16].reshape(8, 2, 64, 16).transpose(1, 2, 0, 3).reshape(128, 8, 16))

    def plc(a):
        return c(a[g0:g0 + 16].reshape(8, 2, 16, 64).transpose(1, 3, 0, 2).reshape(128, 8, 16))
    ldt = np.repeat(d['s5_log_dt'][j][:, None], 64, 1)
    fox = dict(wq=c(w_in[:, 0:512][:, hs]), wk=c(w_in[:, 512:1024][:, hs]), wv=c(w_in[:, 1024:1536][:, hs]),
               wf=c(w_in[:, 1536 + hh * 4:1536 + hh * 4 + 4]), fbias=c(d['fox_f_bias'][j][hh * 4:hh * 4 + 4, None]))
    s5 = dict(wu=c(w_in[:, 1544:][:, hs]), are=pl(d['s5_a_re'][j]), aim=pl(d['s5_a_im'][j]), ldt=pl(ldt),
              bre=plb(d['s5_b_re'][j]), bim=plb(d['s5_b_im'][j]), cre=plc(d['s5_c_re'][j]), cim=plc(d['s5_c_im'][j]),
              dsk=c(d['s5_d'][j][g0:g0 + 16].reshape(2, 128).T), jrow=np.tile(np.arange(512, dtype=np.float32), (128, 1)))
    return fox, s5


def _odd_inputs(j, hh, d):
    c = np.ascontiguousarray
    w_in = d['odd_w_in'][j]
    cs = slice(hh * 512, (hh + 1) * 512)
    cwf = d['mlstm_conv_w'][j]
    cbf = d['mlstm_conv_b'][j]
    qcols = np.arange(hh * 512, (hh + 1) * 512)
    cols = np.concatenate([qcols, 1024 + qcols])
    cw = c(cwf[:, cols].reshape(4, 8, 128).transpose(2, 1, 0))
    cb = c(cbf[cols].reshape(8, 128).T)
    return dict(wq=c(w_in[:, 0:1024][:, cs]), wk=c(w_in[:, 1024:2048][:, cs]), wv=c(w_in[:, 2048:3072][:, cs]),
                wo=c(w_in[:, 3072:4096][:, cs]), wi=c(w_in[:, 4096 + hh * 4:4096 + hh * 4 + 4]),
                wf=c(w_in[:, 4104 + hh * 4:4104 + hh * 4 + 4]), cw=cw, cb=cb,
                ibias=c(d['mlstm_i_bias'][j][hh * 4:hh * 4 + 4, None]), fbias=c(d['mlstm_f_bias'][j][hh * 4:hh * 4 + 4, None]))


def kernel(**d):
    d = {k: np.asarray(v) for k, v in d.items()}
    x = d['x'].astype(np.float32)
    B, S, D = x.shape
    cores = list(range(8))
    c = np.ascontiguousarray
    shared = {}
    for layer in range(4):
        j = layer // 2
        wr = c(np.concatenate([d['moe_w_group'][layer], d['moe_w_expert'][layer].transpose(1, 0, 2).reshape(1024, 16)], 1))
        br = c(np.concatenate([d['moe_b_group'][layer], d['moe_b_expert'][layer].reshape(16)]))
        lnp = c(np.stack([d['ln_g'][layer, 0], d['ln_b'][layer, 0], d['ln_g'][layer, 1], d['ln_b'][layer, 1]]))
        p = dict(lnp=lnp, wr=wr, br=br, wg=d['moe_w_gate'][layer], wu=d['moe_w_up'][layer], wd=d['moe_w_down'][layer])
        if layer % 2 == 0:
            p.update(w_glu=d['s5_w_glu'][j], b_glu=d['s5_b_glu'][j], w_out=d['even_w_out'][j])
        else:
            p.update(w_out=d['odd_w_out'][j])
        for k, v in p.items():
            shared["L%dp_%s" % (layer, k)] = c(v)
    in_maps = []
    for core in cores:
        b, r = core // 2, core % 2
        im = dict(shared)
        im["xT"] = c(x[b].T)
        im["xh"] = c(x[b, r * (S // 2):(r + 1) * (S // 2)].T)
        for layer in range(4):
            j = layer // 2
            if layer % 2 == 0:
                f_, s_ = _even_inputs(j, r, d)
                for k, v in f_.items():
                    im["L%df_%s" % (layer, k)] = v
                for k, v in s_.items():
                    im["L%ds_%s" % (layer, k)] = v
            else:
                for k, v in _odd_inputs(j, r, d).items():
                    im["L%dm_%s" % (layer, k)] = v
        in_maps.append(im)
    res = run_bass_kernel_spmd(build_fused(), in_maps, core_ids=cores).results
    out = np.zeros((B, S, D), np.float32)
    for core in cores:
        b, r = core // 2, core % 2
        out[b, r * (S // 2):(r + 1) * (S // 2), :] = res[core]["L3p_houtT"].T
    return out
```
